# Optimizing a Trainium2 kernel written in Bass

```python
import math
import jax, jax.numpy as jnp
from jax import lax
import numpy as np

D_MODEL = 1024
BATCH = 32
SEQ = 2048
DEPTH = 4

GRID_W = 64
ROPE_THETA = 500000.0
Q_BLOCK = 128

A_HEADS = 4
A_HEAD_DIM = 64
A_ROT = A_HEAD_DIM // 4
A_Q = A_HEADS * 2 * A_HEAD_DIM
A_K = A_HEADS * 2 * A_HEAD_DIM
A_V = A_HEADS * 2 * A_HEAD_DIM

B_HEADS = 8
B_NOPE = 64
B_ROPE = 32
B_V = 64
B_Q_RANK = 256
B_KV_RANK = 128

C_HEADS = 16
C_HEAD_DIM = 64
C_WIDTH = C_HEADS * C_HEAD_DIM
NA_ROWS = 8
NA_COLS = 16

N_GROUPS = 4
EXPERTS_PER_GROUP = 8
N_EXPERTS = N_GROUPS * EXPERTS_PER_GROUP
TOP_K = 2
D_EXPERT = 512
MOE_BLOCK = 128

DN_ALPHA = (2 * DEPTH) ** 0.25
DN_BETA = (8 * DEPTH) ** -0.25
LN_EPS = 1e-5
RMS_EPS = 1e-6

EVEN_IN = A_Q + A_K + A_V + B_Q_RANK + B_KV_RANK + B_ROPE
EVEN_SPLITS = (A_Q, A_Q + A_K, A_Q + A_K + A_V, A_Q + A_K + A_V + B_Q_RANK,
               A_Q + A_K + A_V + B_Q_RANK + B_KV_RANK)
EVEN_OUT = A_HEADS * 2 * A_HEAD_DIM + B_HEADS * B_V
N_EVEN = (DEPTH + 1) // 2
N_ODD = DEPTH // 2

kernel_name = "hybrid_diffattn_mla_natten_hmoe_encoder"


def layer_norm(x, g, b):
    xf = x.astype(jnp.float32)
    mu = jnp.mean(xf, axis=-1, keepdims=True)
    var = jnp.mean(jnp.square(xf - mu), axis=-1, keepdims=True)
    y = (xf - mu) * lax.rsqrt(var + LN_EPS) * g.astype(jnp.float32) + b.astype(jnp.float32)
    return y.astype(x.dtype)


def rms_norm(x, g):
    xf = x.astype(jnp.float32)
    y = xf * lax.rsqrt(jnp.mean(xf * xf, axis=-1, keepdims=True) + RMS_EPS) * g.astype(jnp.float32)
    return y.astype(x.dtype)


def rope_tables(seq, rot_dim):
    inv_freq = ROPE_THETA ** (-jnp.arange(0, rot_dim, 2, dtype=jnp.float32) / rot_dim)
    ang = jnp.arange(seq, dtype=jnp.float32)[:, None] * inv_freq[None, :]
    return jnp.cos(ang), jnp.sin(ang)


def apply_rope(x, cos, sin):
    half = cos.shape[-1]
    r = 2 * half
    cos = cos.astype(x.dtype)
    sin = sin.astype(x.dtype)
    x1 = x[..., :half]
    x2 = x[..., half:r]
    return jnp.concatenate([x1 * cos - x2 * sin, x2 * cos + x1 * sin, x[..., r:]], axis=-1)


def diff_attention(q, k, v, lam, subln_g, lambda_init, cos, sin):
    B, S, H, _, d = q.shape
    nb = S // Q_BLOCK
    q = apply_rope(q.transpose(0, 2, 3, 1, 4), cos, sin)
    k = apply_rope(k.transpose(0, 2, 3, 1, 4), cos, sin)
    v = v.transpose(0, 2, 1, 3)
    q_blocks = q.reshape(B, H, 2, nb, Q_BLOCK, d).transpose(3, 0, 1, 2, 4, 5)
    scale = d ** -0.5

    def block(qb):
        s = jnp.einsum('bhmqd,bhmkd->bhmqk', qb, k).astype(jnp.float32) * scale
        p = jax.nn.softmax(s, axis=-1)
        w = p[:, :, 0] - lam * p[:, :, 1]
        return jnp.einsum('bhqk,bhke->bhqe', w.astype(v.dtype), v)

    o = lax.map(block, q_blocks)
    o = rms_norm(o, subln_g) * (1.0 - lambda_init)
    return o.transpose(1, 0, 3, 2, 4).reshape(B, S, H * 2 * d)


def latent_attention(c_q, c_kv, k_r, q_norm_g, w_uq, kv_norm_g, w_ukv, cos, sin):
    B, S, _ = c_q.shape
    H = B_HEADS
    nb = S // Q_BLOCK
    q = (rms_norm(c_q, q_norm_g) @ w_uq).reshape(B, S, H, B_NOPE + B_ROPE).transpose(0, 2, 1, 3)
    q_nope = q[..., :B_NOPE]
    q_rope = apply_rope(q[..., B_NOPE:], cos, sin)
    kv = (rms_norm(c_kv, kv_norm_g) @ w_ukv).reshape(B, S, H, B_NOPE + B_V).transpose(0, 2, 1, 3)
    k_nope = kv[..., :B_NOPE]
    v = kv[..., B_NOPE:]
    k_rope = apply_rope(k_r, cos, sin)
    qn_blocks = q_nope.reshape(B, H, nb, Q_BLOCK, B_NOPE).transpose(2, 0, 1, 3, 4)
    qr_blocks = q_rope.reshape(B, H, nb, Q_BLOCK, B_ROPE).transpose(2, 0, 1, 3, 4)
    scale = (B_NOPE + B_ROPE) ** -0.5

    def block(args):
        qn, qr = args
        s = (jnp.einsum('bhqd,bhkd->bhqk', qn, k_nope)
             + jnp.einsum('bhqr,bkr->bhqk', qr, k_rope)).astype(jnp.float32) * scale
        p = jax.nn.softmax(s, axis=-1)
        return jnp.einsum('bhqk,bhke->bhqe', p.astype(v.dtype), v)

    o = lax.map(block, (qn_blocks, qr_blocks))
    return o.transpose(1, 0, 3, 2, 4).reshape(B, S, H * B_V)


def neighbourhood_attention(q, k, v, rel_bias):
    B, S, H, d = q.shape
    rows = S // GRID_W
    kh = min(NA_ROWS, rows)
    kw = NA_COLS
    to_grid = lambda t: t.reshape(B, rows, GRID_W, H, d).transpose(0, 3, 1, 2, 4)
    qg, kg, vg = to_grid(q), to_grid(k), to_grid(v)
    cols = jnp.arange(GRID_W)
    col_start = jnp.clip(cols - kw // 2, 0, GRID_W - kw)
    col_idx = col_start[:, None] + jnp.arange(kw)[None, :]
    col_off = col_idx - cols[:, None] + (NA_COLS - 1)
    scale = d ** -0.5

    def row(args):
        r, q_row = args
        r0 = jnp.clip(r - kh // 2, 0, rows - kh)
        k_band = lax.dynamic_slice_in_dim(kg, r0, kh, axis=2)
        v_band = lax.dynamic_slice_in_dim(vg, r0, kh, axis=2)
        k_win = k_band[:, :, :, col_idx]
        v_win = v_band[:, :, :, col_idx]
        row_off = r0 + jnp.arange(kh) - r + (NA_ROWS - 1)
        bias = rel_bias[:, row_off[:, None, None], col_off[None]]
        bias = bias.transpose(0, 2, 1, 3).astype(jnp.float32)
        s = jnp.einsum('bhcd,bhicjd->bhcij', q_row, k_win).astype(jnp.float32) * scale + bias[None]
        p = jax.nn.softmax(s.reshape(B, H, GRID_W, kh * kw), axis=-1).reshape(B, H, GRID_W, kh, kw)
        return jnp.einsum('bhcij,bhicje->bhce', p.astype(v_win.dtype), v_win)

    o = lax.map(row, (jnp.arange(rows), qg.transpose(2, 0, 1, 3, 4)))
    return o.transpose(1, 0, 3, 2, 4).reshape(B, S, H * d)


def even_mixer(x, w_in, lam_q1, lam_k1, lam_q2, lam_k2, subln_g, q_norm_g, w_uq,
               kv_norm_g, w_ukv, w_o, lambda_init, rope_a, rope_b):
    B, S, _ = x.shape
    h = x @ w_in
    qa, ka, va, cq, ckv, kr = jnp.split(h, EVEN_SPLITS, axis=-1)
    f32 = jnp.float32
    lam = (jnp.exp(jnp.sum(lam_q1.astype(f32) * lam_k1.astype(f32)))
           - jnp.exp(jnp.sum(lam_q2.astype(f32) * lam_k2.astype(f32))) + lambda_init)
    a_out = diff_attention(qa.reshape(B, S, A_HEADS, 2, A_HEAD_DIM),
                           ka.reshape(B, S, A_HEADS, 2, A_HEAD_DIM),
                           va.reshape(B, S, A_HEADS, 2 * A_HEAD_DIM),
                           lam, subln_g, lambda_init, rope_a[0], rope_a[1])
    b_out = latent_attention(cq, ckv, kr, q_norm_g, w_uq, kv_norm_g, w_ukv, rope_b[0], rope_b[1])
    return jnp.concatenate([a_out, b_out], axis=-1) @ w_o


def odd_mixer(x, w_qkv, rel_bias, w_o):
    B, S, _ = x.shape
    q, k, v = jnp.split(x @ w_qkv, 3, axis=-1)
    shp = (B, S, C_HEADS, C_HEAD_DIM)
    o = neighbourhood_attention(q.reshape(shp), k.reshape(shp), v.reshape(shp), rel_bias)
    return o @ w_o


def hier_moe(x, w_rg, b_rg, w_re, b_re, w1, w3, w2):
    B, S, D = x.shape
    N = B * S
    xt = x.reshape(N, D)
    g_prob = jax.nn.softmax((xt @ w_rg).astype(jnp.float32) + b_rg.astype(jnp.float32), axis=-1)
    grp = jnp.argmax(g_prob, axis=-1).astype(jnp.int32)
    g_w = jnp.take_along_axis(g_prob, grp[:, None], axis=-1)
    e_logits = ((xt @ w_re).astype(jnp.float32) + b_re.astype(jnp.float32)).reshape(N, N_GROUPS, EXPERTS_PER_GROUP)
    idx = jnp.broadcast_to(grp[:, None, None], (N, 1, EXPERTS_PER_GROUP))
    e_logits = jnp.take_along_axis(e_logits, idx, axis=1)[:, 0]
    top_v, top_i = lax.top_k(e_logits, TOP_K)
    gate = jax.nn.softmax(top_v, axis=-1) * g_w
    expert = grp[:, None] * EXPERTS_PER_GROUP + top_i.astype(jnp.int32)
    A = N * TOP_K
    e_flat = expert.reshape(A)
    w_flat = gate.reshape(A)
    tok = jnp.repeat(jnp.arange(N, dtype=jnp.int32), TOP_K)
    order = jnp.argsort(e_flat)
    e_s, tok_s, w_s = e_flat[order], tok[order], w_flat[order]
    counts = jnp.bincount(e_flat, length=N_EXPERTS)
    start = jnp.cumsum(counts) - counts
    padded = (counts + MOE_BLOCK - 1) // MOE_BLOCK * MOE_BLOCK
    pad_end = jnp.cumsum(padded)
    pad_start = pad_end - padded
    dest = pad_start[e_s] + jnp.arange(A) - start[e_s]
    n_blocks = -(-A // MOE_BLOCK) + N_EXPERTS
    P = n_blocks * MOE_BLOCK
    slot_tok = jnp.full((P,), N, jnp.int32).at[dest].set(tok_s)
    slot_w = jnp.zeros((P,), jnp.float32).at[dest].set(w_s)
    block_exp = jnp.minimum(jnp.searchsorted(pad_end, jnp.arange(n_blocks) * MOE_BLOCK, side='right'),
                            N_EXPERTS - 1).astype(jnp.int32)
    x_pad = jnp.concatenate([xt, jnp.zeros((1, D), xt.dtype)], axis=0)

    def block(args):
        tids, e, w = args
        xb = x_pad[tids]
        hdn = jax.nn.silu(xb @ w1[e]) * (xb @ w3[e])
        return (hdn @ w2[e]) * w[:, None].astype(xb.dtype)

    y = lax.map(block, (slot_tok.reshape(n_blocks, MOE_BLOCK), block_exp,
                        slot_w.reshape(n_blocks, MOE_BLOCK)))
    out = jax.ops.segment_sum(y.reshape(P, D), slot_tok, num_segments=N + 1)[:N]
    return out.reshape(B, S, D)


def setup_inputs(seed: int = 0) -> dict:
    key = jax.random.key(seed)
    ks = iter(jax.random.split(key, 32))
    nrm = lambda shape, scale: jax.random.normal(next(ks), shape, jnp.float32) * scale
    gain = lambda shape: 1.0 + nrm(shape, 0.02)
    D = D_MODEL
    return {
        "x": nrm((BATCH, SEQ, D), 1.0),
        "even_w_in": nrm((N_EVEN, D, EVEN_IN), D ** -0.5),
        "even_lam_q1": nrm((N_EVEN, A_HEAD_DIM), 0.1),
        "even_lam_k1": nrm((N_EVEN, A_HEAD_DIM), 0.1),
        "even_lam_q2": nrm((N_EVEN, A_HEAD_DIM), 0.1),
        "even_lam_k2": nrm((N_EVEN, A_HEAD_DIM), 0.1),
        "even_subln_g": gain((N_EVEN, 2 * A_HEAD_DIM)),
        "even_q_norm_g": gain((N_EVEN, B_Q_RANK)),
        "even_w_uq": nrm((N_EVEN, B_Q_RANK, B_HEADS * (B_NOPE + B_ROPE)), B_Q_RANK ** -0.5),
        "even_kv_norm_g": gain((N_EVEN, B_KV_RANK)),
        "even_w_ukv": nrm((N_EVEN, B_KV_RANK, B_HEADS * (B_NOPE + B_V)), B_KV_RANK ** -0.5),
        "even_w_o": nrm((N_EVEN, EVEN_OUT, D), EVEN_OUT ** -0.5 * DN_BETA),
        "odd_w_qkv": nrm((N_ODD, D, 3 * C_WIDTH), D ** -0.5),
        "odd_rel_bias": nrm((N_ODD, C_HEADS, 2 * NA_ROWS - 1, 2 * NA_COLS - 1), 0.02),
        "odd_w_o": nrm((N_ODD, C_WIDTH, D), C_WIDTH ** -0.5 * DN_BETA),
        "ln1_g": gain((DEPTH, D)),
        "ln1_b": nrm((DEPTH, D), 0.02),
        "ln2_g": gain((DEPTH, D)),
        "ln2_b": nrm((DEPTH, D), 0.02),
        "w_router_group": nrm((DEPTH, D, N_GROUPS), D ** -0.5),
        "b_router_group": nrm((DEPTH, N_GROUPS), 0.01),
        "w_router_expert": nrm((DEPTH, D, N_EXPERTS), D ** -0.5),
        "b_router_expert": nrm((DEPTH, N_EXPERTS), 0.01),
        "w_gate": nrm((DEPTH, N_EXPERTS, D, D_EXPERT), D ** -0.5),
        "w_up": nrm((DEPTH, N_EXPERTS, D, D_EXPERT), D ** -0.5),
        "w_down": nrm((DEPTH, N_EXPERTS, D_EXPERT, D), D_EXPERT ** -0.5 * DN_BETA),
    }


def reference(x, even_w_in, even_lam_q1, even_lam_k1, even_lam_q2, even_lam_k2, even_subln_g,
              even_q_norm_g, even_w_uq, even_kv_norm_g, even_w_ukv, even_w_o,
              odd_w_qkv, odd_rel_bias, odd_w_o, ln1_g, ln1_b, ln2_g, ln2_b,
              w_router_group, b_router_group, w_router_expert, b_router_expert,
              w_gate, w_up, w_down):
    S = x.shape[1]
    rope_a = rope_tables(S, A_ROT)
    rope_b = rope_tables(S, B_ROPE)
    for i in range(DEPTH):
        j = i // 2
        if i % 2 == 0:
            lambda_init = 0.8 - 0.6 * math.exp(-0.3 * i)
            m = even_mixer(x, even_w_in[j], even_lam_q1[j], even_lam_k1[j], even_lam_q2[j],
                           even_lam_k2[j], even_subln_g[j], even_q_norm_g[j], even_w_uq[j],
                           even_kv_norm_g[j], even_w_ukv[j], even_w_o[j], lambda_init, rope_a, rope_b)
        else:
            m = odd_mixer(x, odd_w_qkv[j], odd_rel_bias[j], odd_w_o[j])
        x = layer_norm(DN_ALPHA * x + m, ln1_g[i], ln1_b[i])
        f = hier_moe(x, w_router_group[i], b_router_group[i], w_router_expert[i], b_router_expert[i],
                     w_gate[i], w_up[i], w_down[i])
        x = layer_norm(DN_ALPHA * x + f, ln2_g[i], ln2_b[i])
    return x
```

```python
import numpy as np
import concourse.bass as bass
import concourse.mybir as mybir
from contextlib import ExitStack

F32 = mybir.dt.float32
BF16 = mybir.dt.bfloat16
I32 = mybir.dt.int32
U32 = mybir.dt.uint32
AF = mybir.ActivationFunctionType
ALU = mybir.AluOpType
AX = mybir.AxisListType

SEM_LIMIT = 30000
MAXOPS = 10 ** 9


class Prog:
    def __init__(self, nc):
        self.nc = nc
        self.gstack = ExitStack()
        self.pstack = None
        self.engs = {"pe": nc.tensor, "act": nc.scalar, "dve": nc.vector, "pool": nc.gpsimd, "sp": nc.sync}
        self._nsem = 0
        self.eng_sem, self.eng_cnt, self.grp_sem, self.grp_cnt = {}, {}, {}, {}
        self.waited = {e: {} for e in self.engs}
        self.prev_final = {}
        self.cur_final = {}
        self.need_bar = set()
        self.tot = dict(n_ops=0, n_wait=0)
        self._nm = 0
        self.W, self.R, self.G = {}, {}, {}

    def sbuf(self, name, shape, dt, persist=False):
        self._nm += 1
        st = self.gstack if (persist or self.pstack is None) else self.pstack
        return st.enter_context(self.nc.sbuf_tensor("%s_%d" % (name, self._nm), list(shape), dt))

    def psum(self, name, shape, dt):
        return self.gstack.enter_context(self.nc.psum_tensor(name, list(shape), dt))

    def new_sem(self):
        self._nsem += 1
        return self.gstack.enter_context(self.nc.semaphore("s%d" % self._nsem))

    def begin(self):
        self.pstack = ExitStack()
        self.prev_final = dict(self.cur_final)
        self.need_bar = set(self.engs)
        self.W, self.R, self.G = {}, {}, {}

    def end(self):
        self.pstack.close()
        self.pstack = None

    def _do(self, engn, fn, reads, writes, partial, dma, group):
        if self.tot["n_ops"] >= MAXOPS:
            return
        W, R, G = self.W, self.R, self.G
        deps = []
        for k in reads:
            deps.extend(W.get(k, ()))
            if k.startswith("pb"):
                deps.extend(r for r in R.get(k, ()) if r[2] != engn)
        newgen = []
        for k in writes:
            if partial and k in W and not R.get(k):
                deps.extend(G[k])
            else:
                d = list(W.get(k, ())) + list(R.get(k, ()))
                deps.extend(d)
                newgen.append((k, d))
        eng = self.engs[engn]
        w = self.waited[engn]
        need = {}
        if engn in self.need_bar:
            self.need_bar.discard(engn)
            for num, (s, v) in self.prev_final.items():
                need[num] = (s, v)
        for (s, v, de, ddma) in deps:
            if engn == "pe" and not dma and de == "pe" and not ddma:
                continue
            if need.get(s.num, (None, 0))[1] < v:
                need[s.num] = (s, v)
        for num, (s, v) in need.items():
            if w.get(num, 0) < v:
                eng.wait_ge(s, v)
                w[num] = v
                self.tot["n_wait"] += 1
        ins = fn(eng)
        if dma:
            g = group
            if g not in self.grp_sem or self.grp_cnt[g] + 16 > SEM_LIMIT:
                self.grp_sem[g] = self.new_sem()
                self.grp_cnt[g] = 0
            self.grp_cnt[g] += 16
            sem, val = self.grp_sem[g], self.grp_cnt[g]
            ins.then_inc(sem, 16)
        else:
            e = engn
            if e not in self.eng_sem or self.eng_cnt[e] + 1 > SEM_LIMIT:
                self.eng_sem[e] = self.new_sem()
                self.eng_cnt[e] = 0
            self.eng_cnt[e] += 1
            sem, val = self.eng_sem[e], self.eng_cnt[e]
            ins.then_inc(sem, 1)
        self.cur_final[sem.num] = (sem, val)
        rec = (sem, val, engn, dma)
        for k, d in newgen:
            G[k] = d
            W[k] = []
            R[k] = []
        for k in writes:
            W[k].append(rec)
        for k in reads:
            R.setdefault(k, []).append(rec)
        self.tot["n_ops"] += 1

    def op(self, eng, fn, reads=(), writes=(), partial=False):
        self._do(eng, fn, reads, writes, partial, False, None)

    def dma(self, eng, fn, group, reads=(), writes=(), partial=False):
        self._do(eng, fn, reads, writes, partial, True, group)

    def finish(self):
        sp = self.engs["sp"]
        w = self.waited["sp"]
        for num, (s, v) in self.cur_final.items():
            if w.get(num, 0) < v:
                sp.wait_ge(s, v)
                w[num] = v
        self.tot["n_sem"] = self._nsem
        return self.tot


import math
import numpy as np

D = 1024
S = 2048
T = 16
DEPTH = 4
ALPHA = (2 * DEPTH) ** 0.25
NEG = -30000.0
NPAT = 21


def nbr_patterns():
    pats = {}
    p = 5
    out = []
    for qt in range(16):
        if 2 <= qt <= 13:
            out.append([(qt - 2 + i, i) for i in range(5)])
        else:
            kts = [0, 1, 2, 3] if qt < 2 else [12, 13, 14, 15]
            lst = []
            for kt in kts:
                lst.append((kt, p))
                p += 1
            out.append(lst)
    assert p == NPAT
    return out


def nbr_index_table():
    pl = nbr_patterns()
    idx = np.full((128, NPAT, 128), 15 * 31, np.int64)
    done = set()
    for qt in range(16):
        for kt, p in pl[qt]:
            if p in done:
                continue
            done.add(p)
            for q in range(128):
                r, c = qt * 2 + q // 64, q % 64
                r0 = min(max(r - 4, 0), 24)
                c0 = min(max(c - 8, 0), 48)
                for k in range(128):
                    rk, ck = kt * 2 + k // 64, k % 64
                    if r0 <= rk < r0 + 8 and c0 <= ck < c0 + 16:
                        idx[k, p, q] = (rk - r + 7) * 31 + (ck - c + 15)
    return idx


def rope_host():
    def tab(rot):
        inv = (np.float32(500000.0) ** (-np.arange(0, rot, 2, dtype=np.float32) / np.float32(rot))).astype(np.float32)
        ang = (np.arange(S, dtype=np.float32)[:, None] * inv[None, :]).astype(np.float32)
        cos, sin = np.cos(ang).astype(np.float32), np.sin(ang).astype(np.float32)
        t = np.concatenate([cos, cos, -sin, sin], axis=1)
        return np.ascontiguousarray(t.reshape(T, 128, 2 * rot).transpose(1, 0, 2))
    return tab(16), tab(32)


def multi(fns):
    def f(e):
        ins = None
        for g in fns:
            ins = g(e)
        return ins
    return f


class MK:
    def __init__(self, nseq=4, C=768, layers=(0, 1, 2, 3), debug=False, phases="ABCOEM"):
        self.nseq, self.C, self.layers, self.debug = nseq, C, tuple(layers), debug
        self.phases = phases
        self.NT = nseq * T
        self.NTOK = nseq * S
        self.NSLOT = 32 * C
        nc = self.nc = bass.Bass("TRN2", target_bir_lowering=False)
        P = self.P = Prog(nc)
        NTOK = self.NTOK

        def din(name, shape, dt=F32):
            return nc.dram_tensor(name, list(shape), dt, kind="ExternalInput").ap()

        def dint(name, shape, dt=F32):
            return nc.dram_tensor(name, list(shape), dt, kind="Internal").ap()
        self.x = din("x", [NTOK, D])
        self.w = {}
        for nm, shp in [("even_w_in", [2, D, 1952]), ("even_lam", [2, 4, 64]), ("even_subln_g", [2, 128]),
                        ("even_q_norm_g", [2, 256]), ("even_w_uq", [2, 256, 768]), ("even_kv_norm_g", [2, 128]),
                        ("even_w_ukv", [2, 128, 1024]), ("even_w_o", [2, D, D]), ("odd_w_qkv", [2, D, 3072]),
                        ("nbias", [2, 16, 128, NPAT, 128]), ("odd_w_o", [2, D, D]),
                        ("ln1_g", [4, D]), ("ln1_b", [4, D]), ("ln2_g", [4, D]), ("ln2_b", [4, D]),
                        ("w_router", [4, D, 36]), ("b_router", [4, 36]),
                        ("w_gate", [4, 32, D, 512]), ("w_up", [4, 32, D, 512]), ("w_down", [4, 32, 512, D]),
                        ("ropeA", [128, 16, 32]), ("ropeB", [128, 16, 64])]:
            if nm in ("w_gate", "w_up", "w_down") and "E" not in self.phases:
                continue
            if nm in ("odd_w_qkv", "nbias") and ("C" not in self.phases or all(l % 2 == 0 for l in self.layers)):
                continue
            self.w[nm] = din(nm, shp)
        self.y = nc.dram_tensor("y", [NTOK, D], F32, kind="ExternalOutput").ap()
        self.res1 = dint("res1", [NTOK, D]) if not debug else nc.dram_tensor("res1", [NTOK, D], F32, kind="ExternalOutput").ap()
        self.resA = dint("resA", [NTOK, D])
        self.ao = dint("ao", [NTOK, D], BF16) if not debug else nc.dram_tensor("ao", [NTOK, D], BF16, kind="ExternalOutput").ap()
        self.xs = dint("xs", [self.NSLOT, D], BF16)
        self.ys = dint("ys", [self.NSLOT + 128, D])
        self.PS = P.psum("PS", [128, 4096], F32)
        self.bc_reg = nc.gpsimd.alloc_register("bcr")
        nc.gpsimd.reg_mov(self.bc_reg, self.NSLOT - 1)
        self.consts()
        for l in self.layers:
            src = self.x if l == self.layers[0] else self.resA
            if l % 2 == 0:
                self.even_attn(l, src)
            elif "C" in self.phases:
                self.odd_attn(l, src)
            if "O" in self.phases:
                self.oproj(l, src)
            if "E" in self.phases:
                self.experts(l)
            dst = self.y if l == self.layers[-1] else self.resA
            if "M" in self.phases:
                self.combine(l, dst)
        self.stats = P.finish()

    def bank(self, b, n=1):
        return self.PS[:, b * 512:(b + n) * 512]

    def bankbf(self, b):
        return self.PS[:, b * 512:(b + 1) * 512].bitcast(BF16)

    def consts(self):
        P = self.P
        P.begin()
        self.ident = P.sbuf("ident", [128, 128], BF16, persist=True)
        self.identf = P.sbuf("identf", [128, 128], F32, persist=True)
        self.U = P.sbuf("U", [128, 128], BF16, persist=True)
        self.ones = P.sbuf("ones", [128, 128], BF16, persist=True)
        self.iota32 = P.sbuf("iota32", [128, 32], F32, persist=True)
        self.eC = P.sbuf("eC", [128, 32], F32, persist=True)
        self.ropeA = P.sbuf("ropeA", [128, 16, 32], F32, persist=True)
        self.ropeB = P.sbuf("ropeB", [128, 16, 64], F32, persist=True)
        self.gidx = P.sbuf("gidx", [128, self.NT, 2], I32, persist=True)
        self.gates = P.sbuf("gates", [128, self.NT, 2], F32, persist=True)
        self.zero = P.sbuf("zero", [128, D], F32, persist=True)
        iot = P.sbuf("iot", [128, 128], F32)
        iop = P.sbuf("iop", [128, 1], F32)
        P.op("pool", lambda e: e.iota(iot[:], [[1, 128]], base=0, channel_multiplier=0, allow_small_or_imprecise_dtypes=True), writes=["iot"])
        P.op("pool", lambda e: e.iota(iop[:], [[0, 1]], base=0, channel_multiplier=1, allow_small_or_imprecise_dtypes=True), writes=["iop"])
        P.op("dve", lambda e: e.tensor_scalar(self.ident[:], iot[:], iop[:, 0:1], None, ALU.is_equal), reads=["iot", "iop"], writes=["ident"])
        P.op("dve", lambda e: e.tensor_scalar(self.identf[:], iot[:], iop[:, 0:1], None, ALU.is_equal), reads=["iot", "iop"], writes=["identf"])
        P.op("dve", lambda e: e.tensor_scalar(self.U[:], iot[:], iop[:, 0:1], None, ALU.is_gt), reads=["iot", "iop"], writes=["U"])
        P.op("dve", lambda e: e.memset(self.ones[:], 1.0), writes=["ones"])
        P.op("dve", lambda e: e.tensor_copy(self.iota32[:], iot[:, 0:32]), reads=["iot"], writes=["iota32"])
        P.op("dve", lambda e: e.tensor_scalar(self.eC[:], iot[:, 0:32], float(self.C), None, ALU.mult), reads=["iot"], writes=["eC"])
        P.op("dve", lambda e: e.memset(self.zero[:], 0.0), writes=["zero"])
        P.dma("sp", lambda e: e.dma_start(out=self.ropeA[:], in_=self.w["ropeA"]), "c0", writes=["ropeA"])
        P.dma("sp", lambda e: e.dma_start(out=self.ropeB[:], in_=self.w["ropeB"]), "c0", writes=["ropeB"])
        P.dma("sp", lambda e: e.dma_start(out=self.ys[self.NSLOT:self.NSLOT + 128, :], in_=self.zero[:]), "c0", reads=["zero"], writes=["ysd"])
        zb = P.sbuf("zb", [128, 8, D], BF16)
        P.op("pool", lambda e: e.memset(zb[:], 0.0), writes=["zb"])
        for i in range(self.NSLOT // 1024):
            P.dma("sp", lambda e, i=i: e.dma_start(out=self.xs[i * 1024:(i + 1) * 1024, :].rearrange("(p n) d -> p n d", n=8), in_=zb[:]), "c0", reads=["zb"], writes=["xsz"], partial=True)
        P.end()

    def rstd(self, out, in_, scale, eps, rk, wk):
        P = self.P
        P.op("act", lambda e: e.activation(out, in_, AF.Ln, bias=self.epst(eps), scale=scale), reads=rk, writes=wk)
        P.op("act", lambda e: e.activation(out, out, AF.Exp, scale=-0.5), reads=wk, writes=wk)

    def epst(self, eps):
        return self._eps[eps][:, 0:1]

    def mk_eps(self):
        P = self.P
        self._eps = {}
        for eps in (1e-5, 1e-6):
            t = P.sbuf("eps", [128, 1], F32)
            self._eps[eps] = t
            P.op("dve", lambda e, t=t, eps=eps: e.memset(t[:], eps), writes=["eps%g" % eps])

    def bcast_load(self, name, src_row, n, key, dt=F32):
        P = self.P
        t = P.sbuf(name, [128, n], dt)
        P.dma("sp", lambda e: e.dma_start(out=t[:], in_=src_row.unsqueeze(0).broadcast_to([128, n])), "bc_" + key, writes=[key])
        return t

    def load_w_bf16(self, dst, src2d, kchunks, ncols, key, c0=0):
        P = self.P
        for k in range(kchunks):
            P.dma("pool", lambda e, k=k: e.dma_start(out=dst[:, k, c0:c0 + ncols], in_=src2d[k * 128:(k + 1) * 128, :]),
                  "w_" + key, writes=[key], partial=True)

    def x_front(self, src, tt, b, bufs):
        P = self.P
        xt, xb, xT, pbank = bufs
        P.dma("sp", lambda e: e.dma_start(out=xt[b][:], in_=src[tt * 128:(tt + 1) * 128, :]), "xt%d" % b, writes=["xt%d" % b])
        P.op("act", lambda e: e.activation(xb[b][:], xt[b][:], AF.Copy), reads=["xt%d" % b], writes=["xb%d" % b])
        pT = self.bankbf(pbank).rearrange("p (c t) -> p c t", t=128)
        P.op("pe", multi([lambda e, c=c: e.transpose(pT[:, c, :], xb[b][:, c * 128:(c + 1) * 128], self.ident[:]) for c in range(8)]),
             reads=["xb%d" % b, "ident"], writes=["pb%d" % pbank])
        P.op("dve", lambda e: e.tensor_copy(xT[b][:], pT), reads=["pb%d" % pbank], writes=["xT%d" % b])

    def rope(self, src3, dst3, tab, r0, half, ng, rk, wk, tmpk):
        P = self.P
        t1, t2 = self._ropet
        h2 = 2 * half
        a = t1[:, 0:ng, 0:h2]
        b_ = t2[:, 0:ng, 0:h2]
        cc = tab[:, 0:h2].unsqueeze(1).to_broadcast([128, ng, h2])
        ns = tab[:, h2:h2 + half].unsqueeze(1).to_broadcast([128, ng, half])
        ps = tab[:, h2 + half:h2 + 2 * half].unsqueeze(1).to_broadcast([128, ng, half])
        P.op("dve", lambda e: e.tensor_tensor(a, src3[:, :, r0:r0 + h2], cc, ALU.mult), reads=rk, writes=[tmpk + "1"])
        P.op("dve", lambda e: e.tensor_tensor(b_[:, :, 0:half], src3[:, :, r0 + half:r0 + h2], ns, ALU.mult), reads=rk, writes=[tmpk + "2"])
        P.op("dve", lambda e: e.tensor_tensor(b_[:, :, half:h2], src3[:, :, r0:r0 + half], ps, ALU.mult), reads=rk, writes=[tmpk + "2"], partial=True)
        P.op("pool", lambda e: e.tensor_tensor(dst3[:, :, r0:r0 + h2], a, b_, ALU.add), reads=[tmpk + "1", tmpk + "2"], writes=wk)

    def layer_norm(self, r, out, G, B, rk, wk):
        P = self.P
        st, mv, rs = self._lnt
        P.op("dve", lambda e: e.bn_stats(st[:, 0, :], r[:, 0:512]), reads=rk, writes=["lnst"])
        P.op("dve", lambda e: e.bn_stats(st[:, 1, :], r[:, 512:1024]), reads=rk, writes=["lnst"], partial=True)
        P.op("dve", lambda e: e.bn_aggr(mv[:], st[:]), reads=["lnst"], writes=["lnmv"])
        self.rstd(rs[:], mv[:, 1:2], 1.0, 1e-5, ["lnmv", "eps1e-05"], ["lnrs"])
        P.op("dve", lambda e: e.tensor_scalar(out, r, mv[:, 0:1], rs[:, 0:1], ALU.subtract, ALU.mult), reads=rk + ["lnmv", "lnrs"], writes=wk)
        P.op("pool", lambda e: e.tensor_tensor(out, out, G[:], ALU.mult), reads=wk + ["lnG"], writes=wk)
        P.op("pool", lambda e: e.tensor_tensor(out, out, B[:], ALU.add), reads=wk + ["lnB"], writes=wk)

    def mk_lnt(self):
        P = self.P
        self._lnt = (P.sbuf("lnst", [128, 2, 6], F32), P.sbuf("lnmv", [128, 2], F32), P.sbuf("lnrs", [128, 1], F32))

    def dense_attn(self, maps, QT, KT, V, dk, dv, scale, epilogue):
        P = self.P
        E = self._E
        steps = [(qc, mi, kt) for qc in range(4) for mi in range(len(maps)) for kt in range(16)]
        ps_all = self.PS[:, 2 * 512:6 * 512].rearrange("p (b c) -> p b c", c=512)

        def qk(i):
            qc, mi, kt = steps[i]
            blk, r0, vi = maps[mi]
            sb = 6 + (i % 2)
            P.op("pe", lambda e: e.matmul(self.bank(sb), KT[r0:r0 + dk, blk, kt * 128:(kt + 1) * 128],
                                          QT[r0:r0 + dk, blk, qc * 512:(qc + 1) * 512], start=True, stop=True),
                 reads=["QT", "KT"], writes=["pb%d" % sb])
            eb = i % 3
            P.op("act", lambda e: e.activation(E[eb][:], self.bank(sb), AF.Exp, scale=scale), reads=["pb%d" % sb], writes=["E%d" % eb])

        def pv(i):
            qc, mi, kt = steps[i]
            blk, r0, vi = maps[mi]
            eb = i % 3
            P.op("pe", multi([lambda e, qi=qi: e.matmul(self.bank(2 + qi)[:, 0:dv + 1], E[eb][:, qi * 128:(qi + 1) * 128],
                                                        V[:, kt, vi, :], start=(kt == 0), stop=(kt == 15)) for qi in range(4)]),
                 reads=["E%d" % eb, "V"], writes=["pb2", "pb3", "pb4", "pb5"], partial=(kt != 0))
            if kt == 15:
                epilogue(qc, mi, ps_all)
        n = len(steps)
        for i in range(n + 1):
            if i < n:
                qk(i)
            if i >= 1:
                pv(i - 1)

    def even_attn(self, l, src):
        P = self.P
        j = l // 2
        W = self.w
        lam_init = 0.8 - 0.6 * math.exp(-0.3 * l)
        if "A" not in self.phases:
            return self.even_attn_B(l, src)
        P.begin()
        self.mk_eps()
        wt = P.sbuf("wA", [128, 8, 1536], BF16)
        self.load_w_bf16(wt, W["even_w_in"][j][:, 0:1536], 8, 1536, "wA")
        xt = [P.sbuf("xt", [128, D], F32) for _ in range(2)]
        xb = [P.sbuf("xb", [128, D], BF16) for _ in range(2)]
        xT = [P.sbuf("xT", [128, 8, 128], BF16) for _ in range(2)]
        QT = P.sbuf("QT", [128, 4, S], BF16)
        KT = P.sbuf("KT", [128, 4, S], BF16)
        V = P.sbuf("V", [128, 16, 4, 129], BF16)
        qkb = P.sbuf("qkb", [128, 1024], BF16)
        self._ropet = (P.sbuf("rt1", [128, 16, 32], F32), P.sbuf("rt2", [128, 16, 32], F32))
        self._E = [P.sbuf("E", [128, 512], BF16) for _ in range(3)]
        Om = [P.sbuf("Om", [128, 4, 128], F32) for _ in range(2)]
        rec = P.sbuf("rec", [128, 4], F32)
        dd = P.sbuf("dd", [128, 4, 128], F32)
        sq = P.sbuf("sq", [128, 4, 128], F32)
        ss = P.sbuf("ss", [128, 4], F32)
        AO = [P.sbuf("AO", [128, 4, 512], BF16) for _ in range(2)]
        lamv = P.sbuf("lamv", [128, 4, 64], F32)
        P.dma("sp", lambda e: e.dma_start(out=lamv[:], in_=W["even_lam"][j].unsqueeze(0).broadcast_to([128, 4, 64])), "bc_lamv", writes=["lamv"])
        lp = P.sbuf("lp", [128, 2, 64], F32)
        ls = P.sbuf("ls", [128, 2], F32)
        nlam = P.sbuf("nlam", [128, 1], F32)
        P.op("dve", lambda e: e.tensor_tensor(lp[:], lamv[:, 0:4:2, :], lamv[:, 1:4:2, :], ALU.mult), reads=["lamv"], writes=["lp"])
        P.op("dve", lambda e: e.tensor_reduce(ls[:], lp[:], AX.X, ALU.add), reads=["lp"], writes=["ls"])
        P.op("act", lambda e: e.activation(ls[:], ls[:], AF.Exp), reads=["ls"], writes=["ls"])
        P.op("dve", lambda e: e.tensor_tensor(nlam[:], ls[:, 1:2], ls[:, 0:1], ALU.subtract), reads=["ls"], writes=["nlam"])
        P.op("dve", lambda e: e.tensor_scalar(nlam[:], nlam[:], -lam_init, None, ALU.add), reads=["nlam"], writes=["nlam"])
        gsub = self.bcast_load("gsub", W["even_subln_g"][j], 128, "gsub")
        P.op("dve", lambda e: e.tensor_scalar(gsub[:], gsub[:], 1.0 - lam_init, None, ALU.mult), reads=["gsub"], writes=["gsub"])
        P.op("dve", lambda e: e.memset(V[:, :, :, 128:129], 1.0), writes=["Vones"])
        ph = self.bank(0, 3)
        for s in range(self.nseq):
            for t in range(T):
                b = t % 2
                tt = s * T + t
                self.x_front(src, tt, b, (xt, xb, xT, 3))
                for n in range(3):
                    P.op("pe", multi([lambda e, n=n, c=c: e.matmul(self.bank(n), xT[b][:, c, :], wt[:, c, n * 512:(n + 1) * 512], start=(c == 0), stop=(c == 7)) for c in range(8)]),
                         reads=["xT%d" % b, "wA"], writes=["pb%d" % n])
                P.op("act", lambda e: e.activation(qkb[:], ph[:, 0:1024], AF.Copy), reads=["pb0", "pb1"], writes=["qkb"])
                src3 = ph[:, 0:1024].rearrange("p (g d) -> p g d", d=64)
                dst3 = qkb[:].rearrange("p (g d) -> p g d", d=64)
                self.rope(src3, dst3, self.ropeA[:, t, :], 0, 8, 16, ["pb0", "pb1", "ropeA"], ["qkb"], "rt")
                pT2 = self.bankbf(4).rearrange("p (c t) -> p c t", t=128)
                P.op("pe", multi([lambda e, c=c: e.transpose(pT2[:, c, :], qkb[:, c * 128:(c + 1) * 128], self.ident[:]) for c in range(8)]),
                     reads=["qkb", "ident"], writes=["pb4"])
                P.op("dve", lambda e, t=t: e.tensor_copy(QT[:, :, t * 128:(t + 1) * 128], pT2[:, 0:4, :]), reads=["pb4"], writes=["QT"], partial=True)
                P.op("act", lambda e, t=t: e.activation(KT[:, :, t * 128:(t + 1) * 128], pT2[:, 4:8, :], AF.Copy), reads=["pb4"], writes=["KT"], partial=True)
                P.op("act", lambda e, t=t: e.activation(V[:, t, :, 0:128], self.bank(2).rearrange("p (h d) -> p h d", d=128), AF.Copy),
                     reads=["pb2", "Vones"], writes=["V"], partial=True)

            def epi(qc, mi, ps_all, s=s):
                h, m = mi // 2, mi % 2
                ab = (qc % 2)
                P.op("dve", lambda e: e.reciprocal(rec[:], ps_all[:, :, 128]), reads=["pb2", "pb3", "pb4", "pb5"], writes=["rec"])
                P.op("dve", lambda e: e.tensor_tensor(Om[m][:], ps_all[:, :, 0:128], rec[:].unsqueeze(2).to_broadcast([128, 4, 128]), ALU.mult),
                     reads=["pb2", "pb3", "pb4", "pb5", "rec"], writes=["Om%d" % m])
                if m == 1:
                    P.op("dve", lambda e: e.scalar_tensor_tensor(dd[:], Om[1][:], nlam[:, 0:1], Om[0][:], ALU.mult, ALU.add), reads=["Om0", "Om1", "nlam"], writes=["dd"])
                    P.op("pool", lambda e: e.tensor_tensor(sq[:], dd[:], dd[:], ALU.mult), reads=["dd"], writes=["sq"])
                    P.op("dve", lambda e: e.tensor_reduce(ss[:], sq[:], AX.X, ALU.add), reads=["sq"], writes=["ss"])
                    self.rstd(ss[:], ss[:], 1.0 / 128, 1e-6, ["ss", "eps1e-06"], ["ss"])
                    P.op("dve", lambda e: e.tensor_tensor(dd[:], dd[:], ss[:].unsqueeze(2).to_broadcast([128, 4, 128]), ALU.mult), reads=["dd", "ss"], writes=["dd"])
                    P.op("pool", lambda e: e.tensor_tensor(AO[ab][:, :, h * 128:(h + 1) * 128], dd[:], gsub[:].unsqueeze(1).to_broadcast([128, 4, 128]), ALU.mult),
                         reads=["dd", "gsub"], writes=["AO%d" % ab], partial=True)
                    if h == 3:
                        r0 = s * S + qc * 512
                        P.dma("sp", lambda e: e.dma_start(out=self.ao[r0:r0 + 512, 0:512].rearrange("(q p) n -> p q n", p=128), in_=AO[ab][:]),
                              "ao%d" % ab, reads=["AO%d" % ab], writes=["ao_d"], partial=True)
            maps = [(g // 2, (g % 2) * 64, g // 2) for g in range(8)]
            self.dense_attn(maps, QT, KT, V, 64, 128, 0.125, epi)
        P.end()
        self.even_attn_B(l, src)

    def even_attn_B(self, l, src):
        P = self.P
        j = l // 2
        W = self.w
        if "B" not in self.phases:
            return
        P.begin()
        self.mk_eps()
        wt = P.sbuf("wB", [128, 8, 416], BF16)
        self.load_w_bf16(wt, W["even_w_in"][j][:, 1536:1952], 8, 416, "wB")
        wuq = P.sbuf("wuq", [128, 2, 768], BF16)
        self.load_w_bf16(wuq, W["even_w_uq"][j], 2, 768, "wuq")
        wukv = P.sbuf("wukv", [128, 1, 1024], BF16)
        self.load_w_bf16(wukv, W["even_w_ukv"][j], 1, 1024, "wukv")
        qg = self.bcast_load("qg", W["even_q_norm_g"][j], 256, "qg")
        kvg = self.bcast_load("kvg", W["even_kv_norm_g"][j], 128, "kvg")
        xt = [P.sbuf("xt", [128, D], F32) for _ in range(2)]
        xb = [P.sbuf("xb", [128, D], BF16) for _ in range(2)]
        xT = [P.sbuf("xT", [128, 8, 128], BF16) for _ in range(2)]
        QT = P.sbuf("QT", [128, 8, S], BF16)
        KT = P.sbuf("KT", [128, 8, S], BF16)
        V = P.sbuf("V", [128, 16, 8, 65], BF16)
        self._ropet = (P.sbuf("rt1", [128, 8, 32], F32), P.sbuf("rt2", [128, 8, 32], F32))
        self._E = [P.sbuf("E", [128, 512], BF16) for _ in range(3)]
        junk = P.sbuf("junk", [128, 384], F32)
        ssq = P.sbuf("ssq", [128, 2], F32)
        cn = P.sbuf("cn", [128, 384], BF16)
        cT = P.sbuf("cT", [128, 3, 128], BF16)
        qb = P.sbuf("qb", [128, 8, 96], BF16)
        kcat = P.sbuf("kcat", [128, 8, 96], BF16)
        krr = P.sbuf("krr", [128, 1, 32], BF16)
        rec = P.sbuf("rec", [128, 4], F32)
        AO = [P.sbuf("AO", [128, 4, 512], BF16) for _ in range(2)]
        P.op("dve", lambda e: e.memset(V[:, :, :, 64:65], 1.0), writes=["Vones"])
        for s in range(self.nseq):
            for t in range(T):
                b = t % 2
                tt = s * T + t
                self.x_front(src, tt, b, (xt, xb, xT, 5))
                P.op("pe", multi([lambda e, c=c: e.matmul(self.bank(0)[:, 0:416], xT[b][:, c, :], wt[:, c, :], start=(c == 0), stop=(c == 7)) for c in range(8)]),
                     reads=["xT%d" % b, "wB"], writes=["pb0"])
                hB = self.bank(0)
                P.op("act", lambda e: e.activation(junk[:, 0:256], hB[:, 0:256], AF.Square, accum_out=ssq[:, 0:1]), reads=["pb0"], writes=["ssq", "junk"])
                P.op("act", lambda e: e.activation(junk[:, 256:384], hB[:, 256:384], AF.Square, accum_out=ssq[:, 1:2]), reads=["pb0"], writes=["ssq", "junk"], partial=True)
                self.rstd(ssq[:, 0:1], ssq[:, 0:1], 1.0 / 256, 1e-6, ["ssq", "eps1e-06"], ["ssq"])
                self.rstd(ssq[:, 1:2], ssq[:, 1:2], 1.0 / 128, 1e-6, ["ssq", "eps1e-06"], ["ssq"])
                P.op("dve", lambda e: e.scalar_tensor_tensor(cn[:, 0:256], hB[:, 0:256], ssq[:, 0:1], qg[:], ALU.mult, ALU.mult), reads=["pb0", "ssq", "qg"], writes=["cn"])
                P.op("dve", lambda e: e.scalar_tensor_tensor(cn[:, 256:384], hB[:, 256:384], ssq[:, 1:2], kvg[:], ALU.mult, ALU.mult), reads=["pb0", "ssq", "kvg"], writes=["cn"], partial=True)
                P.op("act", lambda e: e.activation(krr[:, 0, :], hB[:, 384:416], AF.Copy), reads=["pb0"], writes=["krr"])
                self.rope(hB[:, 384:416].unsqueeze(1), krr[:], self.ropeB[:, t, :], 0, 16, 1, ["pb0", "ropeB"], ["krr"], "rt")
                P.op("pool", lambda e: e.tensor_copy(kcat[:, :, 64:96], krr[:].to_broadcast([128, 8, 32])), reads=["krr"], writes=["kcat"])
                pTc = self.bankbf(6).rearrange("p (c t) -> p c t", t=128)
                P.op("pe", multi([lambda e, c=c: e.transpose(pTc[:, c, :], cn[:, c * 128:(c + 1) * 128], self.ident[:]) for c in range(3)]), reads=["cn", "ident"], writes=["pb6"])
                P.op("dve", lambda e: e.tensor_copy(cT[:], pTc[:, 0:3, :]), reads=["pb6"], writes=["cT"])
                fl = [lambda e, c=c, c0=c0, n=n: e.matmul(self.PS[:, 512 + c0:512 + c0 + n], cT[:, c, :], wuq[:, c, c0:c0 + n], start=(c == 0), stop=(c == 1))
                      for (c0, n) in ((0, 512), (512, 256)) for c in range(2)]
                fl += [lambda e, n=n: e.matmul(self.bank(3 + n), cT[:, 2, :], wukv[:, 0, n * 512:(n + 1) * 512], start=True, stop=True) for n in range(2)]
                P.op("pe", multi(fl), reads=["cT", "wuq", "wukv"], writes=["pb1", "pb2", "pb3", "pb4"])
                q3 = self.PS[:, 512:1280].rearrange("p (h d) -> p h d", d=96)
                kv3 = self.PS[:, 1536:2560].rearrange("p (h d) -> p h d", d=128)
                P.op("act", lambda e: e.activation(qb[:], q3, AF.Copy), reads=["pb1", "pb2"], writes=["qb"])
                self.rope(q3, qb[:], self.ropeB[:, t, :], 64, 16, 8, ["pb1", "pb2", "ropeB"], ["qb"], "rt")
                P.op("act", lambda e: e.activation(kcat[:, :, 0:64], kv3[:, :, 0:64], AF.Copy), reads=["pb3", "pb4"], writes=["kcat"], partial=True)
                P.op("dve", lambda e, t=t: e.tensor_copy(V[:, t, :, 0:64], kv3[:, :, 64:128]), reads=["pb3", "pb4", "Vones"], writes=["V"], partial=True)
                pTq = self.bankbf(7).rearrange("p (c t) -> p c t", t=128)
                pTk = self.bankbf(6).rearrange("p (c t) -> p c t", t=128)
                P.op("pe", multi([lambda e, h=h: e.transpose(pTq[0:96, h, :], qb[:, h, :], self.ident[:]) for h in range(8)]), reads=["qb", "ident"], writes=["pb7"])
                P.op("dve", lambda e, t=t: e.tensor_copy(QT[0:96, :, t * 128:(t + 1) * 128], pTq[0:96, :, :]), reads=["pb7"], writes=["QT"], partial=True)
                P.op("pe", multi([lambda e, h=h: e.transpose(pTk[0:96, h, :], kcat[:, h, :], self.ident[:]) for h in range(8)]), reads=["kcat", "ident"], writes=["pb6"])
                P.op("act", lambda e, t=t: e.activation(KT[0:96, :, t * 128:(t + 1) * 128], pTk[0:96, :, :], AF.Copy), reads=["pb6"], writes=["KT"], partial=True)

            def epi(qc, mi, ps_all, s=s):
                h = mi
                ab = qc % 2
                P.op("dve", lambda e: e.reciprocal(rec[:], ps_all[:, :, 64]), reads=["pb2", "pb3", "pb4", "pb5"], writes=["rec"])
                P.op("dve", lambda e: e.tensor_tensor(AO[ab][:, :, h * 64:(h + 1) * 64], ps_all[:, :, 0:64], rec[:].unsqueeze(2).to_broadcast([128, 4, 64]), ALU.mult),
                     reads=["pb2", "pb3", "pb4", "pb5", "rec"], writes=["AO%d" % ab], partial=True)
                if h == 7:
                    r0 = s * S + qc * 512
                    P.dma("sp", lambda e: e.dma_start(out=self.ao[r0:r0 + 512, 512:1024].rearrange("(q p) n -> p q n", p=128), in_=AO[ab][:]),
                          "ao%d" % ab, reads=["AO%d" % ab], writes=["ao_d"], partial=True)
            maps = [(h, 0, h) for h in range(8)]
            self.dense_attn(maps, QT, KT, V, 96, 64, 96 ** -0.5, epi)
        P.end()

    def odd_attn(self, l, src):
        P = self.P
        j = l // 2
        W = self.w
        pl = nbr_patterns()
        for hh in range(2):
            P.begin()
            self.mk_eps()
            wt = P.sbuf("wC", [128, 8, 1536], BF16)
            for i in range(3):
                self.load_w_bf16(wt, W["odd_w_qkv"][j][:, i * 1024 + hh * 512:i * 1024 + hh * 512 + 512], 8, 512, "wC", c0=i * 512)
            xt = [P.sbuf("xt", [128, D], F32) for _ in range(2)]
            xb = [P.sbuf("xb", [128, D], BF16) for _ in range(2)]
            xT = [P.sbuf("xT", [128, 8, 128], BF16) for _ in range(2)]
            QT = P.sbuf("QT", [128, 4, S], BF16)
            KT = P.sbuf("KT", [128, 4, S], BF16)
            V = P.sbuf("V", [128, 16, 8, 65], BF16)
            qkb = P.sbuf("qkb", [128, 1024], BF16)
            bias = [P.sbuf("bias", [128, NPAT, 128], F32) for _ in range(2)]
            tmp = [P.sbuf("tmp", [128, 5, 128], F32) for _ in range(2)]
            E = [P.sbuf("E", [128, 5, 128], BF16) for _ in range(2)]
            rec = P.sbuf("rec", [128, 4], F32)
            AO = P.sbuf("AO", [128, 16, 512], BF16)
            P.op("dve", lambda e: e.memset(V[:, :, :, 64:65], 1.0), writes=["Vones"])
            ph = self.bank(0, 3)
            for s in range(self.nseq):
                for t in range(T):
                    b = t % 2
                    tt = s * T + t
                    self.x_front(src, tt, b, (xt, xb, xT, 3))
                    for n in range(3):
                        P.op("pe", multi([lambda e, n=n, c=c: e.matmul(self.bank(n), xT[b][:, c, :], wt[:, c, n * 512:(n + 1) * 512], start=(c == 0), stop=(c == 7)) for c in range(8)]),
                             reads=["xT%d" % b, "wC"], writes=["pb%d" % n])
                    P.op("act", lambda e: e.activation(qkb[:], ph[:, 0:1024], AF.Copy), reads=["pb0", "pb1"], writes=["qkb"])
                    pT2 = self.bankbf(4).rearrange("p (c t) -> p c t", t=128)
                    P.op("pe", multi([lambda e, c=c: e.transpose(pT2[:, c, :], qkb[:, c * 128:(c + 1) * 128], self.ident[:]) for c in range(8)]),
                         reads=["qkb", "ident"], writes=["pb4"])
                    P.op("dve", lambda e, t=t: e.tensor_copy(QT[:, :, t * 128:(t + 1) * 128], pT2[:, 0:4, :]), reads=["pb4"], writes=["QT"], partial=True)
                    P.op("act", lambda e, t=t: e.activation(KT[:, :, t * 128:(t + 1) * 128], pT2[:, 4:8, :], AF.Copy), reads=["pb4"], writes=["KT"], partial=True)
                    P.op("dve", lambda e, t=t: e.tensor_copy(V[:, t, :, 0:64], self.bank(2).rearrange("p (h d) -> p h d", d=64)),
                         reads=["pb2", "Vones"], writes=["V"], partial=True)
                it = 0
                for h in range(8):
                    bb = h % 2
                    hg = hh * 8 + h
                    P.dma("sp", lambda e, hg=hg, bb=bb: e.dma_start(out=bias[bb][:], in_=W["nbias"][j, hg]), "bias%d" % bb, writes=["bias%d" % bb])
                    blk, r0 = h // 2, (h % 2) * 64
                    for qt in range(16):
                        lst = pl[qt]
                        n = len(lst)
                        ib = it % 2
                        it += 1
                        sbk = 4 + 2 * ib
                        ps_s = self.PS[:, sbk * 512:sbk * 512 + 640].rearrange("p (i q) -> p i q", q=128)
                        P.op("pe", multi([lambda e, i=i, kt=kt: e.matmul(ps_s[:, i, :], KT[r0:r0 + 64, blk, kt * 128:(kt + 1) * 128], QT[r0:r0 + 64, blk, qt * 128:(qt + 1) * 128], start=True, stop=True)
                                          for i, (kt, pat) in enumerate(lst)]),
                             reads=["QT", "KT"], writes=["pb%d" % sbk, "pb%d" % (sbk + 1)])
                        p0 = lst[0][1]
                        P.op("dve", lambda e, ib=ib, n=n, p0=p0, ps_s=ps_s, bb=bb: e.scalar_tensor_tensor(tmp[ib][:, 0:n, :], ps_s[:, 0:n, :], 0.125, bias[bb][:, p0:p0 + n, :], ALU.mult, ALU.add),
                             reads=["pb%d" % sbk, "pb%d" % (sbk + 1), "bias%d" % bb], writes=["tmp%d" % ib])
                        P.op("act", lambda e, ib=ib, n=n: e.activation(E[ib][:, 0:n, :], tmp[ib][:, 0:n, :], AF.Exp), reads=["tmp%d" % ib], writes=["E%d" % ib])
                        ob = qt % 4
                        P.op("pe", multi([lambda e, i=i, kt=kt: e.matmul(self.bank(ob)[:, 0:65], E[ib][:, i, :], V[:, kt, h, :], start=(i == 0), stop=(i == n - 1))
                                          for i, (kt, pat) in enumerate(lst)]),
                             reads=["E%d" % ib, "V"], writes=["pb%d" % ob])
                        if ob == 3:
                            ps_all = self.PS[:, 0:4 * 512].rearrange("p (b c) -> p b c", c=512)
                            q0 = qt - 3
                            P.op("dve", lambda e, ps_all=ps_all: e.reciprocal(rec[:], ps_all[:, :, 64]), reads=["pb0", "pb1", "pb2", "pb3"], writes=["rec"])
                            P.op("dve", lambda e, ps_all=ps_all, q0=q0, h=h: e.tensor_tensor(AO[:, q0:q0 + 4, h * 64:(h + 1) * 64], ps_all[:, :, 0:64], rec[:].unsqueeze(2).to_broadcast([128, 4, 64]), ALU.mult),
                                 reads=["pb0", "pb1", "pb2", "pb3", "rec"], writes=["AO"], partial=True)
                r0_ = s * S
                P.dma("sp", lambda e, r0_=r0_: e.dma_start(out=self.ao[r0_:r0_ + S, hh * 512:(hh + 1) * 512].rearrange("(q p) n -> p q n", p=128), in_=AO[:]),
                      "aoC", reads=["AO"], writes=["ao_d"], partial=True)
            P.end()

    def oproj(self, l, src):
        P = self.P
        j = l // 2
        W = self.w
        C = self.C
        P.begin()
        self.mk_eps()
        self.mk_lnt()
        wo = P.sbuf("wo", [128, 8, D], BF16)
        self.load_w_bf16(wo, (W["even_w_o"] if l % 2 == 0 else W["odd_w_o"])[j], 8, D, "wo")
        G = self.bcast_load("G", W["ln1_g"][l], D, "lnG")
        B = self.bcast_load("B", W["ln1_b"][l], D, "lnB")
        wr = P.sbuf("wr", [128, 8, 36], F32)
        P.dma("sp", lambda e: e.dma_start(out=wr[:], in_=W["w_router"][l].rearrange("(c p) n -> p c n", p=128)), "bc_wr", writes=["wr"])
        br = self.bcast_load("br", W["b_router"][l], 36, "br")
        aot = [P.sbuf("aot", [128, D], BF16) for _ in range(2)]
        aoT = [P.sbuf("aoT", [128, 8, 128], BF16) for _ in range(2)]
        xt = [P.sbuf("xt", [128, D], F32) for _ in range(2)]
        r = [P.sbuf("r", [128, D], F32) for _ in range(2)]
        x1 = [P.sbuf("x1", [128, D], F32) for _ in range(2)]
        x1b = [P.sbuf("x1b", [128, D], BF16) for _ in range(2)]
        x1T = [P.sbuf("x1T", [128, 8, 128], F32) for _ in range(2)]
        cnt = P.sbuf("cnt", [128, 32], F32)
        lg = P.sbuf("lg", [128, 36], F32)
        sm = P.sbuf("sm", [128, 16], F32)
        pen = P.sbuf("pen", [128, 4], F32)
        me = P.sbuf("me", [128, 32], F32)
        top8 = P.sbuf("top8", [128, 8], F32)
        idx8 = P.sbuf("idx8", [128, 8], U32)
        ef = P.sbuf("ef", [128, 2], F32)
        oh = P.sbuf("oh", [128, 2, 32], F32)
        Mb = P.sbuf("Mb", [128, 32], BF16)
        pos = P.sbuf("pos", [128, 32], F32)
        prod = P.sbuf("prod", [128, 2, 32], F32)
        slotf = P.sbuf("slotf", [128, 2], F32)
        posf = P.sbuf("posf", [128, 2], F32)
        ov = P.sbuf("ov", [128, 2], F32)
        sidxf = P.sbuf("sidxf", [128, 2], F32)
        gidxf = P.sbuf("gidxf", [128, 2], F32)
        sidx = [P.sbuf("sidx", [128, 2], I32) for _ in range(2)]
        P.op("dve", lambda e: e.memset(cnt[:], 0.0), writes=["cnt"])
        XS = ["xs%d" % e_ for e_ in range(32)]
        for tt in range(self.NT):
            b = tt % 2
            rows = slice(tt * 128, (tt + 1) * 128)
            P.dma("sp", lambda e, rows=rows: e.dma_start(out=aot[b][:], in_=self.ao[rows, :]), "aot%d" % b, writes=["aot%d" % b])
            P.dma("sp", lambda e, rows=rows: e.dma_start(out=xt[b][:], in_=src[rows, :]), "xt%d" % b, writes=["xt%d" % b])
            pT = self.bankbf(2).rearrange("p (c t) -> p c t", t=128)
            P.op("pe", multi([lambda e, c=c: e.transpose(pT[:, c, :], aot[b][:, c * 128:(c + 1) * 128], self.ident[:]) for c in range(8)]), reads=["aot%d" % b, "ident"], writes=["pb2"])
            P.op("dve", lambda e: e.tensor_copy(aoT[b][:], pT), reads=["pb2"], writes=["aoT%d" % b])
            P.op("pe", multi([lambda e, n=n, c=c: e.matmul(self.bank(n), aoT[b][:, c, :], wo[:, c, n * 512:(n + 1) * 512], start=(c == 0), stop=(c == 7)) for n in range(2) for c in range(8)]),
                 reads=["aoT%d" % b, "wo"], writes=["pb0", "pb1"])
            P.op("dve", lambda e: e.scalar_tensor_tensor(r[b][:], xt[b][:], ALPHA, self.bank(0, 2), ALU.mult, ALU.add), reads=["xt%d" % b, "pb0", "pb1"], writes=["r%d" % b])
            self.layer_norm(r[b][:], x1[b][:], G, B, ["r%d" % b], ["x1_%d" % b])
            P.dma("sp", lambda e, rows=rows: e.dma_start(out=self.res1[rows, :], in_=x1[b][:]), "x1o%d" % b, reads=["x1_%d" % b], writes=["res1_d"], partial=True)
            P.op("act", lambda e: e.activation(x1b[b][:], x1[b][:], AF.Copy), reads=["x1_%d" % b], writes=["x1b%d" % b])
            pTf = self.PS[:, 3 * 512:5 * 512].rearrange("p (c t) -> p c t", t=128)
            P.op("pe", multi([lambda e, c=c: e.transpose(pTf[:, c, :], x1[b][:, c * 128:(c + 1) * 128], self.identf[:]) for c in range(8)]), reads=["x1_%d" % b, "identf"], writes=["pb3", "pb4"])
            P.op("act", lambda e: e.activation(x1T[b][:], pTf, AF.Copy), reads=["pb3", "pb4"], writes=["x1T%d" % b])
            pl_ = self.bank(5)[:, 0:36]
            P.op("pe", multi([lambda e, c=c: e.matmul(pl_, x1T[b][:, c, :], wr[:, c, :], start=(c == 0), stop=(c == 7)) for c in range(8)]), reads=["x1T%d" % b, "wr"], writes=["pb5"])
            P.op("dve", lambda e: e.tensor_tensor(lg[:], pl_, br[:], ALU.add), reads=["pb5", "br"], writes=["lg"])
            P.op("dve", lambda e: e.tensor_reduce(sm[:, 0:1], lg[:, 0:4], AX.X, ALU.max), reads=["lg"], writes=["sm0"])
            P.op("dve", lambda e: e.tensor_scalar(pen[:], lg[:, 0:4], sm[:, 0:1], None, ALU.subtract), reads=["lg", "sm0"], writes=["pen"])
            P.op("act", lambda e: e.activation(sm[:, 4:8], pen[:], AF.Exp, accum_out=sm[:, 1:2]), reads=["pen"], writes=["sm1"])
            P.op("dve", lambda e: e.reciprocal(sm[:, 2:3], sm[:, 1:2]), reads=["sm1"], writes=["sm2"])
            P.op("dve", lambda e: e.tensor_scalar(pen[:], pen[:], 0.0, 10000.0, ALU.is_ge, ALU.mult), reads=["pen", "sm1"], writes=["pen"])
            P.op("dve", lambda e: e.tensor_scalar(pen[:], pen[:], -10000.0, None, ALU.add), reads=["pen"], writes=["pen"])
            P.op("dve", lambda e: e.tensor_tensor(me[:].rearrange("p (g k) -> p g k", k=8), lg[:, 4:36].rearrange("p (g k) -> p g k", k=8),
                                                  pen[:].unsqueeze(2).to_broadcast([128, 4, 8]), ALU.add), reads=["lg", "pen"], writes=["me"])
            P.op("dve", lambda e: e.max(top8[:], me[:]), reads=["me"], writes=["top8"])
            P.op("dve", lambda e: e.max_index(idx8[:], top8[:], me[:]), reads=["me", "top8"], writes=["idx8"])
            P.op("dve", lambda e: e.tensor_tensor(sm[:, 8:9], top8[:, 1:2], top8[:, 0:1], ALU.subtract), reads=["top8"], writes=["sm8"])
            P.op("act", lambda e: e.activation(sm[:, 9:10], sm[:, 8:9], AF.Exp), reads=["sm8"], writes=["sm9"])
            P.op("dve", lambda e: e.tensor_scalar(sm[:, 10:11], sm[:, 9:10], 1.0, None, ALU.add), reads=["sm9"], writes=["sm10"])
            P.op("dve", lambda e: e.reciprocal(sm[:, 10:11], sm[:, 10:11]), reads=["sm10"], writes=["sm10"])
            P.op("dve", lambda e: e.tensor_tensor(sm[:, 12:13], sm[:, 10:11], sm[:, 2:3], ALU.mult), reads=["sm10", "sm2"], writes=["sm12"])
            P.op("dve", lambda e: e.tensor_tensor(sm[:, 13:14], sm[:, 12:13], sm[:, 9:10], ALU.mult), reads=["sm12", "sm9"], writes=["sm12"], partial=True)
            P.op("dve", lambda e: e.tensor_copy(ef[:], idx8[:, 0:2]), reads=["idx8"], writes=["ef"])
            for k in range(2):
                P.op("dve", lambda e, k=k: e.tensor_scalar(oh[:, k, :], self.iota32[:], ef[:, k:k + 1], None, ALU.is_equal), reads=["ef", "iota32"], writes=["oh"], partial=True)
            P.op("dve", lambda e: e.tensor_tensor(Mb[:], oh[:, 0, :], oh[:, 1, :], ALU.add), reads=["oh"], writes=["Mb"])
            pr = self.bank(6)[:, 0:32]
            pr2 = self.bank(6)[:, 64:96]
            P.op("pe", multi([lambda e: e.matmul(pr, self.U[:], Mb[:], start=True, stop=True), lambda e: e.matmul(pr2, self.ones[:], Mb[:], start=True, stop=True)]),
                 reads=["U", "ones", "Mb"], writes=["pb6"])
            P.op("dve", lambda e: e.tensor_tensor(pos[:], pr, cnt[:], ALU.add), reads=["pb6", "cnt"], writes=["pos"])
            P.op("dve", lambda e: e.tensor_tensor(cnt[:], pr2, cnt[:], ALU.add), reads=["pb6", "cnt"], writes=["cnt"])
            P.op("dve", lambda e: e.tensor_tensor(prod[:], oh[:], pos[:].unsqueeze(1).to_broadcast([128, 2, 32]), ALU.mult), reads=["oh", "pos"], writes=["prod"])
            P.op("dve", lambda e: e.tensor_reduce(posf[:], prod[:], AX.X, ALU.add), reads=["prod"], writes=["posf"])
            P.op("dve", lambda e: e.tensor_scalar(slotf[:], ef[:], float(C), None, ALU.mult), reads=["ef"], writes=["slotf"])
            P.op("dve", lambda e: e.tensor_tensor(slotf[:], slotf[:], posf[:], ALU.add), reads=["slotf", "posf"], writes=["slotf"])
            P.op("dve", lambda e: e.tensor_scalar(ov[:], posf[:], float(C), None, ALU.is_ge), reads=["posf"], writes=["ov"])
            P.op("dve", lambda e: e.scalar_tensor_tensor(sidxf[:], ov[:], 1.0e6, slotf[:], ALU.mult, ALU.add), reads=["ov", "slotf"], writes=["sidxf"])
            P.op("dve", lambda e: e.tensor_copy(sidx[b][:], sidxf[:]), reads=["sidxf"], writes=["sidx%d" % b])
            P.op("dve", lambda e: e.tensor_scalar(gidxf[:], slotf[:], -1.0, float(self.NSLOT), ALU.mult, ALU.add), reads=["slotf"], writes=["gidxf"])
            P.op("dve", lambda e: e.tensor_tensor(gidxf[:], gidxf[:], ov[:], ALU.mult), reads=["gidxf", "ov"], writes=["gidxf"])
            P.op("dve", lambda e: e.tensor_tensor(gidxf[:], gidxf[:], slotf[:], ALU.add), reads=["gidxf", "slotf"], writes=["gidxf"])
            P.op("dve", lambda e, tt=tt: e.tensor_copy(self.gidx[:, tt, :], gidxf[:]), reads=["gidxf"], writes=["gidx"], partial=True)
            P.op("dve", lambda e: e.tensor_scalar(ov[:], ov[:], -1.0, 1.0, ALU.mult, ALU.add), reads=["ov", "sidxf", "gidxf"], writes=["ov"])
            P.op("dve", lambda e, tt=tt: e.tensor_tensor(self.gates[:, tt, :], sm[:, 12:14], ov[:], ALU.mult), reads=["sm12", "ov"], writes=["gates"], partial=True)
            for k in range(2):
                P.dma("pool", lambda e, k=k: e.indirect_dma_start(out=self.xs[:, :], out_offset=bass.IndirectOffsetOnAxis(ap=sidx[b][:, k:k + 1], axis=0),
                                                                  in_=x1b[b][:], in_offset=None, bounds_check=self.bc_reg, oob_is_err=False),
                      "scat%d" % b, reads=["x1b%d" % b, "sidx%d" % b], writes=XS, partial=True)
        P.end()

    def experts(self, l):
        P = self.P
        W = self.w
        C = self.C
        NS = C // 128
        HW = C // 2
        P.begin()
        wg = [P.sbuf("wg", [128, 8, 512], BF16) for _ in range(2)]
        wu = [P.sbuf("wu", [128, 8, 512], BF16) for _ in range(2)]
        wd = [P.sbuf("wd", [128, 4, D], BF16) for _ in range(2)]
        xst = [P.sbuf("xst", [128, D], BF16) for _ in range(2)]
        xsT = [P.sbuf("xsT", [128, 8, C], BF16) for _ in range(2)]
        hT = [P.sbuf("hT", [128, 4, C], BF16) for _ in range(2)]
        sg = [P.sbuf("sg", [128, HW], F32) for _ in range(2)]
        yt = [P.sbuf("yt", [128, D], F32) for _ in range(2)]
        it = 0
        for ex in range(32):
            eb = ex % 2
            self.load_w_bf16(wg[eb], W["w_gate"][l, ex], 8, 512, "wg%d" % eb)
            self.load_w_bf16(wu[eb], W["w_up"][l, ex], 8, 512, "wu%d" % eb)
            self.load_w_bf16(wd[eb], W["w_down"][l, ex], 4, D, "wd%d" % eb)
            for st in range(NS):
                b = st % 2
                r0 = ex * C + st * 128
                P.dma("sp", lambda e, r0=r0, b=b: e.dma_start(out=xst[b][:], in_=self.xs[r0:r0 + 128, :]), "xst%d" % b, reads=["xs%d" % ex], writes=["xst%d" % b])
                pT = self.bankbf(st % 2).rearrange("p (c t) -> p c t", t=128)
                P.op("pe", multi([lambda e, c=c: e.transpose(pT[:, c, :], xst[b][:, c * 128:(c + 1) * 128], self.ident[:]) for c in range(8)]), reads=["xst%d" % b, "ident"], writes=["pb%d" % (st % 2)])
                if st % 2 == 0:
                    P.op("dve", lambda e, st=st, pT=pT: e.tensor_copy(xsT[eb][:, :, st * 128:(st + 1) * 128], pT), reads=["pb0"], writes=["xsT%d" % eb], partial=True)
                else:
                    P.op("act", lambda e, st=st, pT=pT: e.activation(xsT[eb][:, :, st * 128:(st + 1) * 128], pT, AF.Copy), reads=["pb1"], writes=["xsT%d" % eb], partial=True)
            for f in range(4):
                for hf in range(2):
                    ib = it % 2
                    it += 1
                    pg = self.bank(2 + 2 * ib)[:, 0:HW]
                    pu = self.bank(3 + 2 * ib)[:, 0:HW]
                    P.op("pe", multi([lambda e, c=c: e.matmul(pg, wg[eb][:, c, f * 128:(f + 1) * 128], xsT[eb][:, c, hf * HW:(hf + 1) * HW], start=(c == 0), stop=(c == 7)) for c in range(8)]),
                         reads=["wg%d" % eb, "xsT%d" % eb], writes=["pb%d" % (2 + 2 * ib)])
                    P.op("pe", multi([lambda e, c=c: e.matmul(pu, wu[eb][:, c, f * 128:(f + 1) * 128], xsT[eb][:, c, hf * HW:(hf + 1) * HW], start=(c == 0), stop=(c == 7)) for c in range(8)]),
                         reads=["wu%d" % eb, "xsT%d" % eb], writes=["pb%d" % (3 + 2 * ib)])
                    P.op("act", lambda e, ib=ib, pg=pg: e.activation(sg[ib][:], pg, AF.Silu), reads=["pb%d" % (2 + 2 * ib)], writes=["sg%d" % ib])
                    P.op("dve", lambda e, ib=ib, pu=pu, f=f, hf=hf: e.tensor_tensor(hT[eb][:, f, hf * HW:(hf + 1) * HW], sg[ib][:], pu, ALU.mult),
                         reads=["sg%d" % ib, "pb%d" % (3 + 2 * ib)], writes=["hT%d" % eb], partial=True)
            for st in range(NS):
                b = st % 2
                r0 = ex * C + st * 128
                P.op("pe", multi([lambda e, n=n, f=f: e.matmul(self.bank(6 + n), hT[eb][:, f, st * 128:(st + 1) * 128], wd[eb][:, f, n * 512:(n + 1) * 512], start=(f == 0), stop=(f == 3)) for n in range(2) for f in range(4)]),
                     reads=["hT%d" % eb, "wd%d" % eb], writes=["pb6", "pb7"])
                if st % 2 == 0:
                    P.op("dve", lambda e, b=b: e.tensor_copy(yt[b][:], self.bank(6, 2)), reads=["pb6", "pb7"], writes=["yt%d" % b])
                else:
                    P.op("act", lambda e, b=b: e.activation(yt[b][:], self.bank(6, 2), AF.Copy), reads=["pb6", "pb7"], writes=["yt%d" % b])
                P.dma("sp", lambda e, r0=r0, b=b: e.dma_start(out=self.ys[r0:r0 + 128, :], in_=yt[b][:]), "yo%d" % b, reads=["yt%d" % b], writes=["ys%d" % ex], partial=True)
        P.end()

    def combine(self, l, dst):
        P = self.P
        W = self.w
        P.begin()
        self.mk_eps()
        self.mk_lnt()
        G = self.bcast_load("G", W["ln2_g"][l], D, "lnG")
        B = self.bcast_load("B", W["ln2_b"][l], D, "lnB")
        x1 = [P.sbuf("x1", [128, D], F32) for _ in range(2)]
        y0 = [P.sbuf("y0", [128, D], F32) for _ in range(2)]
        y1 = [P.sbuf("y1", [128, D], F32) for _ in range(2)]
        f = [P.sbuf("f", [128, D], F32) for _ in range(2)]
        o = [P.sbuf("o", [128, D], F32) for _ in range(2)]
        YS = ["ys%d" % e_ for e_ in range(32)] + ["ysd"]
        for tt in range(self.NT):
            b = tt % 2
            rows = slice(tt * 128, (tt + 1) * 128)
            P.dma("sp", lambda e, rows=rows: e.dma_start(out=x1[b][:], in_=self.res1[rows, :]), "x1l%d" % b, reads=["res1_d"], writes=["x1_%d" % b])
            for k, yb in enumerate((y0, y1)):
                P.dma("pool", lambda e, k=k, yb=yb, tt=tt: e.indirect_dma_start(out=yb[b][:], out_offset=None, in_=self.ys[:, :],
                                                                               in_offset=bass.IndirectOffsetOnAxis(ap=self.gidx[:, tt, k:k + 1], axis=0)),
                      "gat%d_%d" % (k, b), reads=YS + ["gidx"], writes=["y%d_%d" % (k, b)])
            P.op("dve", lambda e, tt=tt: e.tensor_scalar(f[b][:], y0[b][:], self.gates[:, tt, 0:1], None, ALU.mult), reads=["y0_%d" % b, "gates"], writes=["f%d" % b])
            P.op("dve", lambda e, tt=tt: e.scalar_tensor_tensor(f[b][:], y1[b][:], self.gates[:, tt, 1:2], f[b][:], ALU.mult, ALU.add), reads=["y1_%d" % b, "gates", "f%d" % b], writes=["f%d" % b])
            P.op("dve", lambda e: e.scalar_tensor_tensor(f[b][:], x1[b][:], ALPHA, f[b][:], ALU.mult, ALU.add), reads=["x1_%d" % b, "f%d" % b], writes=["f%d" % b])
            self.layer_norm(f[b][:], o[b][:], G, B, ["f%d" % b], ["o%d" % b])
            P.dma("sp", lambda e, rows=rows: e.dma_start(out=dst[rows, :], in_=o[b][:]), "oo%d" % b, reads=["o%d" % b], writes=["dst_d"], partial=True)
        P.end()


def prep_shared(inp):
    f32 = np.float32
    sh = {}
    sh["even_w_in"] = np.ascontiguousarray(inp["even_w_in"], f32)
    sh["even_lam"] = np.ascontiguousarray(np.stack([inp["even_lam_q1"], inp["even_lam_k1"], inp["even_lam_q2"], inp["even_lam_k2"]], axis=1), f32)
    for k in ("even_subln_g", "even_q_norm_g", "even_w_uq", "even_kv_norm_g", "even_w_ukv", "even_w_o", "odd_w_qkv", "odd_w_o",
              "ln1_g", "ln1_b", "ln2_g", "ln2_b", "w_gate", "w_up", "w_down"):
        sh[k] = np.ascontiguousarray(inp[k], f32)
    sh["w_router"] = np.ascontiguousarray(np.concatenate([inp["w_router_group"], inp["w_router_expert"]], axis=2), f32)
    sh["b_router"] = np.ascontiguousarray(np.concatenate([inp["b_router_group"], inp["b_router_expert"]], axis=1), f32)
    idx = nbr_index_table()
    rb = np.asarray(inp["odd_rel_bias"], f32).reshape(2, 16, 15 * 31)
    ext = np.concatenate([rb, np.full((2, 16, 1), NEG, f32)], axis=2)
    sh["nbias"] = np.ascontiguousarray(ext[:, :, idx], f32)
    ra, rb_ = rope_host()
    sh["ropeA"], sh["ropeB"] = ra, rb_
    return sh


def kernel(**inputs):
    from concourse.bass_utils import run_bass_kernel_spmd
    n = 8
    nseq = 4
    inp = {k: np.asarray(v) for k, v in inputs.items()}
    sh = prep_shared(inp)
    x = np.ascontiguousarray(inp["x"], np.float32)
    mk = MK(nseq=nseq, C=768, layers=(0, 1, 2, 3), debug=False)
    in_maps = []
    for c in range(n):
        d = {k: v for k, v in sh.items() if k in mk.w}
        d["x"] = np.ascontiguousarray(x[c * nseq:(c + 1) * nseq].reshape(nseq * S, D))
        in_maps.append(d)
    res = run_bass_kernel_spmd(mk.nc, in_maps, core_ids=list(range(n)))
    out = np.concatenate([np.asarray(r["y"], np.float32).reshape(nseq, S, D) for r in res.results], axis=0)
    return out
```

```python
import numpy as np
import concourse.bass as bass
import concourse.mybir as mybir
from contextlib import ExitStack

F32 = mybir.dt.float32
BF16 = mybir.dt.bfloat16
I32 = mybir.dt.int32
U32 = mybir.dt.uint32
AF = mybir.ActivationFunctionType
ALU = mybir.AluOpType
AX = mybir.AxisListType

SEM_LIMIT = 30000
MAXOPS = 10 ** 9


class Prog:
    def __init__(self, nc):
        self.nc = nc
        self.gstack = ExitStack()
        self.pstack = None
        self.engs = {"pe": nc.tensor, "act": nc.scalar, "dve": nc.vector, "pool": nc.gpsimd, "sp": nc.sync}
        self._nsem = 0
        self.eng_sem, self.eng_cnt, self.grp_sem, self.grp_cnt = {}, {}, {}, {}
        self.waited = {e: {} for e in self.engs}
        self.prev_final = {}
        self.cur_final = {}
        self.need_bar = set()
        self.tot = dict(n_ops=0, n_wait=0)
        self._nm = 0
        self.W, self.R, self.G = {}, {}, {}

    def sbuf(self, name, shape, dt, persist=False):
        self._nm += 1
        st = self.gstack if (persist or self.pstack is None) else self.pstack
        return st.enter_context(self.nc.sbuf_tensor("%s_%d" % (name, self._nm), list(shape), dt))

    def psum(self, name, shape, dt):
        return self.gstack.enter_context(self.nc.psum_tensor(name, list(shape), dt))

    def new_sem(self):
        self._nsem += 1
        return self.gstack.enter_context(self.nc.semaphore("s%d" % self._nsem))

    def begin(self):
        self.pstack = ExitStack()
        self.prev_final = dict(self.cur_final)
        self.need_bar = set(self.engs)
        self.W, self.R, self.G = {}, {}, {}

    def end(self):
        self.pstack.close()
        self.pstack = None

    def _do(self, engn, fn, reads, writes, partial, dma, group, wfull=()):
        if self.tot["n_ops"] >= MAXOPS:
            return
        W, R, G = self.W, self.R, self.G
        deps = []
        for k in reads:
            deps.extend(W.get(k, ()))
            if k.startswith("pb"):
                deps.extend(r for r in R.get(k, ()) if r[2] != engn)
        newgen = []
        for k in wfull:
            d = list(W.get(k, ())) + list(R.get(k, ()))
            deps.extend(d)
            newgen.append((k, d))
        for k in writes:
            if partial and k in W and not R.get(k):
                deps.extend(G[k])
            else:
                d = list(W.get(k, ())) + list(R.get(k, ()))
                deps.extend(d)
                newgen.append((k, d))
        eng = self.engs[engn]
        w = self.waited[engn]
        need = {}
        if engn in self.need_bar:
            self.need_bar.discard(engn)
            for num, (s, v) in self.prev_final.items():
                need[num] = (s, v)
        for (s, v, de, ddma) in deps:
            if engn == "pe" and not dma and de == "pe" and not ddma:
                continue
            if need.get(s.num, (None, 0))[1] < v:
                need[s.num] = (s, v)
        for num, (s, v) in need.items():
            if w.get(num, 0) < v:
                eng.wait_ge(s, v)
                w[num] = v
                self.tot["n_wait"] += 1
        ins = fn(eng)
        if dma:
            g = group
            if g not in self.grp_sem or self.grp_cnt[g] + 16 > SEM_LIMIT:
                self.grp_sem[g] = self.new_sem()
                self.grp_cnt[g] = 0
            self.grp_cnt[g] += 16
            sem, val = self.grp_sem[g], self.grp_cnt[g]
            ins.then_inc(sem, 16)
        else:
            e = engn
            if e not in self.eng_sem or self.eng_cnt[e] + 1 > SEM_LIMIT:
                self.eng_sem[e] = self.new_sem()
                self.eng_cnt[e] = 0
            self.eng_cnt[e] += 1
            sem, val = self.eng_sem[e], self.eng_cnt[e]
            ins.then_inc(sem, 1)
        self.cur_final[sem.num] = (sem, val)
        rec = (sem, val, engn, dma)
        for k, d in newgen:
            G[k] = d
            W[k] = []
            R[k] = []
        for k in tuple(writes) + tuple(wfull):
            W[k].append(rec)
        for k in reads:
            R.setdefault(k, []).append(rec)
        self.tot["n_ops"] += 1

    def op(self, eng, fn, reads=(), writes=(), partial=False):
        self._do(eng, fn, reads, writes, partial, False, None)

    def dma(self, eng, fn, group, reads=(), writes=(), partial=False, wfull=()):
        self._do(eng, fn, reads, writes, partial, True, group, wfull)

    def finish(self):
        sp = self.engs["sp"]
        w = self.waited["sp"]
        for num, (s, v) in self.cur_final.items():
            if w.get(num, 0) < v:
                sp.wait_ge(s, v)
                w[num] = v
        self.tot["n_sem"] = self._nsem
        return self.tot


class Rec:
    def __init__(self):
        self.l = []

    def op(self, *a, **k):
        self.l.append(("op", a, k))

    def dma(self, *a, **k):
        self.l.append(("dma", a, k))


def interleave(P, recs):
    from itertools import zip_longest
    for group in zip_longest(*[r.l for r in recs]):
        for item in group:
            if item is not None and item[0] != "mark":
                getattr(P, item[0])(*item[1], **item[2])


import math
import os
DVX = int(os.environ.get("DVX", 0))
import numpy as np

D = 1024
S = 2048
T = 16
DEPTH = 4
ALPHA = (2 * DEPTH) ** 0.25
NEG = -30000.0
NPAT = 21


def nbr_patterns():
    pats = {}
    p = 5
    out = []
    for qt in range(16):
        if 2 <= qt <= 13:
            out.append([(qt - 2 + i, i) for i in range(5)])
        else:
            kts = [0, 1, 2, 3] if qt < 2 else [12, 13, 14, 15]
            lst = []
            for kt in kts:
                lst.append((kt, p))
                p += 1
            out.append(lst)
    assert p == NPAT
    return out


def nbr_index_table():
    pl = nbr_patterns()
    idx = np.full((128, NPAT, 128), 15 * 31, np.int64)
    done = set()
    for qt in range(16):
        for kt, p in pl[qt]:
            if p in done:
                continue
            done.add(p)
            for q in range(128):
                r, c = qt * 2 + q // 64, q % 64
                r0 = min(max(r - 4, 0), 24)
                c0 = min(max(c - 8, 0), 48)
                for k in range(128):
                    rk, ck = kt * 2 + k // 64, k % 64
                    if r0 <= rk < r0 + 8 and c0 <= ck < c0 + 16:
                        idx[k, p, q] = (rk - r + 7) * 31 + (ck - c + 15)
    return idx


def rope_host():
    def tab(rot):
        inv = (np.float32(500000.0) ** (-np.arange(0, rot, 2, dtype=np.float32) / np.float32(rot))).astype(np.float32)
        ang = (np.arange(S, dtype=np.float32)[:, None] * inv[None, :]).astype(np.float32)
        cos, sin = np.cos(ang).astype(np.float32), np.sin(ang).astype(np.float32)
        t = np.concatenate([cos, cos, -sin, sin], axis=1)
        return np.ascontiguousarray(t.reshape(T, 128, 2 * rot).transpose(1, 0, 2))
    return tab(16), tab(32)


def multi(fns):
    def f(e):
        ins = None
        for g in fns:
            ins = g(e)
        return ins
    return f


class MK:
    def __init__(self, nseq=4, C=768, layers=(0, 1, 2, 3), debug=False, phases="ABCOEM"):
        self.nseq, self.C, self.layers, self.debug = nseq, C, tuple(layers), debug
        self.phases = phases
        self.NT = nseq * T
        self.NTOK = nseq * S
        self.NSLOT = 32 * C
        nc = self.nc = bass.Bass("TRN2", target_bir_lowering=False)
        P = self.P = Prog(nc)
        NTOK = self.NTOK

        def din(name, shape, dt=F32):
            return nc.dram_tensor(name, list(shape), dt, kind="ExternalInput").ap()

        def dint(name, shape, dt=F32):
            return nc.dram_tensor(name, list(shape), dt, kind="Internal").ap()
        self.x = din("x", [NTOK, D])
        self.w = {}
        for nm, shp in [("even_w_in", [2, D, 1952]), ("even_lam", [2, 4, 64]), ("even_subln_g", [2, 128]),
                        ("even_q_norm_g", [2, 256]), ("even_w_uq", [2, 256, 768]), ("even_kv_norm_g", [2, 128]),
                        ("even_w_ukv", [2, 128, 1024]), ("even_w_o", [2, D, D]), ("odd_w_qkv", [2, D, 3072]),
                        ("nbias", [2, 16, 128, NPAT, 128]), ("odd_w_o", [2, D, D]),
                        ("ln1_g", [4, D]), ("ln1_b", [4, D]), ("ln2_g", [4, D]), ("ln2_b", [4, D]),
                        ("w_router", [4, D, 36]), ("b_router", [4, 36]),
                        ("w_gate", [4, 32, D, 512]), ("w_up", [4, 32, D, 512]), ("w_down", [4, 32, 512, D]),
                        ("ropeA", [128, 16, 32]), ("ropeB", [128, 16, 64])]:
            if nm in ("w_gate", "w_up", "w_down") and "E" not in self.phases:
                continue
            if nm in ("odd_w_qkv", "nbias") and ("C" not in self.phases or all(l % 2 == 0 for l in self.layers)):
                continue
            self.w[nm] = din(nm, shp)
        self.y = nc.dram_tensor("y", [NTOK, D], F32, kind="ExternalOutput").ap()
        self.res1 = dint("res1", [NTOK, D]) if not debug else nc.dram_tensor("res1", [NTOK, D], F32, kind="ExternalOutput").ap()
        self.resA = dint("resA", [NTOK, D])
        self.ao = dint("ao", [NTOK, D], BF16) if not debug else nc.dram_tensor("ao", [NTOK, D], BF16, kind="ExternalOutput").ap()
        self.xs = dint("xs", [self.NSLOT, D], BF16)
        self.ys = dint("ys", [self.NSLOT + 128, D])
        self.PS = P.psum("PS", [128, 4096], F32)
        self.bc_reg = nc.gpsimd.alloc_register("bcr")
        nc.gpsimd.reg_mov(self.bc_reg, self.NSLOT - 1)
        self.consts()
        for l in self.layers:
            src = self.x if l == self.layers[0] else self.resA
            if l % 2 == 0:
                self.even_attn(l, src)
            elif "C" in self.phases:
                self.odd_attn(l, src)
            if "O" in self.phases:
                self.oproj(l, src)
            if "E" in self.phases:
                self.experts(l)
            dst = self.y if l == self.layers[-1] else self.resA
            if "M" in self.phases:
                self.combine(l, dst)
        self.stats = P.finish()

    def bank(self, b, n=1):
        return self.PS[:, b * 512:(b + n) * 512]

    def bankbf(self, b):
        return self.PS[:, b * 512:(b + 1) * 512].bitcast(BF16)

    def consts(self):
        P = self.P
        P.begin()
        self.ident = P.sbuf("ident", [128, 128], BF16, persist=True)
        self.identf = P.sbuf("identf", [128, 128], F32, persist=True)
        self.U = P.sbuf("U", [128, 128], BF16, persist=True)
        self.ones = P.sbuf("ones", [128, 128], BF16, persist=True)
        self.iota32 = P.sbuf("iota32", [128, 32], F32, persist=True)
        self.eC = P.sbuf("eC", [128, 32], F32, persist=True)
        self.ropeA = P.sbuf("ropeA", [128, 16, 32], F32, persist=True)
        self.ropeB = P.sbuf("ropeB", [128, 16, 64], F32, persist=True)
        self.gidx = P.sbuf("gidx", [128, self.NT, 2], I32, persist=True)
        self.gates = P.sbuf("gates", [128, self.NT, 2], F32, persist=True)
        self.zero = P.sbuf("zero", [128, D], F32, persist=True)
        iot = P.sbuf("iot", [128, 128], F32)
        iop = P.sbuf("iop", [128, 1], F32)
        P.op("pool", lambda e: e.iota(iot[:], [[1, 128]], base=0, channel_multiplier=0, allow_small_or_imprecise_dtypes=True), writes=["iot"])
        P.op("pool", lambda e: e.iota(iop[:], [[0, 1]], base=0, channel_multiplier=1, allow_small_or_imprecise_dtypes=True), writes=["iop"])
        P.op("dve", lambda e: e.tensor_scalar(self.ident[:], iot[:], iop[:, 0:1], None, ALU.is_equal), reads=["iot", "iop"], writes=["ident"])
        P.op("dve", lambda e: e.tensor_scalar(self.identf[:], iot[:], iop[:, 0:1], None, ALU.is_equal), reads=["iot", "iop"], writes=["identf"])
        P.op("dve", lambda e: e.tensor_scalar(self.U[:], iot[:], iop[:, 0:1], None, ALU.is_gt), reads=["iot", "iop"], writes=["U"])
        P.op("dve", lambda e: e.memset(self.ones[:], 1.0), writes=["ones"])
        P.op("dve", lambda e: e.tensor_copy(self.iota32[:], iot[:, 0:32]), reads=["iot"], writes=["iota32"])
        P.op("dve", lambda e: e.tensor_scalar(self.eC[:], iot[:, 0:32], float(self.C), None, ALU.mult), reads=["iot"], writes=["eC"])
        P.op("dve", lambda e: e.memset(self.zero[:], 0.0), writes=["zero"])
        P.dma("sp", lambda e: e.dma_start(out=self.ropeA[:], in_=self.w["ropeA"]), "c0", writes=["ropeA"])
        P.dma("sp", lambda e: e.dma_start(out=self.ropeB[:], in_=self.w["ropeB"]), "c0", writes=["ropeB"])
        P.dma("sp", lambda e: e.dma_start(out=self.ys[self.NSLOT:self.NSLOT + 128, :], in_=self.zero[:]), "c0", reads=["zero"], writes=["ysd"])
        zb = P.sbuf("zb", [128, 8, D], BF16)
        P.op("pool", lambda e: e.memset(zb[:], 0.0), writes=["zb"])
        for i in range(self.NSLOT // 1024):
            P.dma("sp", lambda e, i=i: e.dma_start(out=self.xs[i * 1024:(i + 1) * 1024, :].rearrange("(p n) d -> p n d", n=8), in_=zb[:]), "c0", reads=["zb"], writes=["xsz"], partial=True)
        P.end()

    def rstd(self, out, in_, scale, eps, rk, wk):
        P = self.P
        P.op("act", lambda e: e.activation(out, in_, AF.Ln, bias=self.epst(eps), scale=scale), reads=rk, writes=wk)
        P.op("act", lambda e: e.activation(out, out, AF.Exp, scale=-0.5), reads=wk, writes=wk)

    def epst(self, eps):
        return self._eps[eps][:, 0:1]

    def mk_eps(self):
        P = self.P
        self._eps = {}
        for eps in (1e-5, 1e-6):
            t = P.sbuf("eps", [128, 1], F32)
            self._eps[eps] = t
            P.op("dve", lambda e, t=t, eps=eps: e.memset(t[:], eps), writes=["eps%g" % eps])

    def bcast_load(self, name, src_row, n, key, dt=F32):
        P = self.P
        t = P.sbuf(name, [128, n], dt)
        P.dma("sp", lambda e: e.dma_start(out=t[:], in_=src_row.unsqueeze(0).broadcast_to([128, n])), "bc_" + key, writes=[key])
        return t

    def load_w_bf16(self, dst, src2d, kchunks, ncols, key, c0=0):
        P = self.P
        for k in range(kchunks):
            P.dma("pool", lambda e, k=k: e.dma_start(out=dst[:, k, c0:c0 + ncols], in_=src2d[k * 128:(k + 1) * 128, :]),
                  "w_" + key, writes=[key], partial=True)

    def x_front(self, src, tt, b, bufs):
        P = self.P
        xt, xb, xT, pbank = bufs
        P.dma("sp", lambda e: e.dma_start(out=xt[b][:], in_=src[tt * 128:(tt + 1) * 128, :]), "xt%d" % b, writes=["xt%d" % b])
        P.op("act", lambda e: e.activation(xb[b][:], xt[b][:], AF.Copy), reads=["xt%d" % b], writes=["xb%d" % b])
        pT = self.bankbf(pbank).rearrange("p (c t) -> p c t", t=128)
        P.op("pe", multi([lambda e, c=c: e.transpose(pT[:, c, :], xb[b][:, c * 128:(c + 1) * 128], self.ident[:]) for c in range(8)]),
             reads=["xb%d" % b, "ident"], writes=["pb%d" % pbank])
        P.op("dve", lambda e: e.tensor_copy(xT[b][:], pT), reads=["pb%d" % pbank], writes=["xT%d" % b])

    def rope(self, src3, dst3, tab, r0, half, ng, rk, wk, tmpk, temps=None):
        P = self.P
        t1, t2 = temps if temps is not None else self._ropet
        h2 = 2 * half
        a = t1[:, 0:ng, 0:h2]
        b_ = t2[:, 0:ng, 0:h2]
        cc = tab[:, 0:h2].unsqueeze(1).to_broadcast([128, ng, h2])
        ns = tab[:, h2:h2 + half].unsqueeze(1).to_broadcast([128, ng, half])
        ps = tab[:, h2 + half:h2 + 2 * half].unsqueeze(1).to_broadcast([128, ng, half])
        P.op("dve", lambda e: e.tensor_tensor(a, src3[:, :, r0:r0 + h2], cc, ALU.mult), reads=rk, writes=[tmpk + "1"])
        P.op("dve", lambda e: e.tensor_tensor(b_[:, :, 0:half], src3[:, :, r0 + half:r0 + h2], ns, ALU.mult), reads=rk, writes=[tmpk + "2"])
        P.op("dve", lambda e: e.tensor_tensor(b_[:, :, half:h2], src3[:, :, r0:r0 + half], ps, ALU.mult), reads=rk, writes=[tmpk + "2"], partial=True)
        P.op("pool", lambda e: e.tensor_tensor(dst3[:, :, r0:r0 + h2], a, b_, ALU.add), reads=[tmpk + "1", tmpk + "2"], writes=wk)

    def layer_norm(self, r, out, G, B, rk, wk, sl=0):
        P = self.P
        st, mv, rs = self._lnt[sl]
        kst, kmv, krs = "lnst%d" % sl, "lnmv%d" % sl, "lnrs%d" % sl
        P.op("dve", lambda e: e.bn_stats(st[:, 0, :], r[:, 0:512]), reads=rk, writes=[kst])
        P.op("dve", lambda e: e.bn_stats(st[:, 1, :], r[:, 512:1024]), reads=rk, writes=[kst], partial=True)
        P.op("dve", lambda e: e.bn_aggr(mv[:], st[:]), reads=[kst], writes=[kmv])
        self.rstd(rs[:], mv[:, 1:2], 1.0, 1e-5, [kmv, "eps1e-05"], [krs])
        P.op("dve", lambda e: e.tensor_scalar(out, r, mv[:, 0:1], rs[:, 0:1], ALU.subtract, ALU.mult), reads=rk + [kmv, krs], writes=wk)
        P.op("pool", lambda e: e.tensor_tensor(out, out, G[:], ALU.mult), reads=wk + ["lnG"], writes=wk)
        P.op("pool", lambda e: e.tensor_tensor(out, out, B[:], ALU.add), reads=wk + ["lnB"], writes=wk)

    def mk_lnt(self):
        P = self.P
        self._lnt = [(P.sbuf("lnst", [128, 2, 6], F32), P.sbuf("lnmv", [128, 2], F32), P.sbuf("lnrs", [128, 1], F32)) for _ in range(2)]

    def qkv_tile(self, src, tt, t, sl, wt, wkey, QT, KT, V, vd, rope_tab, B):
        P = self.P
        xt, xb, xT, qkb, rts = B
        base = 4 * sl
        self.x_front(src, tt, sl, (xt, xb, xT, base + 3))
        for n in range(3):
            P.op("pe", multi([lambda e, n=n, c=c: e.matmul(self.bank(base + n), xT[sl][:, c, :], wt[:, c, n * 512:(n + 1) * 512], start=(c == 0), stop=(c == 7)) for c in range(8)]),
                 reads=["xT%d" % sl, wkey], writes=["pb%d" % (base + n)])
        ph = self.bank(base, 3)
        kq = "qkb%d" % sl
        pk = ["pb%d" % base, "pb%d" % (base + 1)]
        P.op("act", lambda e: e.activation(qkb[sl][:], ph[:, 0:1024], AF.Copy), reads=pk, writes=[kq])
        if rope_tab is not None:
            src3 = ph[:, 0:1024].rearrange("p (g d) -> p g d", d=64)
            dst3 = qkb[sl][:].rearrange("p (g d) -> p g d", d=64)
            self.rope(src3, dst3, rope_tab, 0, 8, 16, pk + ["ropeA"], [kq], "rt%d" % sl, rts[sl])
        pT2 = self.bankbf(base + 3).rearrange("p (c t) -> p c t", t=128)
        P.op("pe", multi([lambda e, c=c: e.transpose(pT2[:, c, :], qkb[sl][:, c * 128:(c + 1) * 128], self.ident[:]) for c in range(8)]),
             reads=[kq, "ident"], writes=["pb%d" % (base + 3)])
        P.op("dve", lambda e: e.tensor_copy(QT[:, :, t * 128:(t + 1) * 128], pT2[:, 0:4, :]), reads=["pb%d" % (base + 3)], writes=["QT"], partial=True)
        P.op("act", lambda e: e.activation(KT[:, :, t * 128:(t + 1) * 128], pT2[:, 4:8, :], AF.Copy), reads=["pb%d" % (base + 3)], writes=["KT"], partial=True)
        P.op("act", lambda e: e.activation(V[:, t, :, 0:vd], self.bank(base + 2).rearrange("p (h d) -> p h d", d=vd), AF.Copy),
             reads=["pb%d" % (base + 2), "Vones"], writes=["V"], partial=True)

    def qkv_seq(self, src, s, wt, wkey, QT, KT, V, vd, rope, B):
        P = self.P
        for t in range(0, T, 2):
            recs = []
            for sl in (0, 1):
                r = Rec()
                self.P = r
                self.qkv_tile(src, s * T + t + sl, t + sl, sl, wt, wkey, QT, KT, V, vd, (self.ropeA[:, t + sl, :] if rope else None), B)
                recs.append(r)
            self.P = P
            interleave(P, recs)

    def dense_attn(self, maps, QT, KT, V, dk, dv, scale, epilogue):
        P = self.P
        E = self._E
        steps = [(qc, mi, kp) for qc in range(4) for mi in range(len(maps)) for kp in range(8)]
        ps_all = self.PS[:, 2 * 512:6 * 512].rearrange("p (b c) -> p b c", c=512)
        pairs = [0, 6]

        def qk(i):
            qc, mi, kp = steps[i]
            blk, r0, vi = maps[mi]
            b0 = pairs[i % 2]
            P.op("pe", multi([lambda e, j=j: e.matmul(self.bank(b0 + j), KT[r0:r0 + dk, blk, (2 * kp + j) * 128:(2 * kp + j + 1) * 128],
                                                      QT[r0:r0 + dk, blk, qc * 512:(qc + 1) * 512], start=True, stop=True) for j in range(2)]),
                 reads=["QT", "KT"], writes=["pb%d" % b0, "pb%d" % (b0 + 1)])
            eb = i % 3
            P.op("act", lambda e: e.activation(E[eb][:], self.bank(b0, 2), AF.Exp, scale=scale), reads=["pb%d" % b0, "pb%d" % (b0 + 1)], writes=["E%d" % eb])

        def pv(i):
            qc, mi, kp = steps[i]
            blk, r0, vi = maps[mi]
            eb = i % 3
            P.op("pe", multi([lambda e, qi=qi, j=j: e.matmul(self.bank(2 + qi)[:, 0:dv + 1 - DVX], E[eb][:, j * 512 + qi * 128:j * 512 + (qi + 1) * 128],
                                                             V[:, 2 * kp + j, vi, 0:dv + 1 - DVX], start=(kp == 0 and j == 0), stop=(kp == 7 and j == 1))
                              for j in range(2) for qi in range(4)]),
                 reads=["E%d" % eb, "V"], writes=["pb2", "pb3", "pb4", "pb5"], partial=(kp != 0))
            if kp == 7 and not os.environ.get("NOEPI"):
                epilogue(qc, mi, ps_all)
        n = len(steps)
        if os.environ.get("NOATT"):
            return
        for i in range(n + 1):
            if i < n:
                qk(i)
            if i >= 1:
                pv(i - 1)

    def even_attn(self, l, src):
        P = self.P
        j = l // 2
        W = self.w
        lam_init = 0.8 - 0.6 * math.exp(-0.3 * l)
        if "A" not in self.phases:
            return self.even_attn_B(l, src)
        P.begin()
        self.mk_eps()
        wt = P.sbuf("wA", [128, 8, 1536], BF16)
        self.load_w_bf16(wt, W["even_w_in"][j][:, 0:1536], 8, 1536, "wA")
        xt = [P.sbuf("xt", [128, D], F32) for _ in range(2)]
        xb = [P.sbuf("xb", [128, D], BF16) for _ in range(2)]
        xT = [P.sbuf("xT", [128, 8, 128], BF16) for _ in range(2)]
        QT = P.sbuf("QT", [128, 4, S], BF16)
        KT = P.sbuf("KT", [128, 4, S], BF16)
        V = P.sbuf("V", [128, 16, 4, 129], BF16)
        qkb = [P.sbuf("qkb", [128, 1024], BF16) for _ in range(2)]
        rts = [(P.sbuf("rt1", [128, 16, 32], F32), P.sbuf("rt2", [128, 16, 32], F32)) for _ in range(2)]
        self._E = [P.sbuf("E", [128, 1024], BF16) for _ in range(3)]
        Om = [P.sbuf("Om", [128, 4, 128], F32) for _ in range(2)]
        rec = P.sbuf("rec", [128, 4], F32)
        dd = P.sbuf("dd", [128, 4, 128], F32)
        sq = P.sbuf("sq", [128, 4, 128], F32)
        ss = P.sbuf("ss", [128, 4], F32)
        AO = [P.sbuf("AO", [128, 4, 512], BF16) for _ in range(2)]
        lamv = P.sbuf("lamv", [128, 4, 64], F32)
        P.dma("sp", lambda e: e.dma_start(out=lamv[:], in_=W["even_lam"][j].unsqueeze(0).broadcast_to([128, 4, 64])), "bc_lamv", writes=["lamv"])
        lp = P.sbuf("lp", [128, 2, 64], F32)
        ls = P.sbuf("ls", [128, 2], F32)
        nlam = P.sbuf("nlam", [128, 1], F32)
        P.op("dve", lambda e: e.tensor_tensor(lp[:], lamv[:, 0:4:2, :], lamv[:, 1:4:2, :], ALU.mult), reads=["lamv"], writes=["lp"])
        P.op("dve", lambda e: e.tensor_reduce(ls[:], lp[:], AX.X, ALU.add), reads=["lp"], writes=["ls"])
        P.op("act", lambda e: e.activation(ls[:], ls[:], AF.Exp), reads=["ls"], writes=["ls"])
        P.op("dve", lambda e: e.tensor_tensor(nlam[:], ls[:, 1:2], ls[:, 0:1], ALU.subtract), reads=["ls"], writes=["nlam"])
        P.op("dve", lambda e: e.tensor_scalar(nlam[:], nlam[:], -lam_init, None, ALU.add), reads=["nlam"], writes=["nlam"])
        gsub = self.bcast_load("gsub", W["even_subln_g"][j], 128, "gsub")
        P.op("dve", lambda e: e.tensor_scalar(gsub[:], gsub[:], 1.0 - lam_init, None, ALU.mult), reads=["gsub"], writes=["gsub"])
        P.op("dve", lambda e: e.memset(V[:, :, :, 128:129], 1.0), writes=["Vones"])
        for s in range(self.nseq):
            self.qkv_seq(src, s, wt, "wA", QT, KT, V, 128, True, (xt, xb, xT, qkb, rts))

            def epi(qc, mi, ps_all, s=s):
                h, m = mi // 2, mi % 2
                ab = (qc % 2)
                P.op("dve", lambda e: e.reciprocal(rec[:], ps_all[:, :, 128]), reads=["pb2", "pb3", "pb4", "pb5"], writes=["rec"])
                P.op("dve", lambda e: e.tensor_tensor(Om[m][:], ps_all[:, :, 0:128], rec[:].unsqueeze(2).to_broadcast([128, 4, 128]), ALU.mult),
                     reads=["pb2", "pb3", "pb4", "pb5", "rec"], writes=["Om%d" % m])
                if m == 1:
                    P.op("dve", lambda e: e.scalar_tensor_tensor(dd[:], Om[1][:], nlam[:, 0:1], Om[0][:], ALU.mult, ALU.add), reads=["Om0", "Om1", "nlam"], writes=["dd"])
                    P.op("pool", lambda e: e.tensor_tensor(sq[:], dd[:], dd[:], ALU.mult), reads=["dd"], writes=["sq"])
                    P.op("dve", lambda e: e.tensor_reduce(ss[:], sq[:], AX.X, ALU.add), reads=["sq"], writes=["ss"])
                    self.rstd(ss[:], ss[:], 1.0 / 128, 1e-6, ["ss", "eps1e-06"], ["ss"])
                    P.op("dve", lambda e: e.tensor_tensor(dd[:], dd[:], ss[:].unsqueeze(2).to_broadcast([128, 4, 128]), ALU.mult), reads=["dd", "ss"], writes=["dd"])
                    P.op("pool", lambda e: e.tensor_tensor(AO[ab][:, :, h * 128:(h + 1) * 128], dd[:], gsub[:].unsqueeze(1).to_broadcast([128, 4, 128]), ALU.mult),
                         reads=["dd", "gsub"], writes=["AO%d" % ab], partial=True)
                    if h == 3:
                        r0 = s * S + qc * 512
                        P.dma("pool", lambda e: e.dma_start(out=self.ao[r0:r0 + 512, 0:512].rearrange("(q p) n -> p q n", p=128), in_=AO[ab][:]),
                              "ao%d" % ab, reads=["AO%d" % ab], writes=["ao_d"], partial=True)
            maps = [(g // 2, (0 if os.environ.get("R0") else (g % 2) * 64), g // 2) for g in range(8)]
            self.dense_attn(maps, QT, KT, V, 64, 128, 0.125, epi)
        P.end()
        self.even_attn_B(l, src)

    def even_attn_B(self, l, src):
        P = self.P
        j = l // 2
        W = self.w
        if "B" not in self.phases:
            return
        P.begin()
        self.mk_eps()
        wt = P.sbuf("wB", [128, 8, 416], BF16)
        self.load_w_bf16(wt, W["even_w_in"][j][:, 1536:1952], 8, 416, "wB")
        wuq = P.sbuf("wuq", [128, 2, 768], BF16)
        self.load_w_bf16(wuq, W["even_w_uq"][j], 2, 768, "wuq")
        wukv = P.sbuf("wukv", [128, 1, 1024], BF16)
        self.load_w_bf16(wukv, W["even_w_ukv"][j], 1, 1024, "wukv")
        qg = self.bcast_load("qg", W["even_q_norm_g"][j], 256, "qg")
        kvg = self.bcast_load("kvg", W["even_kv_norm_g"][j], 128, "kvg")
        xt = [P.sbuf("xt", [128, D], F32) for _ in range(2)]
        xb = [P.sbuf("xb", [128, D], BF16) for _ in range(2)]
        xT = [P.sbuf("xT", [128, 8, 128], BF16) for _ in range(2)]
        QT = P.sbuf("QT", [128, 8, S], BF16)
        KT = P.sbuf("KT", [128, 8, S], BF16)
        V = P.sbuf("V", [128, 16, 8, 65], BF16)
        self._ropet = (P.sbuf("rt1", [128, 8, 32], F32), P.sbuf("rt2", [128, 8, 32], F32))
        self._E = [P.sbuf("E", [128, 1024], BF16) for _ in range(3)]
        junk = P.sbuf("junk", [128, 384], F32)
        ssq = P.sbuf("ssq", [128, 2], F32)
        cn = P.sbuf("cn", [128, 384], BF16)
        cT = P.sbuf("cT", [128, 3, 128], BF16)
        qb = P.sbuf("qb", [128, 8, 96], BF16)
        kcat = P.sbuf("kcat", [128, 8, 96], BF16)
        krr = P.sbuf("krr", [128, 1, 32], BF16)
        rec = P.sbuf("rec", [128, 4], F32)
        AO = [P.sbuf("AO", [128, 4, 512], BF16) for _ in range(2)]
        P.op("dve", lambda e: e.memset(V[:, :, :, 64:65], 1.0), writes=["Vones"])
        for s in range(self.nseq):
            for t in range(T):
                b = t % 2
                tt = s * T + t
                self.x_front(src, tt, b, (xt, xb, xT, 5))
                P.op("pe", multi([lambda e, c=c: e.matmul(self.bank(0)[:, 0:416], xT[b][:, c, :], wt[:, c, :], start=(c == 0), stop=(c == 7)) for c in range(8)]),
                     reads=["xT%d" % b, "wB"], writes=["pb0"])
                hB = self.bank(0)
                P.op("act", lambda e: e.activation(junk[:, 0:256], hB[:, 0:256], AF.Square, accum_out=ssq[:, 0:1]), reads=["pb0"], writes=["ssq", "junk"])
                P.op("act", lambda e: e.activation(junk[:, 256:384], hB[:, 256:384], AF.Square, accum_out=ssq[:, 1:2]), reads=["pb0"], writes=["ssq", "junk"], partial=True)
                self.rstd(ssq[:, 0:1], ssq[:, 0:1], 1.0 / 256, 1e-6, ["ssq", "eps1e-06"], ["ssq"])
                self.rstd(ssq[:, 1:2], ssq[:, 1:2], 1.0 / 128, 1e-6, ["ssq", "eps1e-06"], ["ssq"])
                P.op("dve", lambda e: e.scalar_tensor_tensor(cn[:, 0:256], hB[:, 0:256], ssq[:, 0:1], qg[:], ALU.mult, ALU.mult), reads=["pb0", "ssq", "qg"], writes=["cn"])
                P.op("dve", lambda e: e.scalar_tensor_tensor(cn[:, 256:384], hB[:, 256:384], ssq[:, 1:2], kvg[:], ALU.mult, ALU.mult), reads=["pb0", "ssq", "kvg"], writes=["cn"], partial=True)
                P.op("act", lambda e: e.activation(krr[:, 0, :], hB[:, 384:416], AF.Copy), reads=["pb0"], writes=["krr"])
                self.rope(hB[:, 384:416].unsqueeze(1), krr[:], self.ropeB[:, t, :], 0, 16, 1, ["pb0", "ropeB"], ["krr"], "rt")
                P.op("pool", lambda e: e.tensor_copy(kcat[:, :, 64:96], krr[:].to_broadcast([128, 8, 32])), reads=["krr"], writes=["kcat"])
                pTc = self.bankbf(6).rearrange("p (c t) -> p c t", t=128)
                P.op("pe", multi([lambda e, c=c: e.transpose(pTc[:, c, :], cn[:, c * 128:(c + 1) * 128], self.ident[:]) for c in range(3)]), reads=["cn", "ident"], writes=["pb6"])
                P.op("dve", lambda e: e.tensor_copy(cT[:], pTc[:, 0:3, :]), reads=["pb6"], writes=["cT"])
                fl = [lambda e, c=c, c0=c0, n=n: e.matmul(self.PS[:, 512 + c0:512 + c0 + n], cT[:, c, :], wuq[:, c, c0:c0 + n], start=(c == 0), stop=(c == 1))
                      for (c0, n) in ((0, 512), (512, 256)) for c in range(2)]
                fl += [lambda e, n=n: e.matmul(self.bank(3 + n), cT[:, 2, :], wukv[:, 0, n * 512:(n + 1) * 512], start=True, stop=True) for n in range(2)]
                P.op("pe", multi(fl), reads=["cT", "wuq", "wukv"], writes=["pb1", "pb2", "pb3", "pb4"])
                q3 = self.PS[:, 512:1280].rearrange("p (h d) -> p h d", d=96)
                kv3 = self.PS[:, 1536:2560].rearrange("p (h d) -> p h d", d=128)
                P.op("act", lambda e: e.activation(qb[:], q3, AF.Copy), reads=["pb1", "pb2"], writes=["qb"])
                self.rope(q3, qb[:], self.ropeB[:, t, :], 64, 16, 8, ["pb1", "pb2", "ropeB"], ["qb"], "rt")
                P.op("act", lambda e: e.activation(kcat[:, :, 0:64], kv3[:, :, 0:64], AF.Copy), reads=["pb3", "pb4"], writes=["kcat"], partial=True)
                P.op("dve", lambda e, t=t: e.tensor_copy(V[:, t, :, 0:64], kv3[:, :, 64:128]), reads=["pb3", "pb4", "Vones"], writes=["V"], partial=True)
                pTq = self.bankbf(7).rearrange("p (c t) -> p c t", t=128)
                pTk = self.bankbf(6).rearrange("p (c t) -> p c t", t=128)
                P.op("pe", multi([lambda e, h=h: e.transpose(pTq[0:96, h, :], qb[:, h, :], self.ident[:]) for h in range(8)]), reads=["qb", "ident"], writes=["pb7"])
                P.op("dve", lambda e, t=t: e.tensor_copy(QT[0:96, :, t * 128:(t + 1) * 128], pTq[0:96, :, :]), reads=["pb7"], writes=["QT"], partial=True)
                P.op("pe", multi([lambda e, h=h: e.transpose(pTk[0:96, h, :], kcat[:, h, :], self.ident[:]) for h in range(8)]), reads=["kcat", "ident"], writes=["pb6"])
                P.op("act", lambda e, t=t: e.activation(KT[0:96, :, t * 128:(t + 1) * 128], pTk[0:96, :, :], AF.Copy), reads=["pb6"], writes=["KT"], partial=True)

            def epi(qc, mi, ps_all, s=s):
                h = mi
                ab = qc % 2
                P.op("dve", lambda e: e.reciprocal(rec[:], ps_all[:, :, 64]), reads=["pb2", "pb3", "pb4", "pb5"], writes=["rec"])
                P.op("dve", lambda e: e.tensor_tensor(AO[ab][:, :, h * 64:(h + 1) * 64], ps_all[:, :, 0:64], rec[:].unsqueeze(2).to_broadcast([128, 4, 64]), ALU.mult),
                     reads=["pb2", "pb3", "pb4", "pb5", "rec"], writes=["AO%d" % ab], partial=True)
                if h == 7:
                    r0 = s * S + qc * 512
                    P.dma("pool", lambda e: e.dma_start(out=self.ao[r0:r0 + 512, 512:1024].rearrange("(q p) n -> p q n", p=128), in_=AO[ab][:]),
                          "ao%d" % ab, reads=["AO%d" % ab], writes=["ao_d"], partial=True)
            maps = [(h, 0, h) for h in range(8)]
            self.dense_attn(maps, QT, KT, V, 96, 64, 96 ** -0.5, epi)
        P.end()

    def odd_attn(self, l, src):
        P = self.P
        j = l // 2
        W = self.w
        pl = nbr_patterns()
        for hh in range(2):
            P.begin()
            self.mk_eps()
            wt = P.sbuf("wC", [128, 8, 1536], BF16)
            for i in range(3):
                self.load_w_bf16(wt, W["odd_w_qkv"][j][:, i * 1024 + hh * 512:i * 1024 + hh * 512 + 512], 8, 512, "wC", c0=i * 512)
            xt = [P.sbuf("xt", [128, D], F32) for _ in range(2)]
            xb = [P.sbuf("xb", [128, D], BF16) for _ in range(2)]
            xT = [P.sbuf("xT", [128, 8, 128], BF16) for _ in range(2)]
            QT = P.sbuf("QT", [128, 4, S], BF16)
            KT = P.sbuf("KT", [128, 4, S], BF16)
            V = P.sbuf("V", [128, 16, 8, 65], BF16)
            qkb = [P.sbuf("qkb", [128, 1024], BF16) for _ in range(2)]
            bias = [P.sbuf("bias", [128, NPAT, 128], F32) for _ in range(2)]
            tmp = [P.sbuf("tmp", [128, 5, 128], F32) for _ in range(3)]
            E = [P.sbuf("E", [128, 5, 128], BF16) for _ in range(3)]
            rec = P.sbuf("rec", [128, 4], F32)
            AO = P.sbuf("AO", [128, 16, 512], BF16)
            P.op("dve", lambda e: e.memset(V[:, :, :, 64:65], 1.0), writes=["Vones"])
            iters = [(h, qt) for h in range(8) for qt in range(16)]
            for s in range(self.nseq):
                self.qkv_seq(src, s, wt, "wC", QT, KT, V, 64, False, (xt, xb, xT, qkb, None))

                def front(it):
                    h, qt = iters[it]
                    bb = h % 2
                    if qt == 0:
                        hg = hh * 8 + h
                        P.dma("sp", lambda e: e.dma_start(out=bias[bb][:], in_=W["nbias"][j, hg]), "bias%d" % bb, writes=["bias%d" % bb])
                    blk, r0 = h // 2, (h % 2) * 64
                    lst = pl[qt]
                    n = len(lst)
                    rb = it % 3
                    sbk = 2 + 2 * rb
                    ps_s = self.PS[:, sbk * 512:sbk * 512 + 640].rearrange("p (i q) -> p i q", q=128)
                    P.op("pe", multi([lambda e, i=i, kt=kt: e.matmul(ps_s[:, i, :], KT[r0:r0 + 64, blk, kt * 128:(kt + 1) * 128], QT[r0:r0 + 64, blk, qt * 128:(qt + 1) * 128], start=True, stop=True)
                                      for i, (kt, pat) in enumerate(lst)]),
                         reads=["QT", "KT"], writes=["pb%d" % sbk, "pb%d" % (sbk + 1)])
                    p0 = lst[0][1]
                    P.op("dve", lambda e: e.scalar_tensor_tensor(tmp[rb][:, 0:n, :], ps_s[:, 0:n, :], 0.125, bias[bb][:, p0:p0 + n, :], ALU.mult, ALU.add),
                         reads=["pb%d" % sbk, "pb%d" % (sbk + 1), "bias%d" % bb], writes=["tmp%d" % rb])
                    P.op("act", lambda e: e.activation(E[rb][:, 0:n, :], tmp[rb][:, 0:n, :], AF.Exp), reads=["tmp%d" % rb], writes=["E%d" % rb])

                def back(it):
                    h, qt = iters[it]
                    lst = pl[qt]
                    n = len(lst)
                    rb = it % 3
                    ob = (qt // 4) % 2
                    col = (qt % 4) * 128
                    P.op("pe", multi([lambda e, i=i, kt=kt: e.matmul(self.bank(ob)[:, col:col + 65], E[rb][:, i, :], V[:, kt, h, :], start=(i == 0), stop=(i == n - 1))
                                      for i, (kt, pat) in enumerate(lst)]),
                         reads=["E%d" % rb, "V"], writes=["pb%d" % ob], partial=(qt % 4 != 0))
                    if qt % 4 == 3:
                        ps4 = self.bank(ob).rearrange("p (q c) -> p q c", c=128)
                        q0 = qt - 3
                        P.op("dve", lambda e: e.reciprocal(rec[:], ps4[:, :, 64]), reads=["pb%d" % ob], writes=["rec"])
                        P.op("dve", lambda e: e.tensor_tensor(AO[:, q0:q0 + 4, h * 64:(h + 1) * 64], ps4[:, :, 0:64], rec[:].unsqueeze(2).to_broadcast([128, 4, 64]), ALU.mult),
                             reads=["pb%d" % ob, "rec"], writes=["AO"], partial=True)
                nit = len(iters)
                LOOK = 2
                for jx in range(nit + LOOK):
                    if jx < nit:
                        front(jx)
                    if jx >= LOOK:
                        back(jx - LOOK)
                r0_ = s * S
                P.dma("pool", lambda e: e.dma_start(out=self.ao[r0_:r0_ + S, hh * 512:(hh + 1) * 512].rearrange("(q p) n -> p q n", p=128), in_=AO[:]),
                      "aoC", reads=["AO"], writes=["ao_d"])
            P.end()

    def oproj(self, l, src):
        P = self.P
        j = l // 2
        W = self.w
        C = self.C
        P.begin()
        self.mk_eps()
        self.mk_lnt()
        wo = P.sbuf("wo", [128, 8, D], BF16)
        self.load_w_bf16(wo, (W["even_w_o"] if l % 2 == 0 else W["odd_w_o"])[j], 8, D, "wo")
        G = self.bcast_load("G", W["ln1_g"][l], D, "lnG")
        B = self.bcast_load("B", W["ln1_b"][l], D, "lnB")
        wr = P.sbuf("wr", [128, 8, 36], F32)
        P.dma("sp", lambda e: e.dma_start(out=wr[:], in_=W["w_router"][l].rearrange("(c p) n -> p c n", p=128)), "bc_wr", writes=["wr"])
        br = self.bcast_load("br", W["b_router"][l], 36, "br")

        def two(name, shape, dt):
            return [P.sbuf(name, shape, dt) for _ in range(2)]
        T_ = dict(aot=two("aot", [128, D], BF16), aoT=two("aoT", [128, 8, 128], BF16), xt=two("xt", [128, D], F32), r=two("r", [128, D], F32),
                  x1=[P.sbuf("x1", [128, D], F32) for _ in range(4)], x1b=[P.sbuf("x1b", [128, D], BF16) for _ in range(4)], x1T=two("x1T", [128, 8, 128], F32),
                  lg=two("lg", [128, 36], F32), sm=two("sm", [128, 16], F32), pen=two("pen", [128, 4], F32), me=two("me", [128, 32], F32),
                  top8=two("top8", [128, 8], F32), idx8=two("idx8", [128, 8], U32), ef=two("ef", [128, 2], F32), oh=two("oh", [128, 2, 32], F32),
                  Mb=two("Mb", [128, 32], BF16), pos=two("pos", [128, 32], F32), prod=two("prod", [128, 2, 32], F32), slotf=two("slotf", [128, 2], F32),
                  posf=two("posf", [128, 2], F32), ov=two("ov", [128, 2], F32), sidxf=two("sidxf", [128, 2], F32), gidxf=two("gidxf", [128, 2], F32),
                  sidx=two("sidx", [128, 2], I32))
        cnt = P.sbuf("cnt", [128, 32], F32)
        P.op("dve", lambda e: e.memset(cnt[:], 0.0), writes=["cnt"])
        XS = ["xs%d" % e_ for e_ in range(32)]
        prev2 = []
        for tt in range(0, self.NT, 2):
            p1, p2 = [], []
            for sl in (0, 1):
                r_ = Rec()
                self.P = r_
                self.oproj_tile(l, src, tt + sl, sl, T_, wo, G, B, wr, br, cnt, XS)
                k = r_.l.index(("mark",))
                a_, b_ = Rec(), Rec()
                a_.l, b_.l = r_.l[:k], r_.l[k + 1:]
                p1.append(a_)
                p2.append(b_)
            self.P = P
            interleave(P, p1 + prev2)
            prev2 = p2
        interleave(P, prev2)
        P.end()

    def oproj_tile(self, l, src, tt, b, T_, wo, G, B, wr, br, cnt, XS):
        P = self.P
        C = self.C
        b4 = ((tt // 2) % 2) * 2 + b
        aot, aoT, xt, r, x1, x1b, x1T = T_["aot"][b], T_["aoT"][b], T_["xt"][b], T_["r"][b], T_["x1"][b4], T_["x1b"][b4], T_["x1T"][b]
        lg, sm, pen, me, top8, idx8, ef, oh, Mb, pos, prod = (T_[k][b] for k in ("lg", "sm", "pen", "me", "top8", "idx8", "ef", "oh", "Mb", "pos", "prod"))
        slotf, posf, ov, sidxf, gidxf, sidx = (T_[k][b] for k in ("slotf", "posf", "ov", "sidxf", "gidxf", "sidx"))

        def K(n):
            if n in ("x1", "x1b", "x1o"):
                return "%s_%d" % (n, b4)
            return "%s_%d" % (n, b)
        base = 4 * b
        pb = ["pb%d" % (base + i) for i in range(4)]
        rows = slice(tt * 128, (tt + 1) * 128)
        P.dma("sp", lambda e: e.dma_start(out=aot[:], in_=self.ao[rows, :]), K("aot"), writes=[K("aot")])
        P.dma("sp", lambda e: e.dma_start(out=xt[:], in_=src[rows, :]), K("xt"), writes=[K("xt")])
        pT = self.bankbf(base + 0).rearrange("p (c t) -> p c t", t=128)
        P.op("pe", multi([lambda e, c=c: e.transpose(pT[:, c, :], aot[:, c * 128:(c + 1) * 128], self.ident[:]) for c in range(8)]), reads=[K("aot"), "ident"], writes=[pb[0]])
        P.op("dve", lambda e: e.tensor_copy(aoT[:], pT), reads=[pb[0]], writes=[K("aoT")])
        P.op("pe", multi([lambda e, n=n, c=c: e.matmul(self.bank(base + n), aoT[:, c, :], wo[:, c, n * 512:(n + 1) * 512], start=(c == 0), stop=(c == 7)) for n in range(2) for c in range(8)]),
             reads=[K("aoT"), "wo"], writes=[pb[0], pb[1]])
        P.op("dve", lambda e: e.scalar_tensor_tensor(r[:], xt[:], ALPHA, self.bank(base, 2), ALU.mult, ALU.add), reads=[K("xt"), pb[0], pb[1]], writes=[K("r")])
        self.layer_norm(r[:], x1[:], G, B, [K("r")], [K("x1")], sl=b)
        P.op("act", lambda e: e.activation(x1b[:], x1[:], AF.Copy), reads=[K("x1")], writes=[K("x1b")])
        P.dma("act", lambda e: e.dma_start(out=self.res1[rows, :], in_=x1[:]), K("x1o"), reads=[K("x1")], writes=["res1_d"], partial=True)
        P.l.append(("mark",))
        pTf = self.PS[:, (base + 2) * 512:(base + 4) * 512].rearrange("p (c t) -> p c t", t=128)
        P.op("pe", multi([lambda e, c=c: e.transpose(pTf[:, c, :], x1[:, c * 128:(c + 1) * 128], self.identf[:]) for c in range(8)]), reads=[K("x1"), "identf"], writes=[pb[2], pb[3]])
        P.op("act", lambda e: e.activation(x1T[:], pTf, AF.Copy), reads=[pb[2], pb[3]], writes=[K("x1T")])
        pl_ = self.bank(base + 2)[:, 0:36]
        P.op("pe", multi([lambda e, c=c: e.matmul(pl_, x1T[:, c, :], wr[:, c, :], start=(c == 0), stop=(c == 7)) for c in range(8)]), reads=[K("x1T"), "wr"], writes=[pb[2]])
        P.op("dve", lambda e: e.tensor_tensor(lg[:], pl_, br[:], ALU.add), reads=[pb[2], "br"], writes=[K("lg")])
        P.op("dve", lambda e: e.tensor_reduce(sm[:, 0:1], lg[:, 0:4], AX.X, ALU.max), reads=[K("lg")], writes=[K("sm0")])
        P.op("dve", lambda e: e.tensor_scalar(pen[:], lg[:, 0:4], sm[:, 0:1], None, ALU.subtract), reads=[K("lg"), K("sm0")], writes=[K("pen")])
        P.op("act", lambda e: e.activation(sm[:, 4:8], pen[:], AF.Exp, accum_out=sm[:, 1:2]), reads=[K("pen")], writes=[K("sm1")])
        P.op("dve", lambda e: e.reciprocal(sm[:, 2:3], sm[:, 1:2]), reads=[K("sm1")], writes=[K("sm2")])
        P.op("dve", lambda e: e.tensor_scalar(pen[:], pen[:], 0.0, 10000.0, ALU.is_ge, ALU.mult), reads=[K("pen")], writes=[K("pen")])
        P.op("dve", lambda e: e.tensor_scalar(pen[:], pen[:], -10000.0, None, ALU.add), reads=[K("pen")], writes=[K("pen")])
        P.op("dve", lambda e: e.tensor_tensor(me[:].rearrange("p (g k) -> p g k", k=8), lg[:, 4:36].rearrange("p (g k) -> p g k", k=8),
                                              pen[:].unsqueeze(2).to_broadcast([128, 4, 8]), ALU.add), reads=[K("lg"), K("pen")], writes=[K("me")])
        P.op("dve", lambda e: e.max(top8[:], me[:]), reads=[K("me")], writes=[K("top8")])
        P.op("dve", lambda e: e.max_index(idx8[:], top8[:], me[:]), reads=[K("me"), K("top8")], writes=[K("idx8")])
        P.op("dve", lambda e: e.tensor_tensor(sm[:, 8:9], top8[:, 1:2], top8[:, 0:1], ALU.subtract), reads=[K("top8")], writes=[K("sm8")])
        P.op("act", lambda e: e.activation(sm[:, 9:10], sm[:, 8:9], AF.Exp), reads=[K("sm8")], writes=[K("sm9")])
        P.op("dve", lambda e: e.tensor_scalar(sm[:, 10:11], sm[:, 9:10], 1.0, None, ALU.add), reads=[K("sm9")], writes=[K("sm10")])
        P.op("dve", lambda e: e.reciprocal(sm[:, 10:11], sm[:, 10:11]), reads=[K("sm10")], writes=[K("sm10")])
        P.op("dve", lambda e: e.tensor_tensor(sm[:, 12:13], sm[:, 10:11], sm[:, 2:3], ALU.mult), reads=[K("sm10"), K("sm2")], writes=[K("sm12")])
        P.op("dve", lambda e: e.tensor_tensor(sm[:, 13:14], sm[:, 12:13], sm[:, 9:10], ALU.mult), reads=[K("sm12"), K("sm9")], writes=[K("sm13")])
        P.op("dve", lambda e: e.tensor_copy(ef[:], idx8[:, 0:2]), reads=[K("idx8")], writes=[K("ef")])
        for k in range(2):
            P.op("dve", lambda e, k=k: e.tensor_scalar(oh[:, k, :], self.iota32[:], ef[:, k:k + 1], None, ALU.is_equal), reads=[K("ef"), "iota32"], writes=[K("oh%d" % k)])
        P.op("dve", lambda e: e.tensor_tensor(Mb[:], oh[:, 0, :], oh[:, 1, :], ALU.add), reads=[K("oh0"), K("oh1")], writes=[K("Mb")])
        pr = self.bank(base + 3)[:, 0:32]
        pr2 = self.bank(base + 3)[:, 64:96]
        P.op("pe", multi([lambda e: e.matmul(pr, self.U[:], Mb[:], start=True, stop=True), lambda e: e.matmul(pr2, self.ones[:], Mb[:], start=True, stop=True)]),
             reads=["U", "ones", K("Mb")], writes=[pb[3]])
        P.op("dve", lambda e: e.tensor_tensor(pos[:], pr, cnt[:], ALU.add), reads=[pb[3], "cnt"], writes=[K("pos")])
        if b == 1:
            tot0 = self.bank(3)[:, 64:96]
            P.op("dve", lambda e: e.tensor_tensor(pos[:], tot0, pos[:], ALU.add), reads=["pb3", K("pos")], writes=[K("pos")])
            P.op("dve", lambda e: e.tensor_tensor(cnt[:], tot0, cnt[:], ALU.add), reads=["pb3", "cnt"], writes=["cnt"])
            P.op("dve", lambda e: e.tensor_tensor(cnt[:], pr2, cnt[:], ALU.add), reads=[pb[3], "cnt"], writes=["cnt"])
        P.op("dve", lambda e: e.tensor_tensor(prod[:], oh[:], pos[:].unsqueeze(1).to_broadcast([128, 2, 32]), ALU.mult), reads=[K("oh0"), K("oh1"), K("pos")], writes=[K("prod")])
        P.op("dve", lambda e: e.tensor_reduce(posf[:], prod[:], AX.X, ALU.add), reads=[K("prod")], writes=[K("posf")])
        P.op("dve", lambda e: e.tensor_scalar(slotf[:], ef[:], float(C), None, ALU.mult), reads=[K("ef")], writes=[K("slotf")])
        P.op("dve", lambda e: e.tensor_tensor(slotf[:], slotf[:], posf[:], ALU.add), reads=[K("slotf"), K("posf")], writes=[K("slotf")])
        P.op("dve", lambda e: e.tensor_scalar(ov[:], posf[:], float(C), None, ALU.is_ge), reads=[K("posf")], writes=[K("ov")])
        P.op("dve", lambda e: e.scalar_tensor_tensor(sidxf[:], ov[:], 1.0e6, slotf[:], ALU.mult, ALU.add), reads=[K("ov"), K("slotf")], writes=[K("sidxf")])
        P.op("dve", lambda e: e.tensor_copy(sidx[:], sidxf[:]), reads=[K("sidxf")], writes=[K("sidx")])
        P.op("dve", lambda e: e.tensor_scalar(gidxf[:], slotf[:], -1.0, float(self.NSLOT), ALU.mult, ALU.add), reads=[K("slotf")], writes=[K("gidxf")])
        P.op("dve", lambda e: e.tensor_tensor(gidxf[:], gidxf[:], ov[:], ALU.mult), reads=[K("gidxf"), K("ov")], writes=[K("gidxf")])
        P.op("dve", lambda e: e.tensor_tensor(gidxf[:], gidxf[:], slotf[:], ALU.add), reads=[K("gidxf"), K("slotf")], writes=[K("gidxf")])
        P.op("dve", lambda e: e.tensor_copy(self.gidx[:, tt, :], gidxf[:]), reads=[K("gidxf")], writes=["gidx"], partial=True)
        P.op("dve", lambda e: e.tensor_scalar(ov[:], ov[:], -1.0, 1.0, ALU.mult, ALU.add), reads=[K("ov")], writes=[K("ov")])
        P.op("dve", lambda e: e.tensor_tensor(self.gates[:, tt, :], sm[:, 12:14], ov[:], ALU.mult), reads=[K("sm12"), K("sm13"), K("ov")], writes=["gates"], partial=True)
        for k in range(0 if os.environ.get("NOSCAT") else 2):
            P.dma("pool", lambda e, k=k: e.indirect_dma_start(out=self.xs[:, :], out_offset=bass.IndirectOffsetOnAxis(ap=sidx[:, k:k + 1], axis=0),
                                                              in_=x1b[:], in_offset=None, bounds_check=self.bc_reg, oob_is_err=False),
                  K("scat"), reads=[K("x1b"), K("sidx")], writes=XS, partial=True, wfull=([] if os.environ.get("NOCHAIN") else ["xs_chain"]))

    def experts(self, l):
        P = self.P
        W = self.w
        C = self.C
        NS = C // 128
        HW = C // 2
        P.begin()
        wg = [P.sbuf("wg", [128, 8, 512], BF16) for _ in range(2)]
        wu = [P.sbuf("wu", [128, 8, 512], BF16) for _ in range(2)]
        wd = [P.sbuf("wd", [128, 4, D], BF16) for _ in range(2)]
        xst = [P.sbuf("xst", [128, D], BF16) for _ in range(2)]
        xsT = [P.sbuf("xsT", [128, 8, C], BF16) for _ in range(2)]
        hT = [P.sbuf("hT", [128, 4, C], BF16) for _ in range(2)]
        sg = [P.sbuf("sg", [128, HW], F32) for _ in range(2)]
        yt = [P.sbuf("yt", [128, D], F32) for _ in range(2)]
        it = 0
        for ex in range(32):
            eb = ex % 2
            self.load_w_bf16(wg[eb], W["w_gate"][l, ex], 8, 512, "wg%d" % eb)
            self.load_w_bf16(wu[eb], W["w_up"][l, ex], 8, 512, "wu%d" % eb)
            self.load_w_bf16(wd[eb], W["w_down"][l, ex], 4, D, "wd%d" % eb)
            for st in range(NS):
                b = st % 2
                r0 = ex * C + st * 128
                P.dma("sp", lambda e, r0=r0, b=b: e.dma_start(out=xst[b][:], in_=self.xs[r0:r0 + 128, :]), "xst%d" % b, reads=["xs%d" % ex], writes=["xst%d" % b])
                pT = self.bankbf(st % 2).rearrange("p (c t) -> p c t", t=128)
                P.op("pe", multi([lambda e, c=c: e.transpose(pT[:, c, :], xst[b][:, c * 128:(c + 1) * 128], self.ident[:]) for c in range(8)]), reads=["xst%d" % b, "ident"], writes=["pb%d" % (st % 2)])
                if st % 2 == 0:
                    P.op("dve", lambda e, st=st, pT=pT: e.tensor_copy(xsT[eb][:, :, st * 128:(st + 1) * 128], pT), reads=["pb0"], writes=["xsT%d" % eb], partial=True)
                else:
                    P.op("act", lambda e, st=st, pT=pT: e.activation(xsT[eb][:, :, st * 128:(st + 1) * 128], pT, AF.Copy), reads=["pb1"], writes=["xsT%d" % eb], partial=True)
            for f in range(4):
                for hf in range(2):
                    ib = it % 2
                    it += 1
                    pg = self.bank(2 + 2 * ib)[:, 0:HW]
                    pu = self.bank(3 + 2 * ib)[:, 0:HW]
                    P.op("pe", multi([lambda e, c=c: e.matmul(pg, wg[eb][:, c, f * 128:(f + 1) * 128], xsT[eb][:, c, hf * HW:(hf + 1) * HW], start=(c == 0), stop=(c == 7)) for c in range(8)]),
                         reads=["wg%d" % eb, "xsT%d" % eb], writes=["pb%d" % (2 + 2 * ib)])
                    P.op("pe", multi([lambda e, c=c: e.matmul(pu, wu[eb][:, c, f * 128:(f + 1) * 128], xsT[eb][:, c, hf * HW:(hf + 1) * HW], start=(c == 0), stop=(c == 7)) for c in range(8)]),
                         reads=["wu%d" % eb, "xsT%d" % eb], writes=["pb%d" % (3 + 2 * ib)])
                    P.op("act", lambda e, ib=ib, pg=pg: e.activation(sg[ib][:], pg, AF.Silu), reads=["pb%d" % (2 + 2 * ib)], writes=["sg%d" % ib])
                    P.op("dve", lambda e, ib=ib, pu=pu, f=f, hf=hf: e.tensor_tensor(hT[eb][:, f, hf * HW:(hf + 1) * HW], sg[ib][:], pu, ALU.mult),
                         reads=["sg%d" % ib, "pb%d" % (3 + 2 * ib)], writes=["hT%d" % eb], partial=True)
            for st in range(NS):
                b = st % 2
                r0 = ex * C + st * 128
                P.op("pe", multi([lambda e, n=n, f=f: e.matmul(self.bank(6 + n), hT[eb][:, f, st * 128:(st + 1) * 128], wd[eb][:, f, n * 512:(n + 1) * 512], start=(f == 0), stop=(f == 3)) for n in range(2) for f in range(4)]),
                     reads=["hT%d" % eb, "wd%d" % eb], writes=["pb6", "pb7"])
                if st % 2 == 0:
                    P.op("dve", lambda e, b=b: e.tensor_copy(yt[b][:], self.bank(6, 2)), reads=["pb6", "pb7"], writes=["yt%d" % b])
                else:
                    P.op("act", lambda e, b=b: e.activation(yt[b][:], self.bank(6, 2), AF.Copy), reads=["pb6", "pb7"], writes=["yt%d" % b])
                P.dma("act", lambda e, r0=r0, b=b: e.dma_start(out=self.ys[r0:r0 + 128, :], in_=yt[b][:]), "yo%d" % b, reads=["yt%d" % b], writes=["ys%d" % ex], partial=True)
        P.end()

    def combine(self, l, dst):
        P = self.P
        W = self.w
        P.begin()
        self.mk_eps()
        self.mk_lnt()
        G = self.bcast_load("G", W["ln2_g"][l], D, "lnG")
        B = self.bcast_load("B", W["ln2_b"][l], D, "lnB")
        x1 = [P.sbuf("x1", [128, D], F32) for _ in range(2)]
        y0 = [P.sbuf("y0", [128, D], F32) for _ in range(2)]
        y1 = [P.sbuf("y1", [128, D], F32) for _ in range(2)]
        f = [P.sbuf("f", [128, D], F32) for _ in range(2)]
        o = [P.sbuf("o", [128, D], F32) for _ in range(2)]
        YS = ["ys%d" % e_ for e_ in range(32)] + ["ysd"]

        def tile(tt, b):
            P = self.P
            rows = slice(tt * 128, (tt + 1) * 128)
            P.dma("sp", lambda e: e.dma_start(out=x1[b][:], in_=self.res1[rows, :]), "x1l%d" % b, reads=["res1_d"], writes=["x1_%d" % b])
            for k, yb in enumerate((y0, y1)):
                P.dma("pool", lambda e, k=k, yb=yb: e.indirect_dma_start(out=yb[b][:], out_offset=None, in_=self.ys[:, :],
                                                                        in_offset=bass.IndirectOffsetOnAxis(ap=self.gidx[:, tt, k:k + 1], axis=0)),
                      "gat%d_%d" % (k, b), reads=YS + ["gidx"], writes=["y%d_%d" % (k, b)])
            P.op("dve", lambda e: e.tensor_scalar(f[b][:], y0[b][:], self.gates[:, tt, 0:1], None, ALU.mult), reads=["y0_%d" % b, "gates"], writes=["f%d" % b])
            P.op("dve", lambda e: e.scalar_tensor_tensor(f[b][:], y1[b][:], self.gates[:, tt, 1:2], f[b][:], ALU.mult, ALU.add), reads=["y1_%d" % b, "gates", "f%d" % b], writes=["f%d" % b])
            P.op("dve", lambda e: e.scalar_tensor_tensor(f[b][:], x1[b][:], ALPHA, f[b][:], ALU.mult, ALU.add), reads=["x1_%d" % b, "f%d" % b], writes=["f%d" % b])
            self.layer_norm(f[b][:], o[b][:], G, B, ["f%d" % b], ["o%d" % b], sl=b)
            P.dma("act", lambda e: e.dma_start(out=dst[rows, :], in_=o[b][:]), "oo%d" % b, reads=["o%d" % b], writes=["dst_d"], partial=True)
        for tt in range(0, self.NT, 2):
            recs = []
            for sl in (0, 1):
                r_ = Rec()
                self.P = r_
                tile(tt + sl, sl)
                recs.append(r_)
            self.P = P
            interleave(P, recs)
        P.end()


def prep_shared(inp):
    f32 = np.float32
    sh = {}
    sh["even_w_in"] = np.ascontiguousarray(inp["even_w_in"], f32)
    sh["even_lam"] = np.ascontiguousarray(np.stack([inp["even_lam_q1"], inp["even_lam_k1"], inp["even_lam_q2"], inp["even_lam_k2"]], axis=1), f32)
    for k in ("even_subln_g", "even_q_norm_g", "even_w_uq", "even_kv_norm_g", "even_w_ukv", "even_w_o", "odd_w_qkv", "odd_w_o",
              "ln1_g", "ln1_b", "ln2_g", "ln2_b", "w_gate", "w_up", "w_down"):
        sh[k] = np.ascontiguousarray(inp[k], f32)
    sh["w_router"] = np.ascontiguousarray(np.concatenate([inp["w_router_group"], inp["w_router_expert"]], axis=2), f32)
    sh["b_router"] = np.ascontiguousarray(np.concatenate([inp["b_router_group"], inp["b_router_expert"]], axis=1), f32)
    idx = nbr_index_table()
    rb = np.asarray(inp["odd_rel_bias"], f32).reshape(2, 16, 15 * 31)
    ext = np.concatenate([rb, np.full((2, 16, 1), NEG, f32)], axis=2)
    sh["nbias"] = np.ascontiguousarray(ext[:, :, idx], f32)
    ra, rb_ = rope_host()
    sh["ropeA"], sh["ropeB"] = ra, rb_
    return sh


def kernel(**inputs):
    from concourse.bass_utils import run_bass_kernel_spmd
    n = 8
    nseq = 4
    inp = {k: np.asarray(v) for k, v in inputs.items()}
    sh = prep_shared(inp)
    x = np.ascontiguousarray(inp["x"], np.float32)
    mk = MK(nseq=nseq, C=768, layers=(0, 1, 2, 3), debug=False)
    in_maps = []
    for c in range(n):
        d = {k: v for k, v in sh.items() if k in mk.w}
        d["x"] = np.ascontiguousarray(x[c * nseq:(c + 1) * nseq].reshape(nseq * S, D))
        in_maps.append(d)
    res = run_bass_kernel_spmd(mk.nc, in_maps, core_ids=list(range(n)))
    out = np.concatenate([np.asarray(r["y"], np.float32).reshape(nseq, S, D) for r in res.results], axis=0)
    return out
```

```python
import numpy as np
import concourse.bass as bass
import concourse.mybir as mybir
from contextlib import ExitStack

F32 = mybir.dt.float32
BF16 = mybir.dt.bfloat16
I32 = mybir.dt.int32
U32 = mybir.dt.uint32
AF = mybir.ActivationFunctionType
ALU = mybir.AluOpType
AX = mybir.AxisListType

SEM_LIMIT = 30000
MAXOPS = 10 ** 9


class Prog:
    def __init__(self, nc):
        self.nc = nc
        self.gstack = ExitStack()
        self.pstack = None
        self.engs = {"pe": nc.tensor, "act": nc.scalar, "dve": nc.vector, "pool": nc.gpsimd, "sp": nc.sync}
        self._nsem = 0
        self.eng_sem, self.eng_cnt, self.grp_sem, self.grp_cnt = {}, {}, {}, {}
        self.waited = {e: {} for e in self.engs}
        self.prev_final = {}
        self.cur_final = {}
        self.need_bar = set()
        self.tot = dict(n_ops=0, n_wait=0)
        self._nm = 0
        self.W, self.R, self.G = {}, {}, {}

    def sbuf(self, name, shape, dt, persist=False):
        self._nm += 1
        st = self.gstack if (persist or self.pstack is None) else self.pstack
        return st.enter_context(self.nc.sbuf_tensor("%s_%d" % (name, self._nm), list(shape), dt))

    def psum(self, name, shape, dt):
        return self.gstack.enter_context(self.nc.psum_tensor(name, list(shape), dt))

    def new_sem(self):
        self._nsem += 1
        return self.gstack.enter_context(self.nc.semaphore("s%d" % self._nsem))

    def begin(self):
        self.pstack = ExitStack()
        self.prev_final = dict(self.cur_final)
        self.need_bar = set(self.engs)
        self.W, self.R, self.G = {}, {}, {}

    def end(self):
        self.pstack.close()
        self.pstack = None

    def _do(self, engn, fn, reads, writes, partial, dma, group, wfull=()):
        if self.tot["n_ops"] >= MAXOPS:
            return
        W, R, G = self.W, self.R, self.G
        deps = []
        for k in reads:
            deps.extend(W.get(k, ()))
            if k.startswith("pb"):
                deps.extend(r for r in R.get(k, ()) if r[2] != engn)
        newgen = []
        for k in wfull:
            d = list(W.get(k, ())) + list(R.get(k, ()))
            deps.extend(d)
            newgen.append((k, d))
        for k in writes:
            if partial and k in W and not R.get(k):
                deps.extend(G[k])
            else:
                d = list(W.get(k, ())) + list(R.get(k, ()))
                deps.extend(d)
                newgen.append((k, d))
        eng = self.engs[engn]
        w = self.waited[engn]
        need = {}
        if engn in self.need_bar:
            self.need_bar.discard(engn)
            for num, (s, v) in self.prev_final.items():
                need[num] = (s, v)
        for (s, v, de, ddma) in deps:
            if engn == "pe" and not dma and de == "pe" and not ddma:
                continue
            if need.get(s.num, (None, 0))[1] < v:
                need[s.num] = (s, v)
        for num, (s, v) in need.items():
            if w.get(num, 0) < v:
                eng.wait_ge(s, v)
                w[num] = v
                self.tot["n_wait"] += 1
        ins = fn(eng)
        if dma:
            g = group
            if g not in self.grp_sem or self.grp_cnt[g] + 16 > SEM_LIMIT:
                self.grp_sem[g] = self.new_sem()
                self.grp_cnt[g] = 0
            self.grp_cnt[g] += 16
            sem, val = self.grp_sem[g], self.grp_cnt[g]
            ins.then_inc(sem, 16)
        else:
            e = engn
            if e not in self.eng_sem or self.eng_cnt[e] + 1 > SEM_LIMIT:
                self.eng_sem[e] = self.new_sem()
                self.eng_cnt[e] = 0
            self.eng_cnt[e] += 1
            sem, val = self.eng_sem[e], self.eng_cnt[e]
            ins.then_inc(sem, 1)
        self.cur_final[sem.num] = (sem, val)
        rec = (sem, val, engn, dma)
        for k, d in newgen:
            G[k] = d
            W[k] = []
            R[k] = []
        for k in tuple(writes) + tuple(wfull):
            W[k].append(rec)
        for k in reads:
            R.setdefault(k, []).append(rec)
        self.tot["n_ops"] += 1

    def op(self, eng, fn, reads=(), writes=(), partial=False):
        self._do(eng, fn, reads, writes, partial, False, None)

    def dma(self, eng, fn, group, reads=(), writes=(), partial=False, wfull=()):
        self._do(eng, fn, reads, writes, partial, True, group, wfull)

    def finish(self):
        sp = self.engs["sp"]
        w = self.waited["sp"]
        for num, (s, v) in self.cur_final.items():
            if w.get(num, 0) < v:
                sp.wait_ge(s, v)
                w[num] = v
        self.tot["n_sem"] = self._nsem
        return self.tot


class Rec:
    def __init__(self):
        self.l = []

    def op(self, *a, **k):
        self.l.append(("op", a, k))

    def dma(self, *a, **k):
        self.l.append(("dma", a, k))


def interleave(P, recs):
    from itertools import zip_longest
    for group in zip_longest(*[r.l for r in recs]):
        for item in group:
            if item is not None and item[0] != "mark":
                getattr(P, item[0])(*item[1], **item[2])


import math
import os
DVX = 0
import numpy as np

D = 1024
S = 2048
T = 16
DEPTH = 4
ALPHA = (2 * DEPTH) ** 0.25
NEG = -30000.0
NPAT = 21


def nbr_patterns():
    pats = {}
    p = 5
    out = []
    for qt in range(16):
        if 2 <= qt <= 13:
            out.append([(qt - 2 + i, i) for i in range(5)])
        else:
            kts = [0, 1, 2, 3] if qt < 2 else [12, 13, 14, 15]
            lst = []
            for kt in kts:
                lst.append((kt, p))
                p += 1
            out.append(lst)
    assert p == NPAT
    return out


def nbr_index_table():
    pl = nbr_patterns()
    idx = np.full((128, NPAT, 128), 15 * 31, np.int64)
    done = set()
    for qt in range(16):
        for kt, p in pl[qt]:
            if p in done:
                continue
            done.add(p)
            for q in range(128):
                r, c = qt * 2 + q // 64, q % 64
                r0 = min(max(r - 4, 0), 24)
                c0 = min(max(c - 8, 0), 48)
                for k in range(128):
                    rk, ck = kt * 2 + k // 64, k % 64
                    if r0 <= rk < r0 + 8 and c0 <= ck < c0 + 16:
                        idx[k, p, q] = (rk - r + 7) * 31 + (ck - c + 15)
    return idx


def rope_host():
    def tab(rot):
        inv = (np.float32(500000.0) ** (-np.arange(0, rot, 2, dtype=np.float32) / np.float32(rot))).astype(np.float32)
        ang = (np.arange(S, dtype=np.float32)[:, None] * inv[None, :]).astype(np.float32)
        cos, sin = np.cos(ang).astype(np.float32), np.sin(ang).astype(np.float32)
        t = np.concatenate([cos, cos, -sin, sin], axis=1)
        return np.ascontiguousarray(t.reshape(T, 128, 2 * rot).transpose(1, 0, 2))
    return tab(16), tab(32)


def multi(fns):
    def f(e):
        ins = None
        for g in fns:
            ins = g(e)
        return ins
    return f


class MK:
    def __init__(self, nseq=4, C=768, layers=(0, 1, 2, 3), debug=False, phases="ABCOEM"):
        self.nseq, self.C, self.layers, self.debug = nseq, C, tuple(layers), debug
        self.phases = phases
        self.NT = nseq * T
        self.NTOK = nseq * S
        self.NSLOT = 32 * C
        nc = self.nc = bass.Bass("TRN2", target_bir_lowering=False)
        P = self.P = Prog(nc)
        NTOK = self.NTOK

        def din(name, shape, dt=F32):
            return nc.dram_tensor(name, list(shape), dt, kind="ExternalInput").ap()

        def dint(name, shape, dt=F32):
            return nc.dram_tensor(name, list(shape), dt, kind="Internal").ap()
        self.x = din("x", [NTOK, D])
        self.w = {}
        for nm, shp in [("even_w_in", [2, D, 1952]), ("even_lam", [2, 4, 64]), ("even_subln_g", [2, 128]),
                        ("even_q_norm_g", [2, 256]), ("even_w_uq", [2, 256, 768]), ("even_kv_norm_g", [2, 128]),
                        ("even_w_ukv", [2, 128, 1024]), ("even_w_o", [2, D, D]), ("odd_w_qkv", [2, D, 3072]),
                        ("nbias", [2, 16, 128, NPAT, 128]), ("odd_w_o", [2, D, D]),
                        ("ln1_g", [4, D]), ("ln1_b", [4, D]), ("ln2_g", [4, D]), ("ln2_b", [4, D]),
                        ("w_router", [4, D, 36]), ("b_router", [4, 36]),
                        ("w_gate", [4, 32, D, 512]), ("w_up", [4, 32, D, 512]), ("w_down", [4, 32, 512, D]),
                        ("ropeA", [128, 16, 32]), ("ropeB", [128, 16, 64])]:
            if nm in ("w_gate", "w_up", "w_down") and "E" not in self.phases:
                continue
            if nm in ("odd_w_qkv", "nbias") and ("C" not in self.phases or all(l % 2 == 0 for l in self.layers)):
                continue
            self.w[nm] = din(nm, shp)
        self.y = nc.dram_tensor("y", [NTOK, D], F32, kind="ExternalOutput").ap()
        self.res1 = dint("res1", [NTOK, D]) if not debug else nc.dram_tensor("res1", [NTOK, D], F32, kind="ExternalOutput").ap()
        self.resA = dint("resA", [NTOK, D])
        self.ao = dint("ao", [NTOK, D], BF16) if not debug else nc.dram_tensor("ao", [NTOK, D], BF16, kind="ExternalOutput").ap()
        self.xs = dint("xs", [self.NSLOT, D], BF16)
        self.ys = dint("ys", [self.NSLOT + 128, D])
        self.PS = P.psum("PS", [128, 4096], F32)
        self.bc_reg = nc.gpsimd.alloc_register("bcr")
        nc.gpsimd.reg_mov(self.bc_reg, self.NSLOT - 1)
        self.consts()
        for l in self.layers:
            src = self.x if l == self.layers[0] else self.resA
            if l % 2 == 0:
                self.even_attn(l, src)
            elif "C" in self.phases:
                self.odd_attn(l, src)
            if "O" in self.phases:
                self.oproj(l, src)
            if "E" in self.phases:
                self.experts(l)
            dst = self.y if l == self.layers[-1] else self.resA
            if "M" in self.phases:
                self.combine(l, dst)
        self.stats = P.finish()

    def bank(self, b, n=1):
        return self.PS[:, b * 512:(b + n) * 512]

    def bankbf(self, b):
        return self.PS[:, b * 512:(b + 1) * 512].bitcast(BF16)

    def consts(self):
        P = self.P
        P.begin()
        self.ident = P.sbuf("ident", [128, 128], BF16, persist=True)
        self.identf = P.sbuf("identf", [128, 128], F32, persist=True)
        self.U = P.sbuf("U", [128, 128], BF16, persist=True)
        self.ones = P.sbuf("ones", [128, 128], BF16, persist=True)
        self.iota32 = P.sbuf("iota32", [128, 32], F32, persist=True)
        self.eC = P.sbuf("eC", [128, 32], F32, persist=True)
        self.ropeA = P.sbuf("ropeA", [128, 16, 32], F32, persist=True)
        self.ropeB = P.sbuf("ropeB", [128, 16, 64], F32, persist=True)
        self.gidx = P.sbuf("gidx", [128, self.NT, 2], I32, persist=True)
        self.gates = P.sbuf("gates", [128, self.NT, 2], F32, persist=True)
        self.zero = P.sbuf("zero", [128, D], F32, persist=True)
        iot = P.sbuf("iot", [128, 128], F32)
        iop = P.sbuf("iop", [128, 1], F32)
        P.op("pool", lambda e: e.iota(iot[:], [[1, 128]], base=0, channel_multiplier=0, allow_small_or_imprecise_dtypes=True), writes=["iot"])
        P.op("pool", lambda e: e.iota(iop[:], [[0, 1]], base=0, channel_multiplier=1, allow_small_or_imprecise_dtypes=True), writes=["iop"])
        P.op("dve", lambda e: e.tensor_scalar(self.ident[:], iot[:], iop[:, 0:1], None, ALU.is_equal), reads=["iot", "iop"], writes=["ident"])
        P.op("dve", lambda e: e.tensor_scalar(self.identf[:], iot[:], iop[:, 0:1], None, ALU.is_equal), reads=["iot", "iop"], writes=["identf"])
        P.op("dve", lambda e: e.tensor_scalar(self.U[:], iot[:], iop[:, 0:1], None, ALU.is_gt), reads=["iot", "iop"], writes=["U"])
        P.op("dve", lambda e: e.memset(self.ones[:], 1.0), writes=["ones"])
        P.op("dve", lambda e: e.tensor_copy(self.iota32[:], iot[:, 0:32]), reads=["iot"], writes=["iota32"])
        P.op("dve", lambda e: e.tensor_scalar(self.eC[:], iot[:, 0:32], float(self.C), None, ALU.mult), reads=["iot"], writes=["eC"])
        P.op("dve", lambda e: e.memset(self.zero[:], 0.0), writes=["zero"])
        P.dma("sp", lambda e: e.dma_start(out=self.ropeA[:], in_=self.w["ropeA"]), "c0", writes=["ropeA"])
        P.dma("sp", lambda e: e.dma_start(out=self.ropeB[:], in_=self.w["ropeB"]), "c0", writes=["ropeB"])
        P.dma("sp", lambda e: e.dma_start(out=self.ys[self.NSLOT:self.NSLOT + 128, :], in_=self.zero[:]), "c0", reads=["zero"], writes=["ysd"])
        zb = P.sbuf("zb", [128, 8, D], BF16)
        P.op("pool", lambda e: e.memset(zb[:], 0.0), writes=["zb"])
        for i in range(self.NSLOT // 1024):
            P.dma("sp", lambda e, i=i: e.dma_start(out=self.xs[i * 1024:(i + 1) * 1024, :].rearrange("(p n) d -> p n d", n=8), in_=zb[:]), "c0", reads=["zb"], writes=["xsz"], partial=True)
        P.end()

    def rstd(self, out, in_, scale, eps, rk, wk):
        P = self.P
        P.op("act", lambda e: e.activation(out, in_, AF.Ln, bias=self.epst(eps), scale=scale), reads=rk, writes=wk)
        P.op("act", lambda e: e.activation(out, out, AF.Exp, scale=-0.5), reads=wk, writes=wk)

    def epst(self, eps):
        return self._eps[eps][:, 0:1]

    def mk_eps(self):
        P = self.P
        self._eps = {}
        for eps in (1e-5, 1e-6):
            t = P.sbuf("eps", [128, 1], F32)
            self._eps[eps] = t
            P.op("dve", lambda e, t=t, eps=eps: e.memset(t[:], eps), writes=["eps%g" % eps])

    def bcast_load(self, name, src_row, n, key, dt=F32):
        P = self.P
        t = P.sbuf(name, [128, n], dt)
        P.dma("sp", lambda e: e.dma_start(out=t[:], in_=src_row.unsqueeze(0).broadcast_to([128, n])), "bc_" + key, writes=[key])
        return t

    def load_w_bf16(self, dst, src2d, kchunks, ncols, key, c0=0):
        P = self.P
        for k in range(kchunks):
            P.dma("pool", lambda e, k=k: e.dma_start(out=dst[:, k, c0:c0 + ncols], in_=src2d[k * 128:(k + 1) * 128, :]),
                  "w_" + key, writes=[key], partial=True)

    def x_front(self, src, tt, b, bufs):
        P = self.P
        xt, xb, xT, pbank = bufs
        P.dma("sp", lambda e: e.dma_start(out=xt[b][:], in_=src[tt * 128:(tt + 1) * 128, :]), "xt%d" % b, writes=["xt%d" % b])
        P.op("act", lambda e: e.activation(xb[b][:], xt[b][:], AF.Copy), reads=["xt%d" % b], writes=["xb%d" % b])
        pT = self.bankbf(pbank).rearrange("p (c t) -> p c t", t=128)
        P.op("pe", multi([lambda e, c=c: e.transpose(pT[:, c, :], xb[b][:, c * 128:(c + 1) * 128], self.ident[:]) for c in range(8)]),
             reads=["xb%d" % b, "ident"], writes=["pb%d" % pbank])
        P.op("dve", lambda e: e.tensor_copy(xT[b][:], pT), reads=["pb%d" % pbank], writes=["xT%d" % b])

    def rope(self, src3, dst3, tab, r0, half, ng, rk, wk, tmpk, temps=None):
        P = self.P
        t1, t2 = temps if temps is not None else self._ropet
        h2 = 2 * half
        a = t1[:, 0:ng, 0:h2]
        b_ = t2[:, 0:ng, 0:h2]
        cc = tab[:, 0:h2].unsqueeze(1).to_broadcast([128, ng, h2])
        ns = tab[:, h2:h2 + half].unsqueeze(1).to_broadcast([128, ng, half])
        ps = tab[:, h2 + half:h2 + 2 * half].unsqueeze(1).to_broadcast([128, ng, half])
        P.op("dve", lambda e: e.tensor_tensor(a, src3[:, :, r0:r0 + h2], cc, ALU.mult), reads=rk, writes=[tmpk + "1"])
        P.op("dve", lambda e: e.tensor_tensor(b_[:, :, 0:half], src3[:, :, r0 + half:r0 + h2], ns, ALU.mult), reads=rk, writes=[tmpk + "2"])
        P.op("dve", lambda e: e.tensor_tensor(b_[:, :, half:h2], src3[:, :, r0:r0 + half], ps, ALU.mult), reads=rk, writes=[tmpk + "2"], partial=True)
        P.op("pool", lambda e: e.tensor_tensor(dst3[:, :, r0:r0 + h2], a, b_, ALU.add), reads=[tmpk + "1", tmpk + "2"], writes=wk)

    def layer_norm(self, r, out, G, B, rk, wk, sl=0):
        P = self.P
        st, mv, rs = self._lnt[sl]
        kst, kmv, krs = "lnst%d" % sl, "lnmv%d" % sl, "lnrs%d" % sl
        P.op("dve", lambda e: e.bn_stats(st[:, 0, :], r[:, 0:512]), reads=rk, writes=[kst])
        P.op("dve", lambda e: e.bn_stats(st[:, 1, :], r[:, 512:1024]), reads=rk, writes=[kst], partial=True)
        P.op("dve", lambda e: e.bn_aggr(mv[:], st[:]), reads=[kst], writes=[kmv])
        self.rstd(rs[:], mv[:, 1:2], 1.0, 1e-5, [kmv, "eps1e-05"], [krs])
        P.op("dve", lambda e: e.tensor_scalar(out, r, mv[:, 0:1], rs[:, 0:1], ALU.subtract, ALU.mult), reads=rk + [kmv, krs], writes=wk)
        P.op("pool", lambda e: e.tensor_tensor(out, out, G[:], ALU.mult), reads=wk + ["lnG"], writes=wk)
        P.op("pool", lambda e: e.tensor_tensor(out, out, B[:], ALU.add), reads=wk + ["lnB"], writes=wk)

    def mk_lnt(self):
        P = self.P
        self._lnt = [(P.sbuf("lnst", [128, 2, 6], F32), P.sbuf("lnmv", [128, 2], F32), P.sbuf("lnrs", [128, 1], F32)) for _ in range(2)]

    def qkv_tile(self, src, tt, t, sl, wt, wkey, QT, KT, V, vd, rope_tab, B):
        P = self.P
        xt, xb, xT, qkb, rts = B
        base = 4 * sl
        self.x_front(src, tt, sl, (xt, xb, xT, base + 3))
        for n in range(3):
            P.op("pe", multi([lambda e, n=n, c=c: e.matmul(self.bank(base + n), xT[sl][:, c, :], wt[:, c, n * 512:(n + 1) * 512], start=(c == 0), stop=(c == 7)) for c in range(8)]),
                 reads=["xT%d" % sl, wkey], writes=["pb%d" % (base + n)])
        ph = self.bank(base, 3)
        kq = "qkb%d" % sl
        pk = ["pb%d" % base, "pb%d" % (base + 1)]
        P.op("act", lambda e: e.activation(qkb[sl][:], ph[:, 0:1024], AF.Copy), reads=pk, writes=[kq])
        if rope_tab is not None:
            src3 = ph[:, 0:1024].rearrange("p (g d) -> p g d", d=64)
            dst3 = qkb[sl][:].rearrange("p (g d) -> p g d", d=64)
            self.rope(src3, dst3, rope_tab, 0, 8, 16, pk + ["ropeA"], [kq], "rt%d" % sl, rts[sl])
        pT2 = self.bankbf(base + 3).rearrange("p (c t) -> p c t", t=128)
        P.op("pe", multi([lambda e, c=c: e.transpose(pT2[:, c, :], qkb[sl][:, c * 128:(c + 1) * 128], self.ident[:]) for c in range(8)]),
             reads=[kq, "ident"], writes=["pb%d" % (base + 3)])
        P.op("dve", lambda e: e.tensor_copy(QT[:, :, t * 128:(t + 1) * 128], pT2[:, 0:4, :]), reads=["pb%d" % (base + 3)], writes=["QT"], partial=True)
        P.op("act", lambda e: e.activation(KT[:, :, t * 128:(t + 1) * 128], pT2[:, 4:8, :], AF.Copy), reads=["pb%d" % (base + 3)], writes=["KT"], partial=True)
        P.op("act", lambda e: e.activation(V[:, t, :, 0:vd], self.bank(base + 2).rearrange("p (h d) -> p h d", d=vd), AF.Copy),
             reads=["pb%d" % (base + 2), "Vones"], writes=["V"], partial=True)

    def qkv_seq(self, src, s, wt, wkey, QT, KT, V, vd, rope, B):
        P = self.P
        for t in range(0, T, 2):
            recs = []
            for sl in (0, 1):
                r = Rec()
                self.P = r
                self.qkv_tile(src, s * T + t + sl, t + sl, sl, wt, wkey, QT, KT, V, vd, (self.ropeA[:, t + sl, :] if rope else None), B)
                recs.append(r)
            self.P = P
            interleave(P, recs)

    def dense_attn(self, maps, QT, KT, V, dk, dv, scale, epilogue):
        P = self.P
        E = self._E
        steps = [(qc, mi, kp) for qc in range(4) for mi in range(len(maps)) for kp in range(8)]
        ps_all = self.PS[:, 2 * 512:6 * 512].rearrange("p (b c) -> p b c", c=512)
        pairs = [0, 6]

        def qk(i):
            qc, mi, kp = steps[i]
            blk, r0, vi = maps[mi]
            b0 = pairs[i % 2]
            P.op("pe", multi([lambda e, j=j: e.matmul(self.bank(b0 + j), KT[r0:r0 + dk, blk, (2 * kp + j) * 128:(2 * kp + j + 1) * 128],
                                                      QT[r0:r0 + dk, blk, qc * 512:(qc + 1) * 512], start=True, stop=True) for j in range(2)]),
                 reads=["QT", "KT"], writes=["pb%d" % b0, "pb%d" % (b0 + 1)])
            eb = i % 3
            P.op("act", lambda e: e.activation(E[eb][:], self.bank(b0, 2), AF.Exp, scale=scale), reads=["pb%d" % b0, "pb%d" % (b0 + 1)], writes=["E%d" % eb])

        def pv(i):
            qc, mi, kp = steps[i]
            blk, r0, vi = maps[mi]
            eb = i % 3
            P.op("pe", multi([lambda e, qi=qi, j=j: e.matmul(self.bank(2 + qi)[:, 0:dv + 1 - DVX], E[eb][:, j * 512 + qi * 128:j * 512 + (qi + 1) * 128],
                                                             V[:, 2 * kp + j, vi, 0:dv + 1 - DVX], start=(kp == 0 and j == 0), stop=(kp == 7 and j == 1))
                              for j in range(2) for qi in range(4)]),
                 reads=["E%d" % eb, "V"], writes=["pb2", "pb3", "pb4", "pb5"], partial=(kp != 0))
            if kp == 7 and not None:
                epilogue(qc, mi, ps_all)
        n = len(steps)
        if None:
            return
        for i in range(n + 1):
            if i < n:
                qk(i)
            if i >= 1:
                pv(i - 1)

    def even_attn(self, l, src):
        P = self.P
        j = l // 2
        W = self.w
        lam_init = 0.8 - 0.6 * math.exp(-0.3 * l)
        if "A" not in self.phases:
            return self.even_attn_B(l, src)
        P.begin()
        self.mk_eps()
        wt = P.sbuf("wA", [128, 8, 1536], BF16)
        self.load_w_bf16(wt, W["even_w_in"][j][:, 0:1536], 8, 1536, "wA")
        xt = [P.sbuf("xt", [128, D], F32) for _ in range(2)]
        xb = [P.sbuf("xb", [128, D], BF16) for _ in range(2)]
        xT = [P.sbuf("xT", [128, 8, 128], BF16) for _ in range(2)]
        QT = P.sbuf("QT", [128, 4, S], BF16)
        KT = P.sbuf("KT", [128, 4, S], BF16)
        V = P.sbuf("V", [128, 16, 4, 129], BF16)
        qkb = [P.sbuf("qkb", [128, 1024], BF16) for _ in range(2)]
        rts = [(P.sbuf("rt1", [128, 16, 32], F32), P.sbuf("rt2", [128, 16, 32], F32)) for _ in range(2)]
        self._E = [P.sbuf("E", [128, 1024], BF16) for _ in range(3)]
        Om = [P.sbuf("Om", [128, 4, 128], F32) for _ in range(2)]
        rec = P.sbuf("rec", [128, 4], F32)
        dd = P.sbuf("dd", [128, 4, 128], F32)
        sq = P.sbuf("sq", [128, 4, 128], F32)
        ss = P.sbuf("ss", [128, 4], F32)
        AO = [P.sbuf("AO", [128, 4, 512], BF16) for _ in range(2)]
        lamv = P.sbuf("lamv", [128, 4, 64], F32)
        P.dma("sp", lambda e: e.dma_start(out=lamv[:], in_=W["even_lam"][j].unsqueeze(0).broadcast_to([128, 4, 64])), "bc_lamv", writes=["lamv"])
        lp = P.sbuf("lp", [128, 2, 64], F32)
        ls = P.sbuf("ls", [128, 2], F32)
        nlam = P.sbuf("nlam", [128, 1], F32)
        P.op("dve", lambda e: e.tensor_tensor(lp[:], lamv[:, 0:4:2, :], lamv[:, 1:4:2, :], ALU.mult), reads=["lamv"], writes=["lp"])
        P.op("dve", lambda e: e.tensor_reduce(ls[:], lp[:], AX.X, ALU.add), reads=["lp"], writes=["ls"])
        P.op("act", lambda e: e.activation(ls[:], ls[:], AF.Exp), reads=["ls"], writes=["ls"])
        P.op("dve", lambda e: e.tensor_tensor(nlam[:], ls[:, 1:2], ls[:, 0:1], ALU.subtract), reads=["ls"], writes=["nlam"])
        P.op("dve", lambda e: e.tensor_scalar(nlam[:], nlam[:], -lam_init, None, ALU.add), reads=["nlam"], writes=["nlam"])
        gsub = self.bcast_load("gsub", W["even_subln_g"][j], 128, "gsub")
        P.op("dve", lambda e: e.tensor_scalar(gsub[:], gsub[:], 1.0 - lam_init, None, ALU.mult), reads=["gsub"], writes=["gsub"])
        P.op("dve", lambda e: e.memset(V[:, :, :, 128:129], 1.0), writes=["Vones"])
        for s in range(self.nseq):
            self.qkv_seq(src, s, wt, "wA", QT, KT, V, 128, True, (xt, xb, xT, qkb, rts))

            def epi(qc, mi, ps_all, s=s):
                h, m = mi // 2, mi % 2
                ab = (qc % 2)
                P.op("dve", lambda e: e.reciprocal(rec[:], ps_all[:, :, 128]), reads=["pb2", "pb3", "pb4", "pb5"], writes=["rec"])
                P.op("dve", lambda e: e.tensor_tensor(Om[m][:], ps_all[:, :, 0:128], rec[:].unsqueeze(2).to_broadcast([128, 4, 128]), ALU.mult),
                     reads=["pb2", "pb3", "pb4", "pb5", "rec"], writes=["Om%d" % m])
                if m == 1:
                    P.op("dve", lambda e: e.scalar_tensor_tensor(dd[:], Om[1][:], nlam[:, 0:1], Om[0][:], ALU.mult, ALU.add), reads=["Om0", "Om1", "nlam"], writes=["dd"])
                    P.op("pool", lambda e: e.tensor_tensor(sq[:], dd[:], dd[:], ALU.mult), reads=["dd"], writes=["sq"])
                    P.op("dve", lambda e: e.tensor_reduce(ss[:], sq[:], AX.X, ALU.add), reads=["sq"], writes=["ss"])
                    self.rstd(ss[:], ss[:], 1.0 / 128, 1e-6, ["ss", "eps1e-06"], ["ss"])
                    P.op("dve", lambda e: e.tensor_tensor(dd[:], dd[:], ss[:].unsqueeze(2).to_broadcast([128, 4, 128]), ALU.mult), reads=["dd", "ss"], writes=["dd"])
                    P.op("pool", lambda e: e.tensor_tensor(AO[ab][:, :, h * 128:(h + 1) * 128], dd[:], gsub[:].unsqueeze(1).to_broadcast([128, 4, 128]), ALU.mult),
                         reads=["dd", "gsub"], writes=["AO%d" % ab], partial=True)
                    if h == 3:
                        r0 = s * S + qc * 512
                        P.dma("pool", lambda e: e.dma_start(out=self.ao[r0:r0 + 512, 0:512].rearrange("(q p) n -> p q n", p=128), in_=AO[ab][:]),
                              "ao%d" % ab, reads=["AO%d" % ab], writes=["ao_d"], partial=True)
            maps = [(g // 2, (0 if None else (g % 2) * 64), g // 2) for g in range(8)]
            self.dense_attn(maps, QT, KT, V, 64, 128, 0.125, epi)
        P.end()
        self.even_attn_B(l, src)

    def even_attn_B(self, l, src):
        P = self.P
        j = l // 2
        W = self.w
        if "B" not in self.phases:
            return
        P.begin()
        self.mk_eps()
        wt = P.sbuf("wB", [128, 8, 416], BF16)
        self.load_w_bf16(wt, W["even_w_in"][j][:, 1536:1952], 8, 416, "wB")
        wuq = P.sbuf("wuq", [128, 2, 768], BF16)
        self.load_w_bf16(wuq, W["even_w_uq"][j], 2, 768, "wuq")
        wukv = P.sbuf("wukv", [128, 1, 1024], BF16)
        self.load_w_bf16(wukv, W["even_w_ukv"][j], 1, 1024, "wukv")
        qg = self.bcast_load("qg", W["even_q_norm_g"][j], 256, "qg")
        kvg = self.bcast_load("kvg", W["even_kv_norm_g"][j], 128, "kvg")
        xt = [P.sbuf("xt", [128, D], F32) for _ in range(2)]
        xb = [P.sbuf("xb", [128, D], BF16) for _ in range(2)]
        xT = [P.sbuf("xT", [128, 8, 128], BF16) for _ in range(2)]
        QT = P.sbuf("QT", [128, 8, S], BF16)
        KT = P.sbuf("KT", [128, 8, S], BF16)
        V = P.sbuf("V", [128, 16, 8, 65], BF16)
        self._ropet = (P.sbuf("rt1", [128, 8, 32], F32), P.sbuf("rt2", [128, 8, 32], F32))
        self._E = [P.sbuf("E", [128, 1024], BF16) for _ in range(3)]
        junk = P.sbuf("junk", [128, 384], F32)
        ssq = P.sbuf("ssq", [128, 2], F32)
        cn = P.sbuf("cn", [128, 384], BF16)
        cT = P.sbuf("cT", [128, 3, 128], BF16)
        qb = P.sbuf("qb", [128, 8, 96], BF16)
        kcat = P.sbuf("kcat", [128, 8, 96], BF16)
        krr = P.sbuf("krr", [128, 1, 32], BF16)
        rec = P.sbuf("rec", [128, 4], F32)
        AO = [P.sbuf("AO", [128, 4, 512], BF16) for _ in range(2)]
        P.op("dve", lambda e: e.memset(V[:, :, :, 64:65], 1.0), writes=["Vones"])
        for s in range(self.nseq):
            self.x_front(src, s * T, 0, (xt, xb, xT, 5))
            for t in range(T):
                b = t % 2
                tt = s * T + t
                if t + 1 < T:
                    self.x_front(src, tt + 1, (t + 1) % 2, (xt, xb, xT, 5))
                P.op("pe", multi([lambda e, c=c: e.matmul(self.bank(0)[:, 0:416], xT[b][:, c, :], wt[:, c, :], start=(c == 0), stop=(c == 7)) for c in range(8)]),
                     reads=["xT%d" % b, "wB"], writes=["pb0"])
                hB = self.bank(0)
                P.op("act", lambda e: e.activation(junk[:, 0:256], hB[:, 0:256], AF.Square, accum_out=ssq[:, 0:1]), reads=["pb0"], writes=["ssq", "junk"])
                P.op("act", lambda e: e.activation(junk[:, 256:384], hB[:, 256:384], AF.Square, accum_out=ssq[:, 1:2]), reads=["pb0"], writes=["ssq", "junk"], partial=True)
                self.rstd(ssq[:, 0:1], ssq[:, 0:1], 1.0 / 256, 1e-6, ["ssq", "eps1e-06"], ["ssq"])
                self.rstd(ssq[:, 1:2], ssq[:, 1:2], 1.0 / 128, 1e-6, ["ssq", "eps1e-06"], ["ssq"])
                P.op("dve", lambda e: e.scalar_tensor_tensor(cn[:, 0:256], hB[:, 0:256], ssq[:, 0:1], qg[:], ALU.mult, ALU.mult), reads=["pb0", "ssq", "qg"], writes=["cn"])
                P.op("dve", lambda e: e.scalar_tensor_tensor(cn[:, 256:384], hB[:, 256:384], ssq[:, 1:2], kvg[:], ALU.mult, ALU.mult), reads=["pb0", "ssq", "kvg"], writes=["cn"], partial=True)
                P.op("act", lambda e: e.activation(krr[:, 0, :], hB[:, 384:416], AF.Copy), reads=["pb0"], writes=["krr"])
                self.rope(hB[:, 384:416].unsqueeze(1), krr[:], self.ropeB[:, t, :], 0, 16, 1, ["pb0", "ropeB"], ["krr"], "rt")
                P.op("pool", lambda e: e.tensor_copy(kcat[:, :, 64:96], krr[:].to_broadcast([128, 8, 32])), reads=["krr"], writes=["kcat"])
                pTc = self.bankbf(6).rearrange("p (c t) -> p c t", t=128)
                P.op("pe", multi([lambda e, c=c: e.transpose(pTc[:, c, :], cn[:, c * 128:(c + 1) * 128], self.ident[:]) for c in range(3)]), reads=["cn", "ident"], writes=["pb6"])
                P.op("dve", lambda e: e.tensor_copy(cT[:], pTc[:, 0:3, :]), reads=["pb6"], writes=["cT"])
                fl = [lambda e, c=c, c0=c0, n=n: e.matmul(self.PS[:, 512 + c0:512 + c0 + n], cT[:, c, :], wuq[:, c, c0:c0 + n], start=(c == 0), stop=(c == 1))
                      for (c0, n) in ((0, 512), (512, 256)) for c in range(2)]
                fl += [lambda e, n=n: e.matmul(self.bank(3 + n), cT[:, 2, :], wukv[:, 0, n * 512:(n + 1) * 512], start=True, stop=True) for n in range(2)]
                P.op("pe", multi(fl), reads=["cT", "wuq", "wukv"], writes=["pb1", "pb2", "pb3", "pb4"])
                q3 = self.PS[:, 512:1280].rearrange("p (h d) -> p h d", d=96)
                kv3 = self.PS[:, 1536:2560].rearrange("p (h d) -> p h d", d=128)
                P.op("act", lambda e: e.activation(qb[:], q3, AF.Copy), reads=["pb1", "pb2"], writes=["qb"])
                self.rope(q3, qb[:], self.ropeB[:, t, :], 64, 16, 8, ["pb1", "pb2", "ropeB"], ["qb"], "rt")
                P.op("act", lambda e: e.activation(kcat[:, :, 0:64], kv3[:, :, 0:64], AF.Copy), reads=["pb3", "pb4"], writes=["kcat"], partial=True)
                P.op("dve", lambda e, t=t: e.tensor_copy(V[:, t, :, 0:64], kv3[:, :, 64:128]), reads=["pb3", "pb4", "Vones"], writes=["V"], partial=True)
                pTq = self.bankbf(7).rearrange("p (c t) -> p c t", t=128)
                pTk = self.bankbf(6).rearrange("p (c t) -> p c t", t=128)
                P.op("pe", multi([lambda e, h=h: e.transpose(pTq[0:96, h, :], qb[:, h, :], self.ident[:]) for h in range(8)]), reads=["qb", "ident"], writes=["pb7"])
                P.op("dve", lambda e, t=t: e.tensor_copy(QT[0:96, :, t * 128:(t + 1) * 128], pTq[0:96, :, :]), reads=["pb7"], writes=["QT"], partial=True)
                P.op("pe", multi([lambda e, h=h: e.transpose(pTk[0:96, h, :], kcat[:, h, :], self.ident[:]) for h in range(8)]), reads=["kcat", "ident"], writes=["pb6"])
                P.op("act", lambda e, t=t: e.activation(KT[0:96, :, t * 128:(t + 1) * 128], pTk[0:96, :, :], AF.Copy), reads=["pb6"], writes=["KT"], partial=True)

            def epi(qc, mi, ps_all, s=s):
                h = mi
                ab = qc % 2
                P.op("dve", lambda e: e.reciprocal(rec[:], ps_all[:, :, 64]), reads=["pb2", "pb3", "pb4", "pb5"], writes=["rec"])
                P.op("dve", lambda e: e.tensor_tensor(AO[ab][:, :, h * 64:(h + 1) * 64], ps_all[:, :, 0:64], rec[:].unsqueeze(2).to_broadcast([128, 4, 64]), ALU.mult),
                     reads=["pb2", "pb3", "pb4", "pb5", "rec"], writes=["AO%d" % ab], partial=True)
                if h == 7:
                    r0 = s * S + qc * 512
                    P.dma("pool", lambda e: e.dma_start(out=self.ao[r0:r0 + 512, 512:1024].rearrange("(q p) n -> p q n", p=128), in_=AO[ab][:]),
                          "ao%d" % ab, reads=["AO%d" % ab], writes=["ao_d"], partial=True)
            maps = [(h, 0, h) for h in range(8)]
            self.dense_attn(maps, QT, KT, V, 96, 64, 96 ** -0.5, epi)
        P.end()

    def odd_attn(self, l, src):
        P = self.P
        j = l // 2
        W = self.w
        pl = nbr_patterns()
        for hh in range(2):
            P.begin()
            self.mk_eps()
            wt = P.sbuf("wC", [128, 8, 1536], BF16)
            for i in range(3):
                self.load_w_bf16(wt, W["odd_w_qkv"][j][:, i * 1024 + hh * 512:i * 1024 + hh * 512 + 512], 8, 512, "wC", c0=i * 512)
            xt = [P.sbuf("xt", [128, D], F32) for _ in range(2)]
            xb = [P.sbuf("xb", [128, D], BF16) for _ in range(2)]
            xT = [P.sbuf("xT", [128, 8, 128], BF16) for _ in range(2)]
            QT = P.sbuf("QT", [128, 4, S], BF16)
            KT = P.sbuf("KT", [128, 4, S], BF16)
            V = P.sbuf("V", [128, 16, 8, 65], BF16)
            qkb = [P.sbuf("qkb", [128, 1024], BF16) for _ in range(2)]
            bias = [P.sbuf("bias", [128, NPAT, 128], F32) for _ in range(2)]
            tmp = [P.sbuf("tmp", [128, 5, 128], F32) for _ in range(3)]
            E = [P.sbuf("E", [128, 5, 128], BF16) for _ in range(3)]
            rec = P.sbuf("rec", [128, 4], F32)
            AO = P.sbuf("AO", [128, 16, 512], BF16)
            P.op("dve", lambda e: e.memset(V[:, :, :, 64:65], 1.0), writes=["Vones"])
            iters = [(h, qt) for h in range(8) for qt in range(16)]
            for s in range(self.nseq):
                self.qkv_seq(src, s, wt, "wC", QT, KT, V, 64, False, (xt, xb, xT, qkb, None))

                def front(it):
                    h, qt = iters[it]
                    bb = h % 2
                    if qt == 0:
                        hg = hh * 8 + h
                        P.dma("sp", lambda e: e.dma_start(out=bias[bb][:], in_=W["nbias"][j, hg]), "bias%d" % bb, writes=["bias%d" % bb])
                    blk, r0 = h // 2, (h % 2) * 64
                    lst = pl[qt]
                    n = len(lst)
                    rb = it % 3
                    sbk = 2 + 2 * rb
                    ps_s = self.PS[:, sbk * 512:sbk * 512 + 640].rearrange("p (i q) -> p i q", q=128)
                    P.op("pe", multi([lambda e, i=i, kt=kt: e.matmul(ps_s[:, i, :], KT[r0:r0 + 64, blk, kt * 128:(kt + 1) * 128], QT[r0:r0 + 64, blk, qt * 128:(qt + 1) * 128], start=True, stop=True)
                                      for i, (kt, pat) in enumerate(lst)]),
                         reads=["QT", "KT"], writes=["pb%d" % sbk, "pb%d" % (sbk + 1)])
                    p0 = lst[0][1]
                    P.op("dve", lambda e: e.scalar_tensor_tensor(tmp[rb][:, 0:n, :], ps_s[:, 0:n, :], 0.125, bias[bb][:, p0:p0 + n, :], ALU.mult, ALU.add),
                         reads=["pb%d" % sbk, "pb%d" % (sbk + 1), "bias%d" % bb], writes=["tmp%d" % rb])
                    P.op("act", lambda e: e.activation(E[rb][:, 0:n, :], tmp[rb][:, 0:n, :], AF.Exp), reads=["tmp%d" % rb], writes=["E%d" % rb])

                def back(it):
                    h, qt = iters[it]
                    lst = pl[qt]
                    n = len(lst)
                    rb = it % 3
                    ob = (qt // 4) % 2
                    col = (qt % 4) * 128
                    P.op("pe", multi([lambda e, i=i, kt=kt: e.matmul(self.bank(ob)[:, col:col + 65], E[rb][:, i, :], V[:, kt, h, :], start=(i == 0), stop=(i == n - 1))
                                      for i, (kt, pat) in enumerate(lst)]),
                         reads=["E%d" % rb, "V"], writes=["pb%d" % ob], partial=(qt % 4 != 0))
                    if qt % 4 == 3:
                        ps4 = self.bank(ob).rearrange("p (q c) -> p q c", c=128)
                        q0 = qt - 3
                        P.op("dve", lambda e: e.reciprocal(rec[:], ps4[:, :, 64]), reads=["pb%d" % ob], writes=["rec"])
                        P.op("dve", lambda e: e.tensor_tensor(AO[:, q0:q0 + 4, h * 64:(h + 1) * 64], ps4[:, :, 0:64], rec[:].unsqueeze(2).to_broadcast([128, 4, 64]), ALU.mult),
                             reads=["pb%d" % ob, "rec"], writes=["AO"], partial=True)
                nit = len(iters)
                LOOK = 2
                for jx in range(nit + LOOK):
                    if jx < nit:
                        front(jx)
                    if jx >= LOOK:
                        back(jx - LOOK)
                r0_ = s * S
                P.dma("pool", lambda e: e.dma_start(out=self.ao[r0_:r0_ + S, hh * 512:(hh + 1) * 512].rearrange("(q p) n -> p q n", p=128), in_=AO[:]),
                      "aoC", reads=["AO"], writes=["ao_d"])
            P.end()

    def oproj(self, l, src):
        P = self.P
        j = l // 2
        W = self.w
        C = self.C
        P.begin()
        self.mk_eps()
        self.mk_lnt()
        wo = P.sbuf("wo", [128, 8, D], BF16)
        self.load_w_bf16(wo, (W["even_w_o"] if l % 2 == 0 else W["odd_w_o"])[j], 8, D, "wo")
        G = self.bcast_load("G", W["ln1_g"][l], D, "lnG")
        B = self.bcast_load("B", W["ln1_b"][l], D, "lnB")
        wr = P.sbuf("wr", [128, 8, 36], F32)
        P.dma("sp", lambda e: e.dma_start(out=wr[:], in_=W["w_router"][l].rearrange("(c p) n -> p c n", p=128)), "bc_wr", writes=["wr"])
        br = self.bcast_load("br", W["b_router"][l], 36, "br")

        def two(name, shape, dt):
            return [P.sbuf(name, shape, dt) for _ in range(2)]
        T_ = dict(aot=two("aot", [128, D], BF16), aoT=two("aoT", [128, 8, 128], BF16), xt=two("xt", [128, D], F32), r=two("r", [128, D], F32),
                  x1=[P.sbuf("x1", [128, D], F32) for _ in range(4)], x1b=[P.sbuf("x1b", [128, D], BF16) for _ in range(4)], x1T=two("x1T", [128, 8, 128], F32),
                  lg=two("lg", [128, 36], F32), sm=two("sm", [128, 16], F32), pen=two("pen", [128, 4], F32), me=two("me", [128, 32], F32),
                  top8=two("top8", [128, 8], F32), idx8=two("idx8", [128, 8], U32), ef=two("ef", [128, 2], F32), oh=two("oh", [128, 2, 32], F32),
                  Mb=two("Mb", [128, 32], BF16), pos=two("pos", [128, 32], F32), prod=two("prod", [128, 2, 32], F32), slotf=two("slotf", [128, 2], F32),
                  posf=two("posf", [128, 2], F32), ov=two("ov", [128, 2], F32), sidxf=two("sidxf", [128, 2], F32), gidxf=two("gidxf", [128, 2], F32),
                  sidx=two("sidx", [128, 2], I32))
        cnt = P.sbuf("cnt", [128, 32], F32)
        P.op("dve", lambda e: e.memset(cnt[:], 0.0), writes=["cnt"])
        XS = ["xs%d" % e_ for e_ in range(32)]
        prev2 = []
        for tt in range(0, self.NT, 2):
            p1, p2 = [], []
            for sl in (0, 1):
                r_ = Rec()
                self.P = r_
                self.oproj_tile(l, src, tt + sl, sl, T_, wo, G, B, wr, br, cnt, XS)
                k = r_.l.index(("mark",))
                a_, b_ = Rec(), Rec()
                a_.l, b_.l = r_.l[:k], r_.l[k + 1:]
                p1.append(a_)
                p2.append(b_)
            self.P = P
            interleave(P, p1 + prev2)
            prev2 = p2
        interleave(P, prev2)
        P.end()

    def oproj_tile(self, l, src, tt, b, T_, wo, G, B, wr, br, cnt, XS):
        P = self.P
        C = self.C
        b4 = ((tt // 2) % 2) * 2 + b
        aot, aoT, xt, r, x1, x1b, x1T = T_["aot"][b], T_["aoT"][b], T_["xt"][b], T_["r"][b], T_["x1"][b4], T_["x1b"][b4], T_["x1T"][b]
        lg, sm, pen, me, top8, idx8, ef, oh, Mb, pos, prod = (T_[k][b] for k in ("lg", "sm", "pen", "me", "top8", "idx8", "ef", "oh", "Mb", "pos", "prod"))
        slotf, posf, ov, sidxf, gidxf, sidx = (T_[k][b] for k in ("slotf", "posf", "ov", "sidxf", "gidxf", "sidx"))

        def K(n):
            if n in ("x1", "x1b", "x1o"):
                return "%s_%d" % (n, b4)
            return "%s_%d" % (n, b)
        base = 4 * b
        pb = ["pb%d" % (base + i) for i in range(4)]
        rows = slice(tt * 128, (tt + 1) * 128)
        P.dma("sp", lambda e: e.dma_start(out=aot[:], in_=self.ao[rows, :]), K("aot"), writes=[K("aot")])
        P.dma("sp", lambda e: e.dma_start(out=xt[:], in_=src[rows, :]), K("xt"), writes=[K("xt")])
        pT = self.bankbf(base + 0).rearrange("p (c t) -> p c t", t=128)
        P.op("pe", multi([lambda e, c=c: e.transpose(pT[:, c, :], aot[:, c * 128:(c + 1) * 128], self.ident[:]) for c in range(8)]), reads=[K("aot"), "ident"], writes=[pb[0]])
        P.op("dve", lambda e: e.tensor_copy(aoT[:], pT), reads=[pb[0]], writes=[K("aoT")])
        P.op("pe", multi([lambda e, n=n, c=c: e.matmul(self.bank(base + n), aoT[:, c, :], wo[:, c, n * 512:(n + 1) * 512], start=(c == 0), stop=(c == 7)) for n in range(2) for c in range(8)]),
             reads=[K("aoT"), "wo"], writes=[pb[0], pb[1]])
        P.op("dve", lambda e: e.scalar_tensor_tensor(r[:], xt[:], ALPHA, self.bank(base, 2), ALU.mult, ALU.add), reads=[K("xt"), pb[0], pb[1]], writes=[K("r")])
        self.layer_norm(r[:], x1[:], G, B, [K("r")], [K("x1")], sl=b)
        P.op("act", lambda e: e.activation(x1b[:], x1[:], AF.Copy), reads=[K("x1")], writes=[K("x1b")])
        P.dma("act", lambda e: e.dma_start(out=self.res1[rows, :], in_=x1[:]), K("x1o"), reads=[K("x1")], writes=["res1_d"], partial=True)
        P.l.append(("mark",))
        pTf = self.PS[:, (base + 2) * 512:(base + 4) * 512].rearrange("p (c t) -> p c t", t=128)
        P.op("pe", multi([lambda e, c=c: e.transpose(pTf[:, c, :], x1[:, c * 128:(c + 1) * 128], self.identf[:]) for c in range(8)]), reads=[K("x1"), "identf"], writes=[pb[2], pb[3]])
        P.op("act", lambda e: e.activation(x1T[:], pTf, AF.Copy), reads=[pb[2], pb[3]], writes=[K("x1T")])
        pl_ = self.bank(base + 2)[:, 0:36]
        P.op("pe", multi([lambda e, c=c: e.matmul(pl_, x1T[:, c, :], wr[:, c, :], start=(c == 0), stop=(c == 7)) for c in range(8)]), reads=[K("x1T"), "wr"], writes=[pb[2]])
        P.op("dve", lambda e: e.tensor_tensor(lg[:], pl_, br[:], ALU.add), reads=[pb[2], "br"], writes=[K("lg")])
        P.op("dve", lambda e: e.tensor_reduce(sm[:, 0:1], lg[:, 0:4], AX.X, ALU.max), reads=[K("lg")], writes=[K("sm0")])
        P.op("dve", lambda e: e.tensor_scalar(pen[:], lg[:, 0:4], sm[:, 0:1], None, ALU.subtract), reads=[K("lg"), K("sm0")], writes=[K("pen")])
        P.op("act", lambda e: e.activation(sm[:, 4:8], pen[:], AF.Exp, accum_out=sm[:, 1:2]), reads=[K("pen")], writes=[K("sm1")])
        P.op("dve", lambda e: e.reciprocal(sm[:, 2:3], sm[:, 1:2]), reads=[K("sm1")], writes=[K("sm2")])
        P.op("dve", lambda e: e.tensor_scalar(pen[:], pen[:], 0.0, 10000.0, ALU.is_ge, ALU.mult), reads=[K("pen")], writes=[K("pen")])
        P.op("dve", lambda e: e.tensor_scalar(pen[:], pen[:], -10000.0, None, ALU.add), reads=[K("pen")], writes=[K("pen")])
        P.op("dve", lambda e: e.tensor_tensor(me[:].rearrange("p (g k) -> p g k", k=8), lg[:, 4:36].rearrange("p (g k) -> p g k", k=8),
                                              pen[:].unsqueeze(2).to_broadcast([128, 4, 8]), ALU.add), reads=[K("lg"), K("pen")], writes=[K("me")])
        P.op("dve", lambda e: e.max(top8[:], me[:]), reads=[K("me")], writes=[K("top8")])
        P.op("dve", lambda e: e.max_index(idx8[:], top8[:], me[:]), reads=[K("me"), K("top8")], writes=[K("idx8")])
        P.op("dve", lambda e: e.tensor_tensor(sm[:, 8:9], top8[:, 1:2], top8[:, 0:1], ALU.subtract), reads=[K("top8")], writes=[K("sm8")])
        P.op("act", lambda e: e.activation(sm[:, 9:10], sm[:, 8:9], AF.Exp), reads=[K("sm8")], writes=[K("sm9")])
        P.op("dve", lambda e: e.tensor_scalar(sm[:, 10:11], sm[:, 9:10], 1.0, None, ALU.add), reads=[K("sm9")], writes=[K("sm10")])
        P.op("dve", lambda e: e.reciprocal(sm[:, 10:11], sm[:, 10:11]), reads=[K("sm10")], writes=[K("sm10")])
        P.op("dve", lambda e: e.tensor_tensor(sm[:, 12:13], sm[:, 10:11], sm[:, 2:3], ALU.mult), reads=[K("sm10"), K("sm2")], writes=[K("sm12")])
        P.op("dve", lambda e: e.tensor_tensor(sm[:, 13:14], sm[:, 12:13], sm[:, 9:10], ALU.mult), reads=[K("sm12"), K("sm9")], writes=[K("sm13")])
        P.op("dve", lambda e: e.tensor_copy(ef[:], idx8[:, 0:2]), reads=[K("idx8")], writes=[K("ef")])
        for k in range(2):
            P.op("dve", lambda e, k=k: e.tensor_scalar(oh[:, k, :], self.iota32[:], ef[:, k:k + 1], None, ALU.is_equal), reads=[K("ef"), "iota32"], writes=[K("oh%d" % k)])
        P.op("dve", lambda e: e.tensor_tensor(Mb[:], oh[:, 0, :], oh[:, 1, :], ALU.add), reads=[K("oh0"), K("oh1")], writes=[K("Mb")])
        pr = self.bank(base + 3)[:, 0:32]
        pr2 = self.bank(base + 3)[:, 64:96]
        P.op("pe", multi([lambda e: e.matmul(pr, self.U[:], Mb[:], start=True, stop=True), lambda e: e.matmul(pr2, self.ones[:], Mb[:], start=True, stop=True)]),
             reads=["U", "ones", K("Mb")], writes=[pb[3]])
        P.op("dve", lambda e: e.tensor_tensor(pos[:], pr, cnt[:], ALU.add), reads=[pb[3], "cnt"], writes=[K("pos")])
        if b == 1:
            tot0 = self.bank(3)[:, 64:96]
            P.op("dve", lambda e: e.tensor_tensor(pos[:], tot0, pos[:], ALU.add), reads=["pb3", K("pos")], writes=[K("pos")])
            P.op("dve", lambda e: e.tensor_tensor(cnt[:], tot0, cnt[:], ALU.add), reads=["pb3", "cnt"], writes=["cnt"])
            P.op("dve", lambda e: e.tensor_tensor(cnt[:], pr2, cnt[:], ALU.add), reads=[pb[3], "cnt"], writes=["cnt"])
        P.op("dve", lambda e: e.tensor_tensor(prod[:], oh[:], pos[:].unsqueeze(1).to_broadcast([128, 2, 32]), ALU.mult), reads=[K("oh0"), K("oh1"), K("pos")], writes=[K("prod")])
        P.op("dve", lambda e: e.tensor_reduce(posf[:], prod[:], AX.X, ALU.add), reads=[K("prod")], writes=[K("posf")])
        P.op("dve", lambda e: e.tensor_scalar(slotf[:], ef[:], float(C), None, ALU.mult), reads=[K("ef")], writes=[K("slotf")])
        P.op("dve", lambda e: e.tensor_tensor(slotf[:], slotf[:], posf[:], ALU.add), reads=[K("slotf"), K("posf")], writes=[K("slotf")])
        P.op("dve", lambda e: e.tensor_scalar(ov[:], posf[:], float(C), None, ALU.is_ge), reads=[K("posf")], writes=[K("ov")])
        P.op("dve", lambda e: e.scalar_tensor_tensor(sidxf[:], ov[:], 1.0e6, slotf[:], ALU.mult, ALU.add), reads=[K("ov"), K("slotf")], writes=[K("sidxf")])
        P.op("dve", lambda e: e.tensor_copy(sidx[:], sidxf[:]), reads=[K("sidxf")], writes=[K("sidx")])
        P.op("dve", lambda e: e.tensor_scalar(gidxf[:], slotf[:], -1.0, float(self.NSLOT), ALU.mult, ALU.add), reads=[K("slotf")], writes=[K("gidxf")])
        P.op("dve", lambda e: e.tensor_tensor(gidxf[:], gidxf[:], ov[:], ALU.mult), reads=[K("gidxf"), K("ov")], writes=[K("gidxf")])
        P.op("dve", lambda e: e.tensor_tensor(gidxf[:], gidxf[:], slotf[:], ALU.add), reads=[K("gidxf"), K("slotf")], writes=[K("gidxf")])
        P.op("dve", lambda e: e.tensor_copy(self.gidx[:, tt, :], gidxf[:]), reads=[K("gidxf")], writes=["gidx"], partial=True)
        P.op("dve", lambda e: e.tensor_scalar(ov[:], ov[:], -1.0, 1.0, ALU.mult, ALU.add), reads=[K("ov")], writes=[K("ov")])
        P.op("dve", lambda e: e.tensor_tensor(self.gates[:, tt, :], sm[:, 12:14], ov[:], ALU.mult), reads=[K("sm12"), K("sm13"), K("ov")], writes=["gates"], partial=True)
        for k in range(0 if None else 2):
            P.dma("pool", lambda e, k=k: e.indirect_dma_start(out=self.xs[:, :], out_offset=bass.IndirectOffsetOnAxis(ap=sidx[:, k:k + 1], axis=0),
                                                              in_=x1b[:], in_offset=None, bounds_check=self.bc_reg, oob_is_err=False),
                  K("scat"), reads=[K("x1b"), K("sidx")], writes=XS, partial=True, wfull=([] if None else ["xs_chain"]))

    def experts(self, l):
        P = self.P
        W = self.w
        C = self.C
        NS = C // 128
        HW = C // 2
        P.begin()
        wg = [P.sbuf("wg", [128, 8, 512], BF16) for _ in range(2)]
        wu = [P.sbuf("wu", [128, 8, 512], BF16) for _ in range(2)]
        wd = [P.sbuf("wd", [128, 4, D], BF16) for _ in range(2)]
        xst = [P.sbuf("xst", [128, D], BF16) for _ in range(2)]
        xsT = [P.sbuf("xsT", [128, 8, C], BF16) for _ in range(2)]
        hT = [P.sbuf("hT", [128, 4, C], BF16) for _ in range(2)]
        sg = [P.sbuf("sg", [128, HW], F32) for _ in range(2)]
        yt = [P.sbuf("yt", [128, D], F32) for _ in range(2)]
        it = 0
        for ex in range(32):
            eb = ex % 2
            self.load_w_bf16(wg[eb], W["w_gate"][l, ex], 8, 512, "wg%d" % eb)
            self.load_w_bf16(wu[eb], W["w_up"][l, ex], 8, 512, "wu%d" % eb)
            self.load_w_bf16(wd[eb], W["w_down"][l, ex], 4, D, "wd%d" % eb)
            for st in range(NS):
                b = st % 2
                r0 = ex * C + st * 128
                P.dma("sp", lambda e, r0=r0, b=b: e.dma_start(out=xst[b][:], in_=self.xs[r0:r0 + 128, :]), "xst%d" % b, reads=["xs%d" % ex], writes=["xst%d" % b])
                pT = self.bankbf(st % 2).rearrange("p (c t) -> p c t", t=128)
                P.op("pe", multi([lambda e, c=c: e.transpose(pT[:, c, :], xst[b][:, c * 128:(c + 1) * 128], self.ident[:]) for c in range(8)]), reads=["xst%d" % b, "ident"], writes=["pb%d" % (st % 2)])
                if st % 2 == 0:
                    P.op("dve", lambda e, st=st, pT=pT: e.tensor_copy(xsT[eb][:, :, st * 128:(st + 1) * 128], pT), reads=["pb0"], writes=["xsT%d" % eb], partial=True)
                else:
                    P.op("act", lambda e, st=st, pT=pT: e.activation(xsT[eb][:, :, st * 128:(st + 1) * 128], pT, AF.Copy), reads=["pb1"], writes=["xsT%d" % eb], partial=True)
            for f in range(4):
                for hf in range(2):
                    ib = it % 2
                    it += 1
                    pg = self.bank(2 + 2 * ib)[:, 0:HW]
                    pu = self.bank(3 + 2 * ib)[:, 0:HW]
                    P.op("pe", multi([lambda e, c=c: e.matmul(pg, wg[eb][:, c, f * 128:(f + 1) * 128], xsT[eb][:, c, hf * HW:(hf + 1) * HW], start=(c == 0), stop=(c == 7)) for c in range(8)]),
                         reads=["wg%d" % eb, "xsT%d" % eb], writes=["pb%d" % (2 + 2 * ib)])
                    P.op("pe", multi([lambda e, c=c: e.matmul(pu, wu[eb][:, c, f * 128:(f + 1) * 128], xsT[eb][:, c, hf * HW:(hf + 1) * HW], start=(c == 0), stop=(c == 7)) for c in range(8)]),
                         reads=["wu%d" % eb, "xsT%d" % eb], writes=["pb%d" % (3 + 2 * ib)])
                    P.op("act", lambda e, ib=ib, pg=pg: e.activation(sg[ib][:], pg, AF.Silu), reads=["pb%d" % (2 + 2 * ib)], writes=["sg%d" % ib])
                    P.op("dve", lambda e, ib=ib, pu=pu, f=f, hf=hf: e.tensor_tensor(hT[eb][:, f, hf * HW:(hf + 1) * HW], sg[ib][:], pu, ALU.mult),
                         reads=["sg%d" % ib, "pb%d" % (3 + 2 * ib)], writes=["hT%d" % eb], partial=True)
            for st in range(NS):
                b = st % 2
                r0 = ex * C + st * 128
                yb0 = 6 if st % 2 == 0 else 0
                P.op("pe", multi([lambda e, n=n, f=f: e.matmul(self.bank(yb0 + n), hT[eb][:, f, st * 128:(st + 1) * 128], wd[eb][:, f, n * 512:(n + 1) * 512], start=(f == 0), stop=(f == 3)) for n in range(2) for f in range(4)]),
                     reads=["hT%d" % eb, "wd%d" % eb], writes=["pb%d" % yb0, "pb%d" % (yb0 + 1)])
                if st % 2 == 0:
                    P.op("dve", lambda e, b=b: e.tensor_copy(yt[b][:], self.bank(yb0, 2)), reads=["pb%d" % yb0, "pb%d" % (yb0 + 1)], writes=["yt%d" % b])
                else:
                    P.op("act", lambda e, b=b: e.activation(yt[b][:], self.bank(yb0, 2), AF.Copy), reads=["pb%d" % yb0, "pb%d" % (yb0 + 1)], writes=["yt%d" % b])
                P.dma("act", lambda e, r0=r0, b=b: e.dma_start(out=self.ys[r0:r0 + 128, :], in_=yt[b][:]), "yo%d" % b, reads=["yt%d" % b], writes=["ys%d" % ex], partial=True)
        P.end()

    def combine(self, l, dst):
        P = self.P
        W = self.w
        P.begin()
        self.mk_eps()
        self.mk_lnt()
        G = self.bcast_load("G", W["ln2_g"][l], D, "lnG")
        B = self.bcast_load("B", W["ln2_b"][l], D, "lnB")
        x1 = [P.sbuf("x1", [128, D], F32) for _ in range(2)]
        y0 = [P.sbuf("y0", [128, D], F32) for _ in range(2)]
        y1 = [P.sbuf("y1", [128, D], F32) for _ in range(2)]
        f = [P.sbuf("f", [128, D], F32) for _ in range(2)]
        o = [P.sbuf("o", [128, D], F32) for _ in range(2)]
        YS = ["ys%d" % e_ for e_ in range(32)] + ["ysd"]

        def tile(tt, b):
            P = self.P
            rows = slice(tt * 128, (tt + 1) * 128)
            P.dma("sp", lambda e: e.dma_start(out=x1[b][:], in_=self.res1[rows, :]), "x1l%d" % b, reads=["res1_d"], writes=["x1_%d" % b])
            for k, yb in enumerate((y0, y1)):
                P.dma("pool", lambda e, k=k, yb=yb: e.indirect_dma_start(out=yb[b][:], out_offset=None, in_=self.ys[:, :],
                                                                        in_offset=bass.IndirectOffsetOnAxis(ap=self.gidx[:, tt, k:k + 1], axis=0)),
                      "gat%d_%d" % (k, b), reads=YS + ["gidx"], writes=["y%d_%d" % (k, b)])
            P.op("dve", lambda e: e.tensor_scalar(f[b][:], y0[b][:], self.gates[:, tt, 0:1], None, ALU.mult), reads=["y0_%d" % b, "gates"], writes=["f%d" % b])
            P.op("dve", lambda e: e.scalar_tensor_tensor(f[b][:], y1[b][:], self.gates[:, tt, 1:2], f[b][:], ALU.mult, ALU.add), reads=["y1_%d" % b, "gates", "f%d" % b], writes=["f%d" % b])
            P.op("dve", lambda e: e.scalar_tensor_tensor(f[b][:], x1[b][:], ALPHA, f[b][:], ALU.mult, ALU.add), reads=["x1_%d" % b, "f%d" % b], writes=["f%d" % b])
            self.layer_norm(f[b][:], o[b][:], G, B, ["f%d" % b], ["o%d" % b], sl=b)
            P.dma("act", lambda e: e.dma_start(out=dst[rows, :], in_=o[b][:]), "oo%d" % b, reads=["o%d" % b], writes=["dst_d"], partial=True)
        for tt in range(0, self.NT, 2):
            recs = []
            for sl in (0, 1):
                r_ = Rec()
                self.P = r_
                tile(tt + sl, sl)
                recs.append(r_)
            self.P = P
            interleave(P, recs)
        P.end()


def prep_shared(inp):
    f32 = np.float32
    sh = {}
    sh["even_w_in"] = np.ascontiguousarray(inp["even_w_in"], f32)
    sh["even_lam"] = np.ascontiguousarray(np.stack([inp["even_lam_q1"], inp["even_lam_k1"], inp["even_lam_q2"], inp["even_lam_k2"]], axis=1), f32)
    for k in ("even_subln_g", "even_q_norm_g", "even_w_uq", "even_kv_norm_g", "even_w_ukv", "even_w_o", "odd_w_qkv", "odd_w_o",
              "ln1_g", "ln1_b", "ln2_g", "ln2_b", "w_gate", "w_up", "w_down"):
        sh[k] = np.ascontiguousarray(inp[k], f32)
    sh["w_router"] = np.ascontiguousarray(np.concatenate([inp["w_router_group"], inp["w_router_expert"]], axis=2), f32)
    sh["b_router"] = np.ascontiguousarray(np.concatenate([inp["b_router_group"], inp["b_router_expert"]], axis=1), f32)
    idx = nbr_index_table()
    rb = np.asarray(inp["odd_rel_bias"], f32).reshape(2, 16, 15 * 31)
    ext = np.concatenate([rb, np.full((2, 16, 1), NEG, f32)], axis=2)
    sh["nbias"] = np.ascontiguousarray(ext[:, :, idx], f32)
    ra, rb_ = rope_host()
    sh["ropeA"], sh["ropeB"] = ra, rb_
    return sh


def kernel(**inputs):
    from concourse.bass_utils import run_bass_kernel_spmd
    n = 8
    nseq = 4
    inp = {k: np.asarray(v) for k, v in inputs.items()}
    sh = prep_shared(inp)
    x = np.ascontiguousarray(inp["x"], np.float32)
    mk = MK(nseq=nseq, C=768, layers=(0, 1, 2, 3), debug=False)
    in_maps = []
    for c in range(n):
        d = {k: v for k, v in sh.items() if k in mk.w}
        d["x"] = np.ascontiguousarray(x[c * nseq:(c + 1) * nseq].reshape(nseq * S, D))
        in_maps.append(d)
    res = run_bass_kernel_spmd(mk.nc, in_maps, core_ids=list(range(n)))
    out = np.concatenate([np.asarray(r["y"], np.float32).reshape(nseq, S, D) for r in res.results], axis=0)
    return out
```

```python
import numpy as np
import concourse.bass as bass
import concourse.mybir as mybir
from contextlib import ExitStack

F32 = mybir.dt.float32
BF16 = mybir.dt.bfloat16
I32 = mybir.dt.int32
U32 = mybir.dt.uint32
AF = mybir.ActivationFunctionType
ALU = mybir.AluOpType
AX = mybir.AxisListType

SEM_LIMIT = 30000
MAXOPS = 10 ** 9


class Prog:
    def __init__(self, nc):
        self.nc = nc
        self.gstack = ExitStack()
        self.pstack = None
        self.engs = {"pe": nc.tensor, "act": nc.scalar, "dve": nc.vector, "pool": nc.gpsimd, "sp": nc.sync}
        self._nsem = 0
        self.eng_sem, self.eng_cnt, self.grp_sem, self.grp_cnt = {}, {}, {}, {}
        self.waited = {e: {} for e in self.engs}
        self.prev_final = {}
        self.cur_final = {}
        self.need_bar = set()
        self.tot = dict(n_ops=0, n_wait=0)
        self._nm = 0
        self.W, self.R, self.G = {}, {}, {}

    def sbuf(self, name, shape, dt, persist=False):
        self._nm += 1
        st = self.gstack if (persist or self.pstack is None) else self.pstack
        return st.enter_context(self.nc.sbuf_tensor("%s_%d" % (name, self._nm), list(shape), dt))

    def psum(self, name, shape, dt):
        return self.gstack.enter_context(self.nc.psum_tensor(name, list(shape), dt))

    def new_sem(self):
        self._nsem += 1
        return self.gstack.enter_context(self.nc.semaphore("s%d" % self._nsem))

    def begin(self):
        self.pstack = ExitStack()
        self.prev_final = dict(self.cur_final)
        self.need_bar = set(self.engs)
        self.W, self.R, self.G = {}, {}, {}

    def end(self):
        self.pstack.close()
        self.pstack = None

    def _do(self, engn, fn, reads, writes, partial, dma, group, wfull=()):
        if self.tot["n_ops"] >= MAXOPS:
            return
        W, R, G = self.W, self.R, self.G
        deps = []
        for k in reads:
            deps.extend(W.get(k, ()))
            if k.startswith("pb"):
                deps.extend(r for r in R.get(k, ()) if r[2] != engn)
        newgen = []
        for k in wfull:
            d = list(W.get(k, ())) + list(R.get(k, ()))
            deps.extend(d)
            newgen.append((k, d))
        for k in writes:
            if partial and k in W and not R.get(k):
                deps.extend(G[k])
            else:
                d = list(W.get(k, ())) + list(R.get(k, ()))
                deps.extend(d)
                newgen.append((k, d))
        eng = self.engs[engn]
        w = self.waited[engn]
        need = {}
        if engn in self.need_bar:
            self.need_bar.discard(engn)
            for num, (s, v) in self.prev_final.items():
                need[num] = (s, v)
        for (s, v, de, ddma) in deps:
            if engn == "pe" and not dma and de == "pe" and not ddma:
                continue
            if need.get(s.num, (None, 0))[1] < v:
                need[s.num] = (s, v)
        for num, (s, v) in need.items():
            if w.get(num, 0) < v:
                eng.wait_ge(s, v)
                w[num] = v
                self.tot["n_wait"] += 1
        ins = fn(eng)
        if dma:
            g = group
            if g not in self.grp_sem or self.grp_cnt[g] + 16 > SEM_LIMIT:
                self.grp_sem[g] = self.new_sem()
                self.grp_cnt[g] = 0
            self.grp_cnt[g] += 16
            sem, val = self.grp_sem[g], self.grp_cnt[g]
            ins.then_inc(sem, 16)
        else:
            e = engn
            if e not in self.eng_sem or self.eng_cnt[e] + 1 > SEM_LIMIT:
                self.eng_sem[e] = self.new_sem()
                self.eng_cnt[e] = 0
            self.eng_cnt[e] += 1
            sem, val = self.eng_sem[e], self.eng_cnt[e]
            ins.then_inc(sem, 1)
        self.cur_final[sem.num] = (sem, val)
        rec = (sem, val, engn, dma)
        for k, d in newgen:
            G[k] = d
            W[k] = []
            R[k] = []
        for k in tuple(writes) + tuple(wfull):
            W[k].append(rec)
        for k in reads:
            R.setdefault(k, []).append(rec)
        self.tot["n_ops"] += 1

    def op(self, eng, fn, reads=(), writes=(), partial=False):
        self._do(eng, fn, reads, writes, partial, False, None)

    def dma(self, eng, fn, group, reads=(), writes=(), partial=False, wfull=()):
        self._do(eng, fn, reads, writes, partial, True, group, wfull)

    def finish(self):
        sp = self.engs["sp"]
        w = self.waited["sp"]
        for num, (s, v) in self.cur_final.items():
            if w.get(num, 0) < v:
                sp.wait_ge(s, v)
                w[num] = v
        self.tot["n_sem"] = self._nsem
        return self.tot


class Rec:
    def __init__(self):
        self.l = []

    def op(self, *a, **k):
        self.l.append(("op", a, k))

    def dma(self, *a, **k):
        self.l.append(("dma", a, k))


def interleave(P, recs):
    from itertools import zip_longest
    for group in zip_longest(*[r.l for r in recs]):
        for item in group:
            if item is not None and item[0] != "mark":
                getattr(P, item[0])(*item[1], **item[2])


import math
import os
DVX = 0
import numpy as np

D = 1024
S = 2048
T = 16
DEPTH = 4
ALPHA = (2 * DEPTH) ** 0.25
NEG = -30000.0
NPAT = 21


def nbr_patterns():
    pats = {}
    p = 5
    out = []
    for qt in range(16):
        if 2 <= qt <= 13:
            out.append([(qt - 2 + i, i) for i in range(5)])
        else:
            kts = [0, 1, 2, 3] if qt < 2 else [12, 13, 14, 15]
            lst = []
            for kt in kts:
                lst.append((kt, p))
                p += 1
            out.append(lst)
    assert p == NPAT
    return out


def nbr_index_table():
    pl = nbr_patterns()
    idx = np.full((128, NPAT, 128), 15 * 31, np.int64)
    done = set()
    for qt in range(16):
        for kt, p in pl[qt]:
            if p in done:
                continue
            done.add(p)
            for q in range(128):
                r, c = qt * 2 + q // 64, q % 64
                r0 = min(max(r - 4, 0), 24)
                c0 = min(max(c - 8, 0), 48)
                for k in range(128):
                    rk, ck = kt * 2 + k // 64, k % 64
                    if r0 <= rk < r0 + 8 and c0 <= ck < c0 + 16:
                        idx[k, p, q] = (rk - r + 7) * 31 + (ck - c + 15)
    return idx


def rope_host():
    def tab(rot):
        inv = (np.float32(500000.0) ** (-np.arange(0, rot, 2, dtype=np.float32) / np.float32(rot))).astype(np.float32)
        ang = (np.arange(S, dtype=np.float32)[:, None] * inv[None, :]).astype(np.float32)
        cos, sin = np.cos(ang).astype(np.float32), np.sin(ang).astype(np.float32)
        t = np.concatenate([cos, cos, -sin, sin], axis=1)
        return np.ascontiguousarray(t.reshape(T, 128, 2 * rot).transpose(1, 0, 2))
    return tab(16), tab(32)


def multi(fns):
    def f(e):
        ins = None
        for g in fns:
            ins = g(e)
        return ins
    return f


class MK:
    def __init__(self, nseq=4, C=768, layers=(0, 1, 2, 3), debug=False, phases="ABCOEM"):
        self.nseq, self.C, self.layers, self.debug = nseq, C, tuple(layers), debug
        self.phases = phases
        self.NT = nseq * T
        self.NTOK = nseq * S
        self.NSLOT = 32 * C
        nc = self.nc = bass.Bass("TRN2", target_bir_lowering=False)
        P = self.P = Prog(nc)
        NTOK = self.NTOK

        def din(name, shape, dt=F32):
            return nc.dram_tensor(name, list(shape), dt, kind="ExternalInput").ap()

        def dint(name, shape, dt=F32):
            return nc.dram_tensor(name, list(shape), dt, kind="Internal").ap()
        self.x = din("x", [NTOK, D])
        self.w = {}
        for nm, shp in [("even_w_in", [2, D, 1952]), ("even_lam", [2, 4, 64]), ("even_subln_g", [2, 128]),
                        ("even_q_norm_g", [2, 256]), ("even_w_uq", [2, 256, 768]), ("even_kv_norm_g", [2, 128]),
                        ("even_w_ukv", [2, 128, 1024]), ("even_w_o", [2, D, D]), ("odd_w_qkv", [2, D, 3072]),
                        ("nbias", [2, 16, 128, NPAT, 128]), ("odd_w_o", [2, D, D]),
                        ("ln1_g", [4, D]), ("ln1_b", [4, D]), ("ln2_g", [4, D]), ("ln2_b", [4, D]),
                        ("w_router", [4, D, 36]), ("b_router", [4, 36]),
                        ("w_gate", [4, 32, D, 512]), ("w_up", [4, 32, D, 512]), ("w_down", [4, 32, 512, D]),
                        ("ropeA", [128, 16, 32]), ("ropeB", [128, 16, 64])]:
            if nm in ("w_gate", "w_up", "w_down") and "E" not in self.phases:
                continue
            if nm in ("odd_w_qkv", "nbias") and ("C" not in self.phases or all(l % 2 == 0 for l in self.layers)):
                continue
            self.w[nm] = din(nm, shp)
        self.y = nc.dram_tensor("y", [NTOK, D], F32, kind="ExternalOutput").ap()
        self.res1 = dint("res1", [NTOK, D]) if not debug else nc.dram_tensor("res1", [NTOK, D], F32, kind="ExternalOutput").ap()
        self.resA = dint("resA", [NTOK, D])
        self.ao = dint("ao", [NTOK, D], BF16) if not debug else nc.dram_tensor("ao", [NTOK, D], BF16, kind="ExternalOutput").ap()
        self.xs = dint("xs", [self.NSLOT, D], BF16)
        self.ys = dint("ys", [self.NSLOT + 128, D])
        self.PS = P.psum("PS", [128, 4096], F32)
        self.bc_reg = nc.gpsimd.alloc_register("bcr")
        nc.gpsimd.reg_mov(self.bc_reg, self.NSLOT - 1)
        self.consts()
        for l in self.layers:
            src = self.x if l == self.layers[0] else self.resA
            if l % 2 == 0:
                self.even_attn(l, src)
            elif "C" in self.phases:
                self.odd_attn(l, src)
            if "O" in self.phases:
                self.oproj(l, src)
            if "E" in self.phases:
                self.experts(l)
            dst = self.y if l == self.layers[-1] else self.resA
            if "M" in self.phases:
                self.combine(l, dst)
        self.stats = P.finish()

    def bank(self, b, n=1):
        return self.PS[:, b * 512:(b + n) * 512]

    def bankbf(self, b):
        return self.PS[:, b * 512:(b + 1) * 512].bitcast(BF16)

    def consts(self):
        P = self.P
        P.begin()
        self.ident = P.sbuf("ident", [128, 128], BF16, persist=True)
        self.identf = P.sbuf("identf", [128, 128], F32, persist=True)
        self.U = P.sbuf("U", [128, 128], BF16, persist=True)
        self.ones = P.sbuf("ones", [128, 128], BF16, persist=True)
        self.iota32 = P.sbuf("iota32", [128, 32], F32, persist=True)
        self.eC = P.sbuf("eC", [128, 32], F32, persist=True)
        self.ropeA = P.sbuf("ropeA", [128, 16, 32], F32, persist=True)
        self.ropeB = P.sbuf("ropeB", [128, 16, 64], F32, persist=True)
        self.gidx = P.sbuf("gidx", [128, self.NT, 2], I32, persist=True)
        self.gates = P.sbuf("gates", [128, self.NT, 2], F32, persist=True)
        self.zero = P.sbuf("zero", [128, D], F32, persist=True)
        iot = P.sbuf("iot", [128, 128], F32)
        iop = P.sbuf("iop", [128, 1], F32)
        P.op("pool", lambda e: e.iota(iot[:], [[1, 128]], base=0, channel_multiplier=0, allow_small_or_imprecise_dtypes=True), writes=["iot"])
        P.op("pool", lambda e: e.iota(iop[:], [[0, 1]], base=0, channel_multiplier=1, allow_small_or_imprecise_dtypes=True), writes=["iop"])
        P.op("dve", lambda e: e.tensor_scalar(self.ident[:], iot[:], iop[:, 0:1], None, ALU.is_equal), reads=["iot", "iop"], writes=["ident"])
        P.op("dve", lambda e: e.tensor_scalar(self.identf[:], iot[:], iop[:, 0:1], None, ALU.is_equal), reads=["iot", "iop"], writes=["identf"])
        P.op("dve", lambda e: e.tensor_scalar(self.U[:], iot[:], iop[:, 0:1], None, ALU.is_gt), reads=["iot", "iop"], writes=["U"])
        P.op("dve", lambda e: e.memset(self.ones[:], 1.0), writes=["ones"])
        P.op("dve", lambda e: e.tensor_copy(self.iota32[:], iot[:, 0:32]), reads=["iot"], writes=["iota32"])
        P.op("dve", lambda e: e.tensor_scalar(self.eC[:], iot[:, 0:32], float(self.C), None, ALU.mult), reads=["iot"], writes=["eC"])
        P.op("dve", lambda e: e.memset(self.zero[:], 0.0), writes=["zero"])
        P.dma("sp", lambda e: e.dma_start(out=self.ropeA[:], in_=self.w["ropeA"]), "c0", writes=["ropeA"])
        P.dma("sp", lambda e: e.dma_start(out=self.ropeB[:], in_=self.w["ropeB"]), "c0", writes=["ropeB"])
        P.dma("sp", lambda e: e.dma_start(out=self.ys[self.NSLOT:self.NSLOT + 128, :], in_=self.zero[:]), "c0", reads=["zero"], writes=["ysd"])
        zb = P.sbuf("zb", [128, 8, D], BF16)
        P.op("pool", lambda e: e.memset(zb[:], 0.0), writes=["zb"])
        for i in range(self.NSLOT // 1024):
            P.dma("sp", lambda e, i=i: e.dma_start(out=self.xs[i * 1024:(i + 1) * 1024, :].rearrange("(p n) d -> p n d", n=8), in_=zb[:]), "c0", reads=["zb"], writes=["xsz"], partial=True)
        P.end()

    def rstd(self, out, in_, scale, eps, rk, wk):
        P = self.P
        P.op("act", lambda e: e.activation(out, in_, AF.Ln, bias=self.epst(eps), scale=scale), reads=rk, writes=wk)
        P.op("act", lambda e: e.activation(out, out, AF.Exp, scale=-0.5), reads=wk, writes=wk)

    def epst(self, eps):
        return self._eps[eps][:, 0:1]

    def mk_eps(self):
        P = self.P
        self._eps = {}
        for eps in (1e-5, 1e-6):
            t = P.sbuf("eps", [128, 1], F32)
            self._eps[eps] = t
            P.op("dve", lambda e, t=t, eps=eps: e.memset(t[:], eps), writes=["eps%g" % eps])

    def bcast_load(self, name, src_row, n, key, dt=F32):
        P = self.P
        t = P.sbuf(name, [128, n], dt)
        P.dma("sp", lambda e: e.dma_start(out=t[:], in_=src_row.unsqueeze(0).broadcast_to([128, n])), "bc_" + key, writes=[key])
        return t

    def load_w_bf16(self, dst, src2d, kchunks, ncols, key, c0=0):
        P = self.P
        for k in range(kchunks):
            P.dma("pool", lambda e, k=k: e.dma_start(out=dst[:, k, c0:c0 + ncols], in_=src2d[k * 128:(k + 1) * 128, :]),
                  "w_" + key, writes=[key], partial=True)

    def x_front(self, src, tt, b, bufs):
        P = self.P
        xt, xb, xT, pbank = bufs
        P.dma("sp", lambda e: e.dma_start(out=xt[b][:], in_=src[tt * 128:(tt + 1) * 128, :]), "xt%d" % b, writes=["xt%d" % b])
        P.op("act", lambda e: e.activation(xb[b][:], xt[b][:], AF.Copy), reads=["xt%d" % b], writes=["xb%d" % b])
        pT = self.bankbf(pbank).rearrange("p (c t) -> p c t", t=128)
        P.op("pe", multi([lambda e, c=c: e.transpose(pT[:, c, :], xb[b][:, c * 128:(c + 1) * 128], self.ident[:]) for c in range(8)]),
             reads=["xb%d" % b, "ident"], writes=["pb%d" % pbank])
        P.op("dve", lambda e: e.tensor_copy(xT[b][:], pT), reads=["pb%d" % pbank], writes=["xT%d" % b])

    def rope(self, src3, dst3, tab, r0, half, ng, rk, wk, tmpk, temps=None):
        P = self.P
        t1, t2 = temps if temps is not None else self._ropet
        h2 = 2 * half
        a = t1[:, 0:ng, 0:h2]
        b_ = t2[:, 0:ng, 0:h2]
        cc = tab[:, 0:h2].unsqueeze(1).to_broadcast([128, ng, h2])
        ns = tab[:, h2:h2 + half].unsqueeze(1).to_broadcast([128, ng, half])
        ps = tab[:, h2 + half:h2 + 2 * half].unsqueeze(1).to_broadcast([128, ng, half])
        P.op("dve", lambda e: e.tensor_tensor(a, src3[:, :, r0:r0 + h2], cc, ALU.mult), reads=rk, writes=[tmpk + "1"])
        P.op("dve", lambda e: e.tensor_tensor(b_[:, :, 0:half], src3[:, :, r0 + half:r0 + h2], ns, ALU.mult), reads=rk, writes=[tmpk + "2"])
        P.op("dve", lambda e: e.tensor_tensor(b_[:, :, half:h2], src3[:, :, r0:r0 + half], ps, ALU.mult), reads=rk, writes=[tmpk + "2"], partial=True)
        P.op("pool", lambda e: e.tensor_tensor(dst3[:, :, r0:r0 + h2], a, b_, ALU.add), reads=[tmpk + "1", tmpk + "2"], writes=wk)

    def layer_norm(self, r, out, G, B, rk, wk, sl=0):
        P = self.P
        st, mv, rs = self._lnt[sl]
        kst, kmv, krs = "lnst%d" % sl, "lnmv%d" % sl, "lnrs%d" % sl
        P.op("dve", lambda e: e.bn_stats(st[:, 0, :], r[:, 0:512]), reads=rk, writes=[kst])
        P.op("dve", lambda e: e.bn_stats(st[:, 1, :], r[:, 512:1024]), reads=rk, writes=[kst], partial=True)
        P.op("dve", lambda e: e.bn_aggr(mv[:], st[:]), reads=[kst], writes=[kmv])
        self.rstd(rs[:], mv[:, 1:2], 1.0, 1e-5, [kmv, "eps1e-05"], [krs])
        P.op("dve", lambda e: e.tensor_scalar(out, r, mv[:, 0:1], rs[:, 0:1], ALU.subtract, ALU.mult), reads=rk + [kmv, krs], writes=wk)
        P.op("pool", lambda e: e.tensor_tensor(out, out, G[:], ALU.mult), reads=wk + ["lnG"], writes=wk)
        P.op("pool", lambda e: e.tensor_tensor(out, out, B[:], ALU.add), reads=wk + ["lnB"], writes=wk)

    def mk_lnt(self):
        P = self.P
        self._lnt = [(P.sbuf("lnst", [128, 2, 6], F32), P.sbuf("lnmv", [128, 2], F32), P.sbuf("lnrs", [128, 1], F32)) for _ in range(2)]

    def qkv_tile(self, src, tt, t, sl, wt, wkey, QT, KT, V, vd, rope_tab, B):
        P = self.P
        xt, xb, xT, qkb, rts = B
        base = 4 * sl
        self.x_front(src, tt, sl, (xt, xb, xT, base + 3))
        for n in range(3):
            P.op("pe", multi([lambda e, n=n, c=c: e.matmul(self.bank(base + n), xT[sl][:, c, :], wt[:, c, n * 512:(n + 1) * 512], start=(c == 0), stop=(c == 7)) for c in range(8)]),
                 reads=["xT%d" % sl, wkey], writes=["pb%d" % (base + n)])
        ph = self.bank(base, 3)
        kq = "qkb%d" % sl
        pk = ["pb%d" % base, "pb%d" % (base + 1)]
        P.op("act", lambda e: e.activation(qkb[sl][:], ph[:, 0:1024], AF.Copy), reads=pk, writes=[kq])
        if rope_tab is not None:
            src3 = ph[:, 0:1024].rearrange("p (g d) -> p g d", d=64)
            dst3 = qkb[sl][:].rearrange("p (g d) -> p g d", d=64)
            self.rope(src3, dst3, rope_tab, 0, 8, 16, pk + ["ropeA"], [kq], "rt%d" % sl, rts[sl])
        pT2 = self.bankbf(base + 3).rearrange("p (c t) -> p c t", t=128)
        P.op("pe", multi([lambda e, c=c: e.transpose(pT2[:, c, :], qkb[sl][:, c * 128:(c + 1) * 128], self.ident[:]) for c in range(8)]),
             reads=[kq, "ident"], writes=["pb%d" % (base + 3)])
        P.op("dve", lambda e: e.tensor_copy(QT[0:64, 0:8:2, t * 128:(t + 1) * 128], pT2[0:64, 0:4, :]), reads=["pb%d" % (base + 3), "QTz"], writes=["QT"], partial=True)
        P.op("dve", lambda e: e.tensor_copy(QT[64:128, 1:8:2, t * 128:(t + 1) * 128], pT2[64:128, 0:4, :]), reads=["pb%d" % (base + 3), "QTz"], writes=["QT"], partial=True)
        P.op("act", lambda e: e.activation(KT[:, :, t * 128:(t + 1) * 128], pT2[:, 4:8, :], AF.Copy), reads=["pb%d" % (base + 3)], writes=["KT"], partial=True)
        P.op("act", lambda e: e.activation(V[:, t, :, 0:vd], self.bank(base + 2).rearrange("p (h d) -> p h d", d=vd), AF.Copy),
             reads=["pb%d" % (base + 2), "Vones"], writes=["V"], partial=True)

    def qkv_seq(self, src, s, wt, wkey, QT, KT, V, vd, rope, B):
        P = self.P
        for t in range(0, T, 2):
            recs = []
            for sl in (0, 1):
                r = Rec()
                self.P = r
                self.qkv_tile(src, s * T + t + sl, t + sl, sl, wt, wkey, QT, KT, V, vd, (self.ropeA[:, t + sl, :] if rope else None), B)
                recs.append(r)
            self.P = P
            interleave(P, recs)

    def dense_attn(self, maps, QT, KT, V, dk, dv, scale, epilogue):
        P = self.P
        E = self._E
        steps = [(qc, mi, kp) for qc in range(4) for mi in range(len(maps)) for kp in range(8)]
        ps_all = self.PS[:, 2 * 512:6 * 512].rearrange("p (b c) -> p b c", c=512)
        pairs = [0, 6]

        def qk(i):
            qc, mi, kp = steps[i]
            kblk, qblk, r0, vi = maps[mi]
            b0 = pairs[i % 2]
            P.op("pe", multi([lambda e, j=j: e.matmul(self.bank(b0 + j), KT[r0:r0 + dk, kblk, (2 * kp + j) * 128:(2 * kp + j + 1) * 128],
                                                      QT[r0:r0 + dk, qblk, qc * 512:(qc + 1) * 512], start=True, stop=True) for j in range(2)]),
                 reads=["QT", "KT"], writes=["pb%d" % b0, "pb%d" % (b0 + 1)])
            eb = i % 3
            P.op("act", lambda e: e.activation(E[eb][:], self.bank(b0, 2), AF.Exp, scale=scale), reads=["pb%d" % b0, "pb%d" % (b0 + 1)], writes=["E%d" % eb])

        def pv(i):
            qc, mi, kp = steps[i]
            kblk, qblk, r0, vi = maps[mi]
            eb = i % 3
            P.op("pe", multi([lambda e, qi=qi, j=j: e.matmul(self.bank(2 + qi)[:, 0:dv + 1 - DVX], E[eb][:, j * 512 + qi * 128:j * 512 + (qi + 1) * 128],
                                                             V[:, 2 * kp + j, vi, 0:dv + 1 - DVX], start=(kp == 0 and j == 0), stop=(kp == 7 and j == 1))
                              for j in range(2) for qi in range(4)]),
                 reads=["E%d" % eb, "V"], writes=["pb2", "pb3", "pb4", "pb5"], partial=(kp != 0))
            if kp == 7 and not None:
                epilogue(qc, mi, ps_all)
        n = len(steps)
        if None:
            return
        for i in range(n + 1):
            if i < n:
                qk(i)
            if i >= 1:
                pv(i - 1)

    def even_attn(self, l, src):
        P = self.P
        j = l // 2
        W = self.w
        lam_init = 0.8 - 0.6 * math.exp(-0.3 * l)
        if "A" not in self.phases:
            return self.even_attn_B(l, src)
        P.begin()
        self.mk_eps()
        wt = P.sbuf("wA", [128, 8, 1536], BF16)
        self.load_w_bf16(wt, W["even_w_in"][j][:, 0:1536], 8, 1536, "wA")
        xt = [P.sbuf("xt", [128, D], F32) for _ in range(2)]
        xb = [P.sbuf("xb", [128, D], BF16) for _ in range(2)]
        xT = [P.sbuf("xT", [128, 8, 128], BF16) for _ in range(2)]
        QT = P.sbuf("QT", [128, 8, S], BF16)
        KT = P.sbuf("KT", [128, 4, S], BF16)
        V = P.sbuf("V", [128, 16, 4, 129], BF16)
        P.op("pool", lambda e: e.memset(QT[:], 0.0), writes=["QTz"])
        qkb = [P.sbuf("qkb", [128, 1024], BF16) for _ in range(2)]
        rts = [(P.sbuf("rt1", [128, 16, 32], F32), P.sbuf("rt2", [128, 16, 32], F32)) for _ in range(2)]
        self._E = [P.sbuf("E", [128, 1024], BF16) for _ in range(3)]
        Om = [P.sbuf("Om", [128, 4, 128], F32) for _ in range(2)]
        rec = P.sbuf("rec", [128, 4], F32)
        dd = P.sbuf("dd", [128, 4, 128], F32)
        sq = P.sbuf("sq", [128, 4, 128], F32)
        ss = P.sbuf("ss", [128, 4], F32)
        AO = [P.sbuf("AO", [128, 4, 512], BF16) for _ in range(2)]
        lamv = P.sbuf("lamv", [128, 4, 64], F32)
        P.dma("sp", lambda e: e.dma_start(out=lamv[:], in_=W["even_lam"][j].unsqueeze(0).broadcast_to([128, 4, 64])), "bc_lamv", writes=["lamv"])
        lp = P.sbuf("lp", [128, 2, 64], F32)
        ls = P.sbuf("ls", [128, 2], F32)
        nlam = P.sbuf("nlam", [128, 1], F32)
        P.op("dve", lambda e: e.tensor_tensor(lp[:], lamv[:, 0:4:2, :], lamv[:, 1:4:2, :], ALU.mult), reads=["lamv"], writes=["lp"])
        P.op("dve", lambda e: e.tensor_reduce(ls[:], lp[:], AX.X, ALU.add), reads=["lp"], writes=["ls"])
        P.op("act", lambda e: e.activation(ls[:], ls[:], AF.Exp), reads=["ls"], writes=["ls"])
        P.op("dve", lambda e: e.tensor_tensor(nlam[:], ls[:, 1:2], ls[:, 0:1], ALU.subtract), reads=["ls"], writes=["nlam"])
        P.op("dve", lambda e: e.tensor_scalar(nlam[:], nlam[:], -lam_init, None, ALU.add), reads=["nlam"], writes=["nlam"])
        gsub = self.bcast_load("gsub", W["even_subln_g"][j], 128, "gsub")
        P.op("dve", lambda e: e.tensor_scalar(gsub[:], gsub[:], 1.0 - lam_init, None, ALU.mult), reads=["gsub"], writes=["gsub"])
        P.op("dve", lambda e: e.memset(V[:, :, :, 128:129], 1.0), writes=["Vones"])
        for s in range(self.nseq):
            self.qkv_seq(src, s, wt, "wA", QT, KT, V, 128, True, (xt, xb, xT, qkb, rts))

            def epi(qc, mi, ps_all, s=s):
                h, m = mi // 2, mi % 2
                ab = (qc % 2)
                P.op("dve", lambda e: e.reciprocal(rec[:], ps_all[:, :, 128]), reads=["pb2", "pb3", "pb4", "pb5"], writes=["rec"])
                P.op("dve", lambda e: e.tensor_tensor(Om[m][:], ps_all[:, :, 0:128], rec[:].unsqueeze(2).to_broadcast([128, 4, 128]), ALU.mult),
                     reads=["pb2", "pb3", "pb4", "pb5", "rec"], writes=["Om%d" % m])
                if m == 1:
                    P.op("dve", lambda e: e.scalar_tensor_tensor(dd[:], Om[1][:], nlam[:, 0:1], Om[0][:], ALU.mult, ALU.add), reads=["Om0", "Om1", "nlam"], writes=["dd"])
                    P.op("pool", lambda e: e.tensor_tensor(sq[:], dd[:], dd[:], ALU.mult), reads=["dd"], writes=["sq"])
                    P.op("dve", lambda e: e.tensor_reduce(ss[:], sq[:], AX.X, ALU.add), reads=["sq"], writes=["ss"])
                    self.rstd(ss[:], ss[:], 1.0 / 128, 1e-6, ["ss", "eps1e-06"], ["ss"])
                    P.op("dve", lambda e: e.tensor_tensor(dd[:], dd[:], ss[:].unsqueeze(2).to_broadcast([128, 4, 128]), ALU.mult), reads=["dd", "ss"], writes=["dd"])
                    P.op("pool", lambda e: e.tensor_tensor(AO[ab][:, :, h * 128:(h + 1) * 128], dd[:], gsub[:].unsqueeze(1).to_broadcast([128, 4, 128]), ALU.mult),
                         reads=["dd", "gsub"], writes=["AO%d" % ab], partial=True)
                    if h == 3:
                        r0 = s * S + qc * 512
                        P.dma("pool", lambda e: e.dma_start(out=self.ao[r0:r0 + 512, 0:512].rearrange("(q p) n -> p q n", p=128), in_=AO[ab][:]),
                              "ao%d" % ab, reads=["AO%d" % ab], writes=["ao_d"], partial=True)
            maps = [(g // 2, g, 0, g // 2) for g in range(8)]
            self.dense_attn(maps, QT, KT, V, 128, 128, 0.125, epi)
        P.end()
        self.even_attn_B(l, src)

    def even_attn_B(self, l, src):
        P = self.P
        j = l // 2
        W = self.w
        if "B" not in self.phases:
            return
        P.begin()
        self.mk_eps()
        wt = P.sbuf("wB", [128, 8, 416], BF16)
        self.load_w_bf16(wt, W["even_w_in"][j][:, 1536:1952], 8, 416, "wB")
        wuq = P.sbuf("wuq", [128, 2, 768], BF16)
        self.load_w_bf16(wuq, W["even_w_uq"][j], 2, 768, "wuq")
        wukv = P.sbuf("wukv", [128, 1, 1024], BF16)
        self.load_w_bf16(wukv, W["even_w_ukv"][j], 1, 1024, "wukv")
        qg = self.bcast_load("qg", W["even_q_norm_g"][j], 256, "qg")
        kvg = self.bcast_load("kvg", W["even_kv_norm_g"][j], 128, "kvg")
        xt = [P.sbuf("xt", [128, D], F32) for _ in range(2)]
        xb = [P.sbuf("xb", [128, D], BF16) for _ in range(2)]
        xT = [P.sbuf("xT", [128, 8, 128], BF16) for _ in range(2)]
        QT = P.sbuf("QT", [128, 8, S], BF16)
        KT = P.sbuf("KT", [128, 8, S], BF16)
        V = P.sbuf("V", [128, 16, 8, 65], BF16)
        self._ropet = (P.sbuf("rt1", [128, 8, 32], F32), P.sbuf("rt2", [128, 8, 32], F32))
        self._E = [P.sbuf("E", [128, 1024], BF16) for _ in range(3)]
        junk = P.sbuf("junk", [128, 384], F32)
        ssq = P.sbuf("ssq", [128, 2], F32)
        cn = P.sbuf("cn", [128, 384], BF16)
        cT = P.sbuf("cT", [128, 3, 128], BF16)
        qb = P.sbuf("qb", [128, 8, 96], BF16)
        kcat = P.sbuf("kcat", [128, 8, 96], BF16)
        krr = P.sbuf("krr", [128, 1, 32], BF16)
        rec = P.sbuf("rec", [128, 4], F32)
        AO = [P.sbuf("AO", [128, 4, 512], BF16) for _ in range(2)]
        P.op("dve", lambda e: e.memset(V[:, :, :, 64:65], 1.0), writes=["Vones"])
        for s in range(self.nseq):
            self.x_front(src, s * T, 0, (xt, xb, xT, 5))
            for t in range(T):
                b = t % 2
                tt = s * T + t
                if t + 1 < T:
                    self.x_front(src, tt + 1, (t + 1) % 2, (xt, xb, xT, 5))
                P.op("pe", multi([lambda e, c=c: e.matmul(self.bank(0)[:, 0:416], xT[b][:, c, :], wt[:, c, :], start=(c == 0), stop=(c == 7)) for c in range(8)]),
                     reads=["xT%d" % b, "wB"], writes=["pb0"])
                hB = self.bank(0)
                P.op("act", lambda e: e.activation(junk[:, 0:256], hB[:, 0:256], AF.Square, accum_out=ssq[:, 0:1]), reads=["pb0"], writes=["ssq", "junk"])
                P.op("act", lambda e: e.activation(junk[:, 256:384], hB[:, 256:384], AF.Square, accum_out=ssq[:, 1:2]), reads=["pb0"], writes=["ssq", "junk"], partial=True)
                self.rstd(ssq[:, 0:1], ssq[:, 0:1], 1.0 / 256, 1e-6, ["ssq", "eps1e-06"], ["ssq"])
                self.rstd(ssq[:, 1:2], ssq[:, 1:2], 1.0 / 128, 1e-6, ["ssq", "eps1e-06"], ["ssq"])
                P.op("dve", lambda e: e.scalar_tensor_tensor(cn[:, 0:256], hB[:, 0:256], ssq[:, 0:1], qg[:], ALU.mult, ALU.mult), reads=["pb0", "ssq", "qg"], writes=["cn"])
                P.op("dve", lambda e: e.scalar_tensor_tensor(cn[:, 256:384], hB[:, 256:384], ssq[:, 1:2], kvg[:], ALU.mult, ALU.mult), reads=["pb0", "ssq", "kvg"], writes=["cn"], partial=True)
                P.op("act", lambda e: e.activation(krr[:, 0, :], hB[:, 384:416], AF.Copy), reads=["pb0"], writes=["krr"])
                self.rope(hB[:, 384:416].unsqueeze(1), krr[:], self.ropeB[:, t, :], 0, 16, 1, ["pb0", "ropeB"], ["krr"], "rt")
                P.op("pool", lambda e: e.tensor_copy(kcat[:, :, 64:96], krr[:].to_broadcast([128, 8, 32])), reads=["krr"], writes=["kcat"])
                pTc = self.bankbf(6).rearrange("p (c t) -> p c t", t=128)
                P.op("pe", multi([lambda e, c=c: e.transpose(pTc[:, c, :], cn[:, c * 128:(c + 1) * 128], self.ident[:]) for c in range(3)]), reads=["cn", "ident"], writes=["pb6"])
                P.op("dve", lambda e: e.tensor_copy(cT[:], pTc[:, 0:3, :]), reads=["pb6"], writes=["cT"])
                fl = [lambda e, c=c, c0=c0, n=n: e.matmul(self.PS[:, 512 + c0:512 + c0 + n], cT[:, c, :], wuq[:, c, c0:c0 + n], start=(c == 0), stop=(c == 1))
                      for (c0, n) in ((0, 512), (512, 256)) for c in range(2)]
                fl += [lambda e, n=n: e.matmul(self.bank(3 + n), cT[:, 2, :], wukv[:, 0, n * 512:(n + 1) * 512], start=True, stop=True) for n in range(2)]
                P.op("pe", multi(fl), reads=["cT", "wuq", "wukv"], writes=["pb1", "pb2", "pb3", "pb4"])
                q3 = self.PS[:, 512:1280].rearrange("p (h d) -> p h d", d=96)
                kv3 = self.PS[:, 1536:2560].rearrange("p (h d) -> p h d", d=128)
                P.op("act", lambda e: e.activation(qb[:], q3, AF.Copy), reads=["pb1", "pb2"], writes=["qb"])
                self.rope(q3, qb[:], self.ropeB[:, t, :], 64, 16, 8, ["pb1", "pb2", "ropeB"], ["qb"], "rt")
                P.op("act", lambda e: e.activation(kcat[:, :, 0:64], kv3[:, :, 0:64], AF.Copy), reads=["pb3", "pb4"], writes=["kcat"], partial=True)
                P.op("dve", lambda e, t=t: e.tensor_copy(V[:, t, :, 0:64], kv3[:, :, 64:128]), reads=["pb3", "pb4", "Vones"], writes=["V"], partial=True)
                pTq = self.bankbf(7).rearrange("p (c t) -> p c t", t=128)
                pTk = self.bankbf(6).rearrange("p (c t) -> p c t", t=128)
                P.op("pe", multi([lambda e, h=h: e.transpose(pTq[0:96, h, :], qb[:, h, :], self.ident[:]) for h in range(8)]), reads=["qb", "ident"], writes=["pb7"])
                P.op("dve", lambda e, t=t: e.tensor_copy(QT[0:96, :, t * 128:(t + 1) * 128], pTq[0:96, :, :]), reads=["pb7"], writes=["QT"], partial=True)
                P.op("pe", multi([lambda e, h=h: e.transpose(pTk[0:96, h, :], kcat[:, h, :], self.ident[:]) for h in range(8)]), reads=["kcat", "ident"], writes=["pb6"])
                P.op("act", lambda e, t=t: e.activation(KT[0:96, :, t * 128:(t + 1) * 128], pTk[0:96, :, :], AF.Copy), reads=["pb6"], writes=["KT"], partial=True)

            def epi(qc, mi, ps_all, s=s):
                h = mi
                ab = qc % 2
                P.op("dve", lambda e: e.reciprocal(rec[:], ps_all[:, :, 64]), reads=["pb2", "pb3", "pb4", "pb5"], writes=["rec"])
                P.op("dve", lambda e: e.tensor_tensor(AO[ab][:, :, h * 64:(h + 1) * 64], ps_all[:, :, 0:64], rec[:].unsqueeze(2).to_broadcast([128, 4, 64]), ALU.mult),
                     reads=["pb2", "pb3", "pb4", "pb5", "rec"], writes=["AO%d" % ab], partial=True)
                if h == 7:
                    r0 = s * S + qc * 512
                    P.dma("pool", lambda e: e.dma_start(out=self.ao[r0:r0 + 512, 512:1024].rearrange("(q p) n -> p q n", p=128), in_=AO[ab][:]),
                          "ao%d" % ab, reads=["AO%d" % ab], writes=["ao_d"], partial=True)
            maps = [(h, h, 0, h) for h in range(8)]
            self.dense_attn(maps, QT, KT, V, 96, 64, 96 ** -0.5, epi)
        P.end()

    def odd_attn(self, l, src):
        P = self.P
        j = l // 2
        W = self.w
        pl = nbr_patterns()
        for hh in range(2):
            P.begin()
            self.mk_eps()
            wt = P.sbuf("wC", [128, 8, 1536], BF16)
            for i in range(3):
                self.load_w_bf16(wt, W["odd_w_qkv"][j][:, i * 1024 + hh * 512:i * 1024 + hh * 512 + 512], 8, 512, "wC", c0=i * 512)
            xt = [P.sbuf("xt", [128, D], F32) for _ in range(2)]
            xb = [P.sbuf("xb", [128, D], BF16) for _ in range(2)]
            xT = [P.sbuf("xT", [128, 8, 128], BF16) for _ in range(2)]
            QT = P.sbuf("QT", [128, 8, S], BF16)
            KT = P.sbuf("KT", [128, 4, S], BF16)
            V = P.sbuf("V", [128, 16, 8, 65], BF16)
            P.op("pool", lambda e: e.memset(QT[:], 0.0), writes=["QTz"])
            qkb = [P.sbuf("qkb", [128, 1024], BF16) for _ in range(2)]
            bias = [P.sbuf("bias", [128, NPAT, 128], F32) for _ in range(2)]
            tmp = [P.sbuf("tmp", [128, 5, 128], F32) for _ in range(3)]
            E = [P.sbuf("E", [128, 5, 128], BF16) for _ in range(3)]
            rec = P.sbuf("rec", [128, 4], F32)
            AO = P.sbuf("AO", [128, 16, 512], BF16)
            P.op("dve", lambda e: e.memset(V[:, :, :, 64:65], 1.0), writes=["Vones"])
            iters = [(h, qt) for h in range(8) for qt in range(16)]
            for s in range(self.nseq):
                self.qkv_seq(src, s, wt, "wC", QT, KT, V, 64, False, (xt, xb, xT, qkb, None))

                def front(it):
                    h, qt = iters[it]
                    bb = h % 2
                    if qt == 0:
                        hg = hh * 8 + h
                        P.dma("sp", lambda e: e.dma_start(out=bias[bb][:], in_=W["nbias"][j, hg]), "bias%d" % bb, writes=["bias%d" % bb])
                    blk, r0 = h // 2, (h % 2) * 64
                    lst = pl[qt]
                    n = len(lst)
                    rb = it % 3
                    sbk = 2 + 2 * rb
                    ps_s = self.PS[:, sbk * 512:sbk * 512 + 640].rearrange("p (i q) -> p i q", q=128)
                    P.op("pe", multi([lambda e, i=i, kt=kt: e.matmul(ps_s[:, i, :], KT[:, blk, kt * 128:(kt + 1) * 128], QT[:, h, qt * 128:(qt + 1) * 128], start=True, stop=True)
                                      for i, (kt, pat) in enumerate(lst)]),
                         reads=["QT", "KT"], writes=["pb%d" % sbk, "pb%d" % (sbk + 1)])
                    p0 = lst[0][1]
                    P.op("dve", lambda e: e.scalar_tensor_tensor(tmp[rb][:, 0:n, :], ps_s[:, 0:n, :], 0.125, bias[bb][:, p0:p0 + n, :], ALU.mult, ALU.add),
                         reads=["pb%d" % sbk, "pb%d" % (sbk + 1), "bias%d" % bb], writes=["tmp%d" % rb])
                    P.op("act", lambda e: e.activation(E[rb][:, 0:n, :], tmp[rb][:, 0:n, :], AF.Exp), reads=["tmp%d" % rb], writes=["E%d" % rb])

                def back(it):
                    h, qt = iters[it]
                    lst = pl[qt]
                    n = len(lst)
                    rb = it % 3
                    ob = (qt // 4) % 2
                    col = (qt % 4) * 128
                    P.op("pe", multi([lambda e, i=i, kt=kt: e.matmul(self.bank(ob)[:, col:col + 65], E[rb][:, i, :], V[:, kt, h, :], start=(i == 0), stop=(i == n - 1))
                                      for i, (kt, pat) in enumerate(lst)]),
                         reads=["E%d" % rb, "V"], writes=["pb%d" % ob], partial=(qt % 4 != 0))
                    if qt % 4 == 3:
                        ps4 = self.bank(ob).rearrange("p (q c) -> p q c", c=128)
                        q0 = qt - 3
                        P.op("dve", lambda e: e.reciprocal(rec[:], ps4[:, :, 64]), reads=["pb%d" % ob], writes=["rec"])
                        P.op("dve", lambda e: e.tensor_tensor(AO[:, q0:q0 + 4, h * 64:(h + 1) * 64], ps4[:, :, 0:64], rec[:].unsqueeze(2).to_broadcast([128, 4, 64]), ALU.mult),
                             reads=["pb%d" % ob, "rec"], writes=["AO"], partial=True)
                nit = len(iters)
                LOOK = 2
                for jx in range(nit + LOOK):
                    if jx < nit:
                        front(jx)
                    if jx >= LOOK:
                        back(jx - LOOK)
                r0_ = s * S
                P.dma("pool", lambda e: e.dma_start(out=self.ao[r0_:r0_ + S, hh * 512:(hh + 1) * 512].rearrange("(q p) n -> p q n", p=128), in_=AO[:]),
                      "aoC", reads=["AO"], writes=["ao_d"])
            P.end()

    def oproj(self, l, src):
        P = self.P
        j = l // 2
        W = self.w
        C = self.C
        P.begin()
        self.mk_eps()
        self.mk_lnt()
        wo = P.sbuf("wo", [128, 8, D], BF16)
        self.load_w_bf16(wo, (W["even_w_o"] if l % 2 == 0 else W["odd_w_o"])[j], 8, D, "wo")
        G = self.bcast_load("G", W["ln1_g"][l], D, "lnG")
        B = self.bcast_load("B", W["ln1_b"][l], D, "lnB")
        wr = P.sbuf("wr", [128, 8, 36], F32)
        P.dma("sp", lambda e: e.dma_start(out=wr[:], in_=W["w_router"][l].rearrange("(c p) n -> p c n", p=128)), "bc_wr", writes=["wr"])
        br = self.bcast_load("br", W["b_router"][l], 36, "br")

        def two(name, shape, dt):
            return [P.sbuf(name, shape, dt) for _ in range(2)]
        T_ = dict(aot=two("aot", [128, D], BF16), aoT=two("aoT", [128, 8, 128], BF16), xt=two("xt", [128, D], F32), r=two("r", [128, D], F32),
                  x1=[P.sbuf("x1", [128, D], F32) for _ in range(4)], x1b=[P.sbuf("x1b", [128, D], BF16) for _ in range(4)], x1T=two("x1T", [128, 8, 128], F32),
                  lg=two("lg", [128, 36], F32), sm=two("sm", [128, 16], F32), pen=two("pen", [128, 4], F32), me=two("me", [128, 32], F32),
                  top8=two("top8", [128, 8], F32), idx8=two("idx8", [128, 8], U32), ef=two("ef", [128, 2], F32), oh=two("oh", [128, 2, 32], F32),
                  Mb=two("Mb", [128, 32], BF16), pos=two("pos", [128, 32], F32), prod=two("prod", [128, 2, 32], F32), slotf=two("slotf", [128, 2], F32),
                  posf=two("posf", [128, 2], F32), ov=two("ov", [128, 2], F32), sidxf=two("sidxf", [128, 2], F32), gidxf=two("gidxf", [128, 2], F32),
                  sidx=two("sidx", [128, 2], I32))
        cnt = P.sbuf("cnt", [128, 32], F32)
        P.op("dve", lambda e: e.memset(cnt[:], 0.0), writes=["cnt"])
        XS = ["xs%d" % e_ for e_ in range(32)]
        prev2 = []
        for tt in range(0, self.NT, 2):
            p1, p2 = [], []
            for sl in (0, 1):
                r_ = Rec()
                self.P = r_
                self.oproj_tile(l, src, tt + sl, sl, T_, wo, G, B, wr, br, cnt, XS)
                k = r_.l.index(("mark",))
                a_, b_ = Rec(), Rec()
                a_.l, b_.l = r_.l[:k], r_.l[k + 1:]
                p1.append(a_)
                p2.append(b_)
            self.P = P
            interleave(P, p1 + prev2)
            prev2 = p2
        interleave(P, prev2)
        P.end()

    def oproj_tile(self, l, src, tt, b, T_, wo, G, B, wr, br, cnt, XS):
        P = self.P
        C = self.C
        b4 = ((tt // 2) % 2) * 2 + b
        aot, aoT, xt, r, x1, x1b, x1T = T_["aot"][b], T_["aoT"][b], T_["xt"][b], T_["r"][b], T_["x1"][b4], T_["x1b"][b4], T_["x1T"][b]
        lg, sm, pen, me, top8, idx8, ef, oh, Mb, pos, prod = (T_[k][b] for k in ("lg", "sm", "pen", "me", "top8", "idx8", "ef", "oh", "Mb", "pos", "prod"))
        slotf, posf, ov, sidxf, gidxf, sidx = (T_[k][b] for k in ("slotf", "posf", "ov", "sidxf", "gidxf", "sidx"))

        def K(n):
            if n in ("x1", "x1b", "x1o"):
                return "%s_%d" % (n, b4)
            return "%s_%d" % (n, b)
        base = 4 * b
        pb = ["pb%d" % (base + i) for i in range(4)]
        rows = slice(tt * 128, (tt + 1) * 128)
        P.dma("sp", lambda e: e.dma_start(out=aot[:], in_=self.ao[rows, :]), K("aot"), writes=[K("aot")])
        P.dma("sp", lambda e: e.dma_start(out=xt[:], in_=src[rows, :]), K("xt"), writes=[K("xt")])
        pT = self.bankbf(base + 0).rearrange("p (c t) -> p c t", t=128)
        P.op("pe", multi([lambda e, c=c: e.transpose(pT[:, c, :], aot[:, c * 128:(c + 1) * 128], self.ident[:]) for c in range(8)]), reads=[K("aot"), "ident"], writes=[pb[0]])
        P.op("dve", lambda e: e.tensor_copy(aoT[:], pT), reads=[pb[0]], writes=[K("aoT")])
        P.op("pe", multi([lambda e, n=n, c=c: e.matmul(self.bank(base + n), aoT[:, c, :], wo[:, c, n * 512:(n + 1) * 512], start=(c == 0), stop=(c == 7)) for n in range(2) for c in range(8)]),
             reads=[K("aoT"), "wo"], writes=[pb[0], pb[1]])
        P.op("dve", lambda e: e.scalar_tensor_tensor(r[:], xt[:], ALPHA, self.bank(base, 2), ALU.mult, ALU.add), reads=[K("xt"), pb[0], pb[1]], writes=[K("r")])
        self.layer_norm(r[:], x1[:], G, B, [K("r")], [K("x1")], sl=b)
        P.op("act", lambda e: e.activation(x1b[:], x1[:], AF.Copy), reads=[K("x1")], writes=[K("x1b")])
        P.dma("act", lambda e: e.dma_start(out=self.res1[rows, :], in_=x1[:]), K("x1o"), reads=[K("x1")], writes=["res1_d"], partial=True)
        P.l.append(("mark",))
        pTf = self.PS[:, (base + 2) * 512:(base + 4) * 512].rearrange("p (c t) -> p c t", t=128)
        P.op("pe", multi([lambda e, c=c: e.transpose(pTf[:, c, :], x1[:, c * 128:(c + 1) * 128], self.identf[:]) for c in range(8)]), reads=[K("x1"), "identf"], writes=[pb[2], pb[3]])
        P.op("act", lambda e: e.activation(x1T[:], pTf, AF.Copy), reads=[pb[2], pb[3]], writes=[K("x1T")])
        pl_ = self.bank(base + 2)[:, 0:36]
        P.op("pe", multi([lambda e, c=c: e.matmul(pl_, x1T[:, c, :], wr[:, c, :], start=(c == 0), stop=(c == 7)) for c in range(8)]), reads=[K("x1T"), "wr"], writes=[pb[2]])
        P.op("dve", lambda e: e.tensor_tensor(lg[:], pl_, br[:], ALU.add), reads=[pb[2], "br"], writes=[K("lg")])
        P.op("dve", lambda e: e.tensor_reduce(sm[:, 0:1], lg[:, 0:4], AX.X, ALU.max), reads=[K("lg")], writes=[K("sm0")])
        P.op("dve", lambda e: e.tensor_scalar(pen[:], lg[:, 0:4], sm[:, 0:1], None, ALU.subtract), reads=[K("lg"), K("sm0")], writes=[K("pen")])
        P.op("act", lambda e: e.activation(sm[:, 4:8], pen[:], AF.Exp, accum_out=sm[:, 1:2]), reads=[K("pen")], writes=[K("sm1")])
        P.op("dve", lambda e: e.reciprocal(sm[:, 2:3], sm[:, 1:2]), reads=[K("sm1")], writes=[K("sm2")])
        P.op("dve", lambda e: e.tensor_scalar(pen[:], pen[:], 0.0, 10000.0, ALU.is_ge, ALU.mult), reads=[K("pen")], writes=[K("pen")])
        P.op("dve", lambda e: e.tensor_scalar(pen[:], pen[:], -10000.0, None, ALU.add), reads=[K("pen")], writes=[K("pen")])
        P.op("dve", lambda e: e.tensor_tensor(me[:].rearrange("p (g k) -> p g k", k=8), lg[:, 4:36].rearrange("p (g k) -> p g k", k=8),
                                              pen[:].unsqueeze(2).to_broadcast([128, 4, 8]), ALU.add), reads=[K("lg"), K("pen")], writes=[K("me")])
        P.op("dve", lambda e: e.max(top8[:], me[:]), reads=[K("me")], writes=[K("top8")])
        P.op("dve", lambda e: e.max_index(idx8[:], top8[:], me[:]), reads=[K("me"), K("top8")], writes=[K("idx8")])
        P.op("dve", lambda e: e.tensor_tensor(sm[:, 8:9], top8[:, 1:2], top8[:, 0:1], ALU.subtract), reads=[K("top8")], writes=[K("sm8")])
        P.op("act", lambda e: e.activation(sm[:, 9:10], sm[:, 8:9], AF.Exp), reads=[K("sm8")], writes=[K("sm9")])
        P.op("dve", lambda e: e.tensor_scalar(sm[:, 10:11], sm[:, 9:10], 1.0, None, ALU.add), reads=[K("sm9")], writes=[K("sm10")])
        P.op("dve", lambda e: e.reciprocal(sm[:, 10:11], sm[:, 10:11]), reads=[K("sm10")], writes=[K("sm10")])
        P.op("dve", lambda e: e.tensor_tensor(sm[:, 12:13], sm[:, 10:11], sm[:, 2:3], ALU.mult), reads=[K("sm10"), K("sm2")], writes=[K("sm12")])
        P.op("dve", lambda e: e.tensor_tensor(sm[:, 13:14], sm[:, 12:13], sm[:, 9:10], ALU.mult), reads=[K("sm12"), K("sm9")], writes=[K("sm13")])
        P.op("dve", lambda e: e.tensor_copy(ef[:], idx8[:, 0:2]), reads=[K("idx8")], writes=[K("ef")])
        for k in range(2):
            P.op("dve", lambda e, k=k: e.tensor_scalar(oh[:, k, :], self.iota32[:], ef[:, k:k + 1], None, ALU.is_equal), reads=[K("ef"), "iota32"], writes=[K("oh%d" % k)])
        P.op("dve", lambda e: e.tensor_tensor(Mb[:], oh[:, 0, :], oh[:, 1, :], ALU.add), reads=[K("oh0"), K("oh1")], writes=[K("Mb")])
        pr = self.bank(base + 3)[:, 0:32]
        pr2 = self.bank(base + 3)[:, 64:96]
        P.op("pe", multi([lambda e: e.matmul(pr, self.U[:], Mb[:], start=True, stop=True), lambda e: e.matmul(pr2, self.ones[:], Mb[:], start=True, stop=True)]),
             reads=["U", "ones", K("Mb")], writes=[pb[3]])
        P.op("dve", lambda e: e.tensor_tensor(pos[:], pr, cnt[:], ALU.add), reads=[pb[3], "cnt"], writes=[K("pos")])
        if b == 1:
            tot0 = self.bank(3)[:, 64:96]
            P.op("dve", lambda e: e.tensor_tensor(pos[:], tot0, pos[:], ALU.add), reads=["pb3", K("pos")], writes=[K("pos")])
            P.op("dve", lambda e: e.tensor_tensor(cnt[:], tot0, cnt[:], ALU.add), reads=["pb3", "cnt"], writes=["cnt"])
            P.op("dve", lambda e: e.tensor_tensor(cnt[:], pr2, cnt[:], ALU.add), reads=[pb[3], "cnt"], writes=["cnt"])
        P.op("dve", lambda e: e.tensor_tensor(prod[:], oh[:], pos[:].unsqueeze(1).to_broadcast([128, 2, 32]), ALU.mult), reads=[K("oh0"), K("oh1"), K("pos")], writes=[K("prod")])
        P.op("dve", lambda e: e.tensor_reduce(posf[:], prod[:], AX.X, ALU.add), reads=[K("prod")], writes=[K("posf")])
        P.op("dve", lambda e: e.tensor_scalar(slotf[:], ef[:], float(C), None, ALU.mult), reads=[K("ef")], writes=[K("slotf")])
        P.op("dve", lambda e: e.tensor_tensor(slotf[:], slotf[:], posf[:], ALU.add), reads=[K("slotf"), K("posf")], writes=[K("slotf")])
        P.op("dve", lambda e: e.tensor_scalar(ov[:], posf[:], float(C), None, ALU.is_ge), reads=[K("posf")], writes=[K("ov")])
        P.op("dve", lambda e: e.scalar_tensor_tensor(sidxf[:], ov[:], 1.0e6, slotf[:], ALU.mult, ALU.add), reads=[K("ov"), K("slotf")], writes=[K("sidxf")])
        P.op("dve", lambda e: e.tensor_copy(sidx[:], sidxf[:]), reads=[K("sidxf")], writes=[K("sidx")])
        P.op("dve", lambda e: e.tensor_scalar(gidxf[:], slotf[:], -1.0, float(self.NSLOT), ALU.mult, ALU.add), reads=[K("slotf")], writes=[K("gidxf")])
        P.op("dve", lambda e: e.tensor_tensor(gidxf[:], gidxf[:], ov[:], ALU.mult), reads=[K("gidxf"), K("ov")], writes=[K("gidxf")])
        P.op("dve", lambda e: e.tensor_tensor(gidxf[:], gidxf[:], slotf[:], ALU.add), reads=[K("gidxf"), K("slotf")], writes=[K("gidxf")])
        P.op("dve", lambda e: e.tensor_copy(self.gidx[:, tt, :], gidxf[:]), reads=[K("gidxf")], writes=["gidx"], partial=True)
        P.op("dve", lambda e: e.tensor_scalar(ov[:], ov[:], -1.0, 1.0, ALU.mult, ALU.add), reads=[K("ov")], writes=[K("ov")])
        P.op("dve", lambda e: e.tensor_tensor(self.gates[:, tt, :], sm[:, 12:14], ov[:], ALU.mult), reads=[K("sm12"), K("sm13"), K("ov")], writes=["gates"], partial=True)
        for k in range(0 if None else 2):
            P.dma("pool", lambda e, k=k: e.indirect_dma_start(out=self.xs[:, :], out_offset=bass.IndirectOffsetOnAxis(ap=sidx[:, k:k + 1], axis=0),
                                                              in_=x1b[:], in_offset=None, bounds_check=self.bc_reg, oob_is_err=False),
                  K("scat"), reads=[K("x1b"), K("sidx")], writes=XS, partial=True, wfull=([] if None else ["xs_chain"]))

    def experts(self, l):
        P = self.P
        W = self.w
        C = self.C
        NS = C // 128
        HW = C // 2
        P.begin()
        wg = [P.sbuf("wg", [128, 8, 512], BF16) for _ in range(2)]
        wu = [P.sbuf("wu", [128, 8, 512], BF16) for _ in range(2)]
        wd = [P.sbuf("wd", [128, 4, D], BF16) for _ in range(2)]
        xst = [P.sbuf("xst", [128, D], BF16) for _ in range(2)]
        xsT = [P.sbuf("xsT", [128, 8, C], BF16) for _ in range(2)]
        hT = [P.sbuf("hT", [128, 4, C], BF16) for _ in range(2)]
        sg = [P.sbuf("sg", [128, HW], F32) for _ in range(2)]
        yt = [P.sbuf("yt", [128, D], F32) for _ in range(2)]
        it = 0
        for ex in range(32):
            eb = ex % 2
            self.load_w_bf16(wg[eb], W["w_gate"][l, ex], 8, 512, "wg%d" % eb)
            self.load_w_bf16(wu[eb], W["w_up"][l, ex], 8, 512, "wu%d" % eb)
            self.load_w_bf16(wd[eb], W["w_down"][l, ex], 4, D, "wd%d" % eb)
            for st in range(NS):
                b = st % 2
                r0 = ex * C + st * 128
                P.dma("sp", lambda e, r0=r0, b=b: e.dma_start(out=xst[b][:], in_=self.xs[r0:r0 + 128, :]), "xst%d" % b, reads=["xs%d" % ex], writes=["xst%d" % b])
                pT = self.bankbf(st % 2).rearrange("p (c t) -> p c t", t=128)
                P.op("pe", multi([lambda e, c=c: e.transpose(pT[:, c, :], xst[b][:, c * 128:(c + 1) * 128], self.ident[:]) for c in range(8)]), reads=["xst%d" % b, "ident"], writes=["pb%d" % (st % 2)])
                if st % 2 == 0:
                    P.op("dve", lambda e, st=st, pT=pT: e.tensor_copy(xsT[eb][:, :, st * 128:(st + 1) * 128], pT), reads=["pb0"], writes=["xsT%d" % eb], partial=True)
                else:
                    P.op("act", lambda e, st=st, pT=pT: e.activation(xsT[eb][:, :, st * 128:(st + 1) * 128], pT, AF.Copy), reads=["pb1"], writes=["xsT%d" % eb], partial=True)
            for f in range(4):
                for hf in range(2):
                    ib = it % 2
                    it += 1
                    pg = self.bank(2 + 2 * ib)[:, 0:HW]
                    pu = self.bank(3 + 2 * ib)[:, 0:HW]
                    P.op("pe", multi([lambda e, c=c: e.matmul(pg, wg[eb][:, c, f * 128:(f + 1) * 128], xsT[eb][:, c, hf * HW:(hf + 1) * HW], start=(c == 0), stop=(c == 7)) for c in range(8)]),
                         reads=["wg%d" % eb, "xsT%d" % eb], writes=["pb%d" % (2 + 2 * ib)])
                    P.op("pe", multi([lambda e, c=c: e.matmul(pu, wu[eb][:, c, f * 128:(f + 1) * 128], xsT[eb][:, c, hf * HW:(hf + 1) * HW], start=(c == 0), stop=(c == 7)) for c in range(8)]),
                         reads=["wu%d" % eb, "xsT%d" % eb], writes=["pb%d" % (3 + 2 * ib)])
                    P.op("act", lambda e, ib=ib, pg=pg: e.activation(sg[ib][:], pg, AF.Silu), reads=["pb%d" % (2 + 2 * ib)], writes=["sg%d" % ib])
                    P.op("dve", lambda e, ib=ib, pu=pu, f=f, hf=hf: e.tensor_tensor(hT[eb][:, f, hf * HW:(hf + 1) * HW], sg[ib][:], pu, ALU.mult),
                         reads=["sg%d" % ib, "pb%d" % (3 + 2 * ib)], writes=["hT%d" % eb], partial=True)
            for st in range(NS):
                b = st % 2
                r0 = ex * C + st * 128
                yb0 = 6 if st % 2 == 0 else 0
                P.op("pe", multi([lambda e, n=n, f=f: e.matmul(self.bank(yb0 + n), hT[eb][:, f, st * 128:(st + 1) * 128], wd[eb][:, f, n * 512:(n + 1) * 512], start=(f == 0), stop=(f == 3)) for n in range(2) for f in range(4)]),
                     reads=["hT%d" % eb, "wd%d" % eb], writes=["pb%d" % yb0, "pb%d" % (yb0 + 1)])
                if st % 2 == 0:
                    P.op("dve", lambda e, b=b: e.tensor_copy(yt[b][:], self.bank(yb0, 2)), reads=["pb%d" % yb0, "pb%d" % (yb0 + 1)], writes=["yt%d" % b])
                else:
                    P.op("act", lambda e, b=b: e.activation(yt[b][:], self.bank(yb0, 2), AF.Copy), reads=["pb%d" % yb0, "pb%d" % (yb0 + 1)], writes=["yt%d" % b])
                P.dma("act", lambda e, r0=r0, b=b: e.dma_start(out=self.ys[r0:r0 + 128, :], in_=yt[b][:]), "yo%d" % b, reads=["yt%d" % b], writes=["ys%d" % ex], partial=True)
        P.end()

    def combine(self, l, dst):
        P = self.P
        W = self.w
        P.begin()
        self.mk_eps()
        self.mk_lnt()
        G = self.bcast_load("G", W["ln2_g"][l], D, "lnG")
        B = self.bcast_load("B", W["ln2_b"][l], D, "lnB")
        x1 = [P.sbuf("x1", [128, D], F32) for _ in range(2)]
        y0 = [P.sbuf("y0", [128, D], F32) for _ in range(2)]
        y1 = [P.sbuf("y1", [128, D], F32) for _ in range(2)]
        f = [P.sbuf("f", [128, D], F32) for _ in range(2)]
        o = [P.sbuf("o", [128, D], F32) for _ in range(2)]
        YS = ["ys%d" % e_ for e_ in range(32)] + ["ysd"]

        def tile(tt, b):
            P = self.P
            rows = slice(tt * 128, (tt + 1) * 128)
            P.dma("sp", lambda e: e.dma_start(out=x1[b][:], in_=self.res1[rows, :]), "x1l%d" % b, reads=["res1_d"], writes=["x1_%d" % b])
            for k, yb in enumerate((y0, y1)):
                P.dma("pool", lambda e, k=k, yb=yb: e.indirect_dma_start(out=yb[b][:], out_offset=None, in_=self.ys[:, :],
                                                                        in_offset=bass.IndirectOffsetOnAxis(ap=self.gidx[:, tt, k:k + 1], axis=0)),
                      "gat%d_%d" % (k, b), reads=YS + ["gidx"], writes=["y%d_%d" % (k, b)])
            P.op("dve", lambda e: e.tensor_scalar(f[b][:], y0[b][:], self.gates[:, tt, 0:1], None, ALU.mult), reads=["y0_%d" % b, "gates"], writes=["f%d" % b])
            P.op("dve", lambda e: e.scalar_tensor_tensor(f[b][:], y1[b][:], self.gates[:, tt, 1:2], f[b][:], ALU.mult, ALU.add), reads=["y1_%d" % b, "gates", "f%d" % b], writes=["f%d" % b])
            P.op("dve", lambda e: e.scalar_tensor_tensor(f[b][:], x1[b][:], ALPHA, f[b][:], ALU.mult, ALU.add), reads=["x1_%d" % b, "f%d" % b], writes=["f%d" % b])
            self.layer_norm(f[b][:], o[b][:], G, B, ["f%d" % b], ["o%d" % b], sl=b)
            P.dma("act", lambda e: e.dma_start(out=dst[rows, :], in_=o[b][:]), "oo%d" % b, reads=["o%d" % b], writes=["dst_d"], partial=True)
        for tt in range(0, self.NT, 2):
            recs = []
            for sl in (0, 1):
                r_ = Rec()
                self.P = r_
                tile(tt + sl, sl)
                recs.append(r_)
            self.P = P
            interleave(P, recs)
        P.end()


def prep_shared(inp):
    f32 = np.float32
    sh = {}
    sh["even_w_in"] = np.ascontiguousarray(inp["even_w_in"], f32)
    sh["even_lam"] = np.ascontiguousarray(np.stack([inp["even_lam_q1"], inp["even_lam_k1"], inp["even_lam_q2"], inp["even_lam_k2"]], axis=1), f32)
    for k in ("even_subln_g", "even_q_norm_g", "even_w_uq", "even_kv_norm_g", "even_w_ukv", "even_w_o", "odd_w_qkv", "odd_w_o",
              "ln1_g", "ln1_b", "ln2_g", "ln2_b", "w_gate", "w_up", "w_down"):
        sh[k] = np.ascontiguousarray(inp[k], f32)
    sh["w_router"] = np.ascontiguousarray(np.concatenate([inp["w_router_group"], inp["w_router_expert"]], axis=2), f32)
    sh["b_router"] = np.ascontiguousarray(np.concatenate([inp["b_router_group"], inp["b_router_expert"]], axis=1), f32)
    idx = nbr_index_table()
    rb = np.asarray(inp["odd_rel_bias"], f32).reshape(2, 16, 15 * 31)
    ext = np.concatenate([rb, np.full((2, 16, 1), NEG, f32)], axis=2)
    sh["nbias"] = np.ascontiguousarray(ext[:, :, idx], f32)
    ra, rb_ = rope_host()
    sh["ropeA"], sh["ropeB"] = ra, rb_
    return sh


def kernel(**inputs):
    from concourse.bass_utils import run_bass_kernel_spmd
    n = 8
    nseq = 4
    inp = {k: np.asarray(v) for k, v in inputs.items()}
    sh = prep_shared(inp)
    x = np.ascontiguousarray(inp["x"], np.float32)
    mk = MK(nseq=nseq, C=768, layers=(0, 1, 2, 3), debug=False)
    in_maps = []
    for c in range(n):
        d = {k: v for k, v in sh.items() if k in mk.w}
        d["x"] = np.ascontiguousarray(x[c * nseq:(c + 1) * nseq].reshape(nseq * S, D))
        in_maps.append(d)
    res = run_bass_kernel_spmd(mk.nc, in_maps, core_ids=list(range(n)))
    out = np.concatenate([np.asarray(r["y"], np.float32).reshape(nseq, S, D) for r in res.results], axis=0)
    return out
```

```python
import numpy as np
import concourse.bass as bass
import concourse.mybir as mybir
from contextlib import ExitStack

F32 = mybir.dt.float32
BF16 = mybir.dt.bfloat16
I32 = mybir.dt.int32
U32 = mybir.dt.uint32
AF = mybir.ActivationFunctionType
ALU = mybir.AluOpType
AX = mybir.AxisListType

SEM_LIMIT = 30000
MAXOPS = 10 ** 9


class Prog:
    def __init__(self, nc):
        self.nc = nc
        self.gstack = ExitStack()
        self.pstack = None
        self.engs = {"pe": nc.tensor, "act": nc.scalar, "dve": nc.vector, "pool": nc.gpsimd, "sp": nc.sync}
        self._nsem = 0
        self.eng_sem, self.eng_cnt, self.grp_sem, self.grp_cnt = {}, {}, {}, {}
        self.waited = {e: {} for e in self.engs}
        self.prev_final = {}
        self.cur_final = {}
        self.need_bar = set()
        self.tot = dict(n_ops=0, n_wait=0)
        self._nm = 0
        self.W, self.R, self.G = {}, {}, {}

    def sbuf(self, name, shape, dt, persist=False):
        self._nm += 1
        st = self.gstack if (persist or self.pstack is None) else self.pstack
        return st.enter_context(self.nc.sbuf_tensor("%s_%d" % (name, self._nm), list(shape), dt))

    def psum(self, name, shape, dt):
        return self.gstack.enter_context(self.nc.psum_tensor(name, list(shape), dt))

    def new_sem(self):
        self._nsem += 1
        return self.gstack.enter_context(self.nc.semaphore("s%d" % self._nsem))

    def begin(self):
        self.pstack = ExitStack()
        self.prev_final = dict(self.cur_final)
        self.need_bar = set(self.engs)
        self.W, self.R, self.G = {}, {}, {}

    def end(self):
        self.pstack.close()
        self.pstack = None

    def _do(self, engn, fn, reads, writes, partial, dma, group, wfull=()):
        if self.tot["n_ops"] >= MAXOPS:
            return
        W, R, G = self.W, self.R, self.G
        deps = []
        for k in reads:
            deps.extend(W.get(k, ()))
            if k.startswith("pb"):
                deps.extend(r for r in R.get(k, ()) if r[2] != engn)
        newgen = []
        for k in wfull:
            d = list(W.get(k, ())) + list(R.get(k, ()))
            deps.extend(d)
            newgen.append((k, d))
        for k in writes:
            if partial and k in W and not R.get(k):
                deps.extend(G[k])
            else:
                d = list(W.get(k, ())) + list(R.get(k, ()))
                deps.extend(d)
                newgen.append((k, d))
        eng = self.engs[engn]
        w = self.waited[engn]
        need = {}
        if engn in self.need_bar:
            self.need_bar.discard(engn)
            for num, (s, v) in self.prev_final.items():
                need[num] = (s, v)
        for (s, v, de, ddma) in deps:
            if engn == "pe" and not dma and de == "pe" and not ddma:
                continue
            if need.get(s.num, (None, 0))[1] < v:
                need[s.num] = (s, v)
        for num, (s, v) in need.items():
            if w.get(num, 0) < v:
                eng.wait_ge(s, v)
                w[num] = v
                self.tot["n_wait"] += 1
        ins = fn(eng)
        if dma:
            g = group
            if g not in self.grp_sem or self.grp_cnt[g] + 16 > SEM_LIMIT:
                self.grp_sem[g] = self.new_sem()
                self.grp_cnt[g] = 0
            self.grp_cnt[g] += 16
            sem, val = self.grp_sem[g], self.grp_cnt[g]
            ins.then_inc(sem, 16)
        else:
            e = engn
            if e not in self.eng_sem or self.eng_cnt[e] + 1 > SEM_LIMIT:
                self.eng_sem[e] = self.new_sem()
                self.eng_cnt[e] = 0
            self.eng_cnt[e] += 1
            sem, val = self.eng_sem[e], self.eng_cnt[e]
            ins.then_inc(sem, 1)
        self.cur_final[sem.num] = (sem, val)
        rec = (sem, val, engn, dma)
        for k, d in newgen:
            G[k] = d
            W[k] = []
            R[k] = []
        for k in tuple(writes) + tuple(wfull):
            W[k].append(rec)
        for k in reads:
            R.setdefault(k, []).append(rec)
        self.tot["n_ops"] += 1

    def op(self, eng, fn, reads=(), writes=(), partial=False):
        self._do(eng, fn, reads, writes, partial, False, None)

    def dma(self, eng, fn, group, reads=(), writes=(), partial=False, wfull=()):
        self._do(eng, fn, reads, writes, partial, True, group, wfull)

    def finish(self):
        sp = self.engs["sp"]
        w = self.waited["sp"]
        for num, (s, v) in self.cur_final.items():
            if w.get(num, 0) < v:
                sp.wait_ge(s, v)
                w[num] = v
        self.tot["n_sem"] = self._nsem
        return self.tot


class Rec:
    def __init__(self):
        self.l = []

    def op(self, *a, **k):
        self.l.append(("op", a, k))

    def dma(self, *a, **k):
        self.l.append(("dma", a, k))


def interleave(P, recs):
    from itertools import zip_longest
    for group in zip_longest(*[r.l for r in recs]):
        for item in group:
            if item is not None and item[0] != "mark":
                getattr(P, item[0])(*item[1], **item[2])


import math
import os
DVX = 0
import numpy as np

D = 1024
S = 2048
T = 16
DEPTH = 4
ALPHA = (2 * DEPTH) ** 0.25
NEG = -30000.0
NPAT = 21


def nbr_patterns():
    pats = {}
    p = 5
    out = []
    for qt in range(16):
        if 2 <= qt <= 13:
            out.append([(qt - 2 + i, i) for i in range(5)])
        else:
            kts = [0, 1, 2, 3] if qt < 2 else [12, 13, 14, 15]
            lst = []
            for kt in kts:
                lst.append((kt, p))
                p += 1
            out.append(lst)
    assert p == NPAT
    return out


def nbr_index_table():
    pl = nbr_patterns()
    idx = np.full((128, NPAT, 128), 15 * 31, np.int64)
    done = set()
    for qt in range(16):
        for kt, p in pl[qt]:
            if p in done:
                continue
            done.add(p)
            for q in range(128):
                r, c = qt * 2 + q // 64, q % 64
                r0 = min(max(r - 4, 0), 24)
                c0 = min(max(c - 8, 0), 48)
                for k in range(128):
                    rk, ck = kt * 2 + k // 64, k % 64
                    if r0 <= rk < r0 + 8 and c0 <= ck < c0 + 16:
                        idx[k, p, q] = (rk - r + 7) * 31 + (ck - c + 15)
    return idx


def rope_host():
    def tab(rot):
        inv = (np.float32(500000.0) ** (-np.arange(0, rot, 2, dtype=np.float32) / np.float32(rot))).astype(np.float32)
        ang = (np.arange(S, dtype=np.float32)[:, None] * inv[None, :]).astype(np.float32)
        cos, sin = np.cos(ang).astype(np.float32), np.sin(ang).astype(np.float32)
        t = np.concatenate([cos, cos, -sin, sin], axis=1)
        return np.ascontiguousarray(t.reshape(T, 128, 2 * rot).transpose(1, 0, 2))
    return tab(16), tab(32)


def multi(fns):
    def f(e):
        ins = None
        for g in fns:
            ins = g(e)
        return ins
    return f


class MK:
    def __init__(self, nseq=4, C=768, layers=(0, 1, 2, 3), debug=False, phases="ABCOEM"):
        self.nseq, self.C, self.layers, self.debug = nseq, C, tuple(layers), debug
        self.phases = phases
        self.NT = nseq * T
        self.NTOK = nseq * S
        self.NSLOT = 32 * C
        nc = self.nc = bass.Bass("TRN2", target_bir_lowering=False)
        P = self.P = Prog(nc)
        NTOK = self.NTOK

        def din(name, shape, dt=F32):
            return nc.dram_tensor(name, list(shape), dt, kind="ExternalInput").ap()

        def dint(name, shape, dt=F32):
            return nc.dram_tensor(name, list(shape), dt, kind="Internal").ap()
        self.x = din("x", [NTOK, D])
        self.w = {}
        for nm, shp in [("even_w_in", [2, D, 1952]), ("even_lam", [2, 4, 64]), ("even_subln_g", [2, 128]),
                        ("even_q_norm_g", [2, 256]), ("even_w_uq", [2, 256, 768]), ("even_kv_norm_g", [2, 128]),
                        ("even_w_ukv", [2, 128, 1024]), ("even_w_o", [2, D, D]), ("odd_w_qkv", [2, D, 3072]),
                        ("nbias", [2, 16, 128, NPAT, 128]), ("odd_w_o", [2, D, D]),
                        ("ln1_g", [4, D]), ("ln1_b", [4, D]), ("ln2_g", [4, D]), ("ln2_b", [4, D]),
                        ("w_router", [4, D, 36]), ("b_router", [4, 36]),
                        ("w_gate", [4, 32, D, 512]), ("w_up", [4, 32, D, 512]), ("w_down", [4, 32, 512, D]),
                        ("ropeA", [128, 16, 32]), ("ropeB", [128, 16, 64])]:
            if nm in ("w_gate", "w_up", "w_down") and "E" not in self.phases:
                continue
            if nm in ("odd_w_qkv", "nbias") and ("C" not in self.phases or all(l % 2 == 0 for l in self.layers)):
                continue
            self.w[nm] = din(nm, shp)
        self.y = nc.dram_tensor("y", [NTOK, D], F32, kind="ExternalOutput").ap()
        self.res1 = dint("res1", [NTOK, D]) if not debug else nc.dram_tensor("res1", [NTOK, D], F32, kind="ExternalOutput").ap()
        self.resA = dint("resA", [NTOK, D])
        self.ao = dint("ao", [NTOK, D], BF16) if not debug else nc.dram_tensor("ao", [NTOK, D], BF16, kind="ExternalOutput").ap()
        self.xs = dint("xs", [self.NSLOT, D], BF16)
        self.ys = dint("ys", [self.NSLOT + 128, D])
        self.PS = P.psum("PS", [128, 4096], F32)
        self.bc_reg = nc.gpsimd.alloc_register("bcr")
        nc.gpsimd.reg_mov(self.bc_reg, self.NSLOT - 1)
        self.consts()
        for l in self.layers:
            src = self.x if l == self.layers[0] else self.resA
            if l % 2 == 0:
                self.even_attn(l, src)
            elif "C" in self.phases:
                self.odd_attn(l, src)
            if "O" in self.phases:
                self.oproj(l, src)
            if "E" in self.phases:
                self.experts(l)
            dst = self.y if l == self.layers[-1] else self.resA
            if "M" in self.phases:
                self.combine(l, dst)
        self.stats = P.finish()

    def bank(self, b, n=1):
        return self.PS[:, b * 512:(b + n) * 512]

    def bankbf(self, b):
        return self.PS[:, b * 512:(b + 1) * 512].bitcast(BF16)

    def consts(self):
        P = self.P
        P.begin()
        self.ident = P.sbuf("ident", [128, 128], BF16, persist=True)
        self.identf = P.sbuf("identf", [128, 128], F32, persist=True)
        self.U = P.sbuf("U", [128, 128], BF16, persist=True)
        self.ones = P.sbuf("ones", [128, 128], BF16, persist=True)
        self.iota32 = P.sbuf("iota32", [128, 32], F32, persist=True)
        self.eC = P.sbuf("eC", [128, 32], F32, persist=True)
        self.ropeA = P.sbuf("ropeA", [128, 16, 32], F32, persist=True)
        self.ropeB = P.sbuf("ropeB", [128, 16, 64], F32, persist=True)
        self.gidx = P.sbuf("gidx", [128, self.NT, 2], I32, persist=True)
        self.gates = P.sbuf("gates", [128, self.NT, 2], F32, persist=True)
        self.zero = P.sbuf("zero", [128, D], F32, persist=True)
        iot = P.sbuf("iot", [128, 128], F32)
        iop = P.sbuf("iop", [128, 1], F32)
        P.op("pool", lambda e: e.iota(iot[:], [[1, 128]], base=0, channel_multiplier=0, allow_small_or_imprecise_dtypes=True), writes=["iot"])
        P.op("pool", lambda e: e.iota(iop[:], [[0, 1]], base=0, channel_multiplier=1, allow_small_or_imprecise_dtypes=True), writes=["iop"])
        P.op("dve", lambda e: e.tensor_scalar(self.ident[:], iot[:], iop[:, 0:1], None, ALU.is_equal), reads=["iot", "iop"], writes=["ident"])
        P.op("dve", lambda e: e.tensor_scalar(self.identf[:], iot[:], iop[:, 0:1], None, ALU.is_equal), reads=["iot", "iop"], writes=["identf"])
        P.op("dve", lambda e: e.tensor_scalar(self.U[:], iot[:], iop[:, 0:1], None, ALU.is_gt), reads=["iot", "iop"], writes=["U"])
        P.op("dve", lambda e: e.memset(self.ones[:], 1.0), writes=["ones"])
        P.op("dve", lambda e: e.tensor_copy(self.iota32[:], iot[:, 0:32]), reads=["iot"], writes=["iota32"])
        P.op("dve", lambda e: e.tensor_scalar(self.eC[:], iot[:, 0:32], float(self.C), None, ALU.mult), reads=["iot"], writes=["eC"])
        P.op("dve", lambda e: e.memset(self.zero[:], 0.0), writes=["zero"])
        P.dma("sp", lambda e: e.dma_start(out=self.ropeA[:], in_=self.w["ropeA"]), "c0", writes=["ropeA"])
        P.dma("sp", lambda e: e.dma_start(out=self.ropeB[:], in_=self.w["ropeB"]), "c0", writes=["ropeB"])
        P.dma("sp", lambda e: e.dma_start(out=self.ys[self.NSLOT:self.NSLOT + 128, :], in_=self.zero[:]), "c0", reads=["zero"], writes=["ysd"])
        zb = P.sbuf("zb", [128, 8, D], BF16)
        P.op("pool", lambda e: e.memset(zb[:], 0.0), writes=["zb"])
        for i in range(self.NSLOT // 1024):
            P.dma("sp", lambda e, i=i: e.dma_start(out=self.xs[i * 1024:(i + 1) * 1024, :].rearrange("(p n) d -> p n d", n=8), in_=zb[:]), "c0", reads=["zb"], writes=["xsz"], partial=True)
        P.end()

    def rstd(self, out, in_, scale, eps, rk, wk):
        P = self.P
        P.op("act", lambda e: e.activation(out, in_, AF.Ln, bias=self.epst(eps), scale=scale), reads=rk, writes=wk)
        P.op("act", lambda e: e.activation(out, out, AF.Exp, scale=-0.5), reads=wk, writes=wk)

    def epst(self, eps):
        return self._eps[eps][:, 0:1]

    def mk_eps(self):
        P = self.P
        self._eps = {}
        for eps in (1e-5, 1e-6):
            t = P.sbuf("eps", [128, 1], F32)
            self._eps[eps] = t
            P.op("dve", lambda e, t=t, eps=eps: e.memset(t[:], eps), writes=["eps%g" % eps])

    def bcast_load(self, name, src_row, n, key, dt=F32):
        P = self.P
        t = P.sbuf(name, [128, n], dt)
        P.dma("sp", lambda e: e.dma_start(out=t[:], in_=src_row.unsqueeze(0).broadcast_to([128, n])), "bc_" + key, writes=[key])
        return t

    def load_w_bf16(self, dst, src2d, kchunks, ncols, key, c0=0):
        P = self.P
        for k in range(kchunks):
            P.dma("pool", lambda e, k=k: e.dma_start(out=dst[:, k, c0:c0 + ncols], in_=src2d[k * 128:(k + 1) * 128, :]),
                  "w_" + key, writes=[key], partial=True)

    def x_front(self, src, tt, b, bufs):
        P = self.P
        xt, xb, xT, pbank = bufs
        P.dma("sp", lambda e: e.dma_start(out=xt[b][:], in_=src[tt * 128:(tt + 1) * 128, :]), "xt%d" % b, writes=["xt%d" % b])
        P.op("act", lambda e: e.activation(xb[b][:], xt[b][:], AF.Copy), reads=["xt%d" % b], writes=["xb%d" % b])
        pT = self.bankbf(pbank).rearrange("p (c t) -> p c t", t=128)
        P.op("pe", multi([lambda e, c=c: e.transpose(pT[:, c, :], xb[b][:, c * 128:(c + 1) * 128], self.ident[:]) for c in range(8)]),
             reads=["xb%d" % b, "ident"], writes=["pb%d" % pbank])
        P.op("dve", lambda e: e.tensor_copy(xT[b][:], pT), reads=["pb%d" % pbank], writes=["xT%d" % b])

    def rope(self, src3, dst3, tab, r0, half, ng, rk, wk, tmpk, temps=None):
        P = self.P
        t1, t2 = temps if temps is not None else self._ropet
        h2 = 2 * half
        a = t1[:, 0:ng, 0:h2]
        b_ = t2[:, 0:ng, 0:h2]
        cc = tab[:, 0:h2].unsqueeze(1).to_broadcast([128, ng, h2])
        ns = tab[:, h2:h2 + half].unsqueeze(1).to_broadcast([128, ng, half])
        ps = tab[:, h2 + half:h2 + 2 * half].unsqueeze(1).to_broadcast([128, ng, half])
        P.op("dve", lambda e: e.tensor_tensor(a, src3[:, :, r0:r0 + h2], cc, ALU.mult), reads=rk, writes=[tmpk + "1"])
        P.op("dve", lambda e: e.tensor_tensor(b_[:, :, 0:half], src3[:, :, r0 + half:r0 + h2], ns, ALU.mult), reads=rk, writes=[tmpk + "2"])
        P.op("dve", lambda e: e.tensor_tensor(b_[:, :, half:h2], src3[:, :, r0:r0 + half], ps, ALU.mult), reads=rk, writes=[tmpk + "2"], partial=True)
        P.op("pool", lambda e: e.tensor_tensor(dst3[:, :, r0:r0 + h2], a, b_, ALU.add), reads=[tmpk + "1", tmpk + "2"], writes=wk)

    def layer_norm(self, r, out, G, B, rk, wk, sl=0):
        P = self.P
        st, mv, rs = self._lnt[sl]
        kst, kmv, krs = "lnst%d" % sl, "lnmv%d" % sl, "lnrs%d" % sl
        P.op("dve", lambda e: e.bn_stats(st[:, 0, :], r[:, 0:512]), reads=rk, writes=[kst])
        P.op("dve", lambda e: e.bn_stats(st[:, 1, :], r[:, 512:1024]), reads=rk, writes=[kst], partial=True)
        P.op("dve", lambda e: e.bn_aggr(mv[:], st[:]), reads=[kst], writes=[kmv])
        self.rstd(rs[:], mv[:, 1:2], 1.0, 1e-5, [kmv, "eps1e-05"], [krs])
        P.op("dve", lambda e: e.tensor_scalar(out, r, mv[:, 0:1], rs[:, 0:1], ALU.subtract, ALU.mult), reads=rk + [kmv, krs], writes=wk)
        P.op("pool", lambda e: e.tensor_tensor(out, out, G[:], ALU.mult), reads=wk + ["lnG"], writes=wk)
        P.op("pool", lambda e: e.tensor_tensor(out, out, B[:], ALU.add), reads=wk + ["lnB"], writes=wk)

    def mk_lnt(self):
        P = self.P
        self._lnt = [(P.sbuf("lnst", [128, 2, 6], F32), P.sbuf("lnmv", [128, 2], F32), P.sbuf("lnrs", [128, 1], F32)) for _ in range(2)]

    def qkv_tile(self, src, tt, t, sl, wt, wkey, QT, KT, V, vd, rope_tab, B):
        P = self.P
        xt, xb, xT, qkb, rts = B
        base = 4 * sl
        self.x_front(src, tt, sl, (xt, xb, xT, base + 3))
        for n in range(3):
            P.op("pe", multi([lambda e, n=n, c=c: e.matmul(self.bank(base + n), xT[sl][:, c, :], wt[:, c, n * 512:(n + 1) * 512], start=(c == 0), stop=(c == 7)) for c in range(8)]),
                 reads=["xT%d" % sl, wkey], writes=["pb%d" % (base + n)])
        ph = self.bank(base, 3)
        kq = "qkb%d" % sl
        pk = ["pb%d" % base, "pb%d" % (base + 1)]
        P.op("act", lambda e: e.activation(qkb[sl][:], ph[:, 0:1024], AF.Copy), reads=pk, writes=[kq])
        if rope_tab is not None:
            src3 = ph[:, 0:1024].rearrange("p (g d) -> p g d", d=64)
            dst3 = qkb[sl][:].rearrange("p (g d) -> p g d", d=64)
            self.rope(src3, dst3, rope_tab, 0, 8, 16, pk + ["ropeA"], [kq], "rt%d" % sl, rts[sl])
        pT2 = self.bankbf(base + 3).rearrange("p (c t) -> p c t", t=128)
        P.op("pe", multi([lambda e, c=c: e.transpose(pT2[:, c, :], qkb[sl][:, c * 128:(c + 1) * 128], self.ident[:]) for c in range(8)]),
             reads=[kq, "ident"], writes=["pb%d" % (base + 3)])
        P.op("dve", lambda e: e.tensor_copy(QT[0:64, 0:8:2, t * 128:(t + 1) * 128], pT2[0:64, 0:4, :]), reads=["pb%d" % (base + 3), "QTz"], writes=["QT"], partial=True)
        P.op("dve", lambda e: e.tensor_copy(QT[64:128, 1:8:2, t * 128:(t + 1) * 128], pT2[64:128, 0:4, :]), reads=["pb%d" % (base + 3), "QTz"], writes=["QT"], partial=True)
        P.op("act", lambda e: e.activation(KT[:, :, t * 128:(t + 1) * 128], pT2[:, 4:8, :], AF.Copy), reads=["pb%d" % (base + 3)], writes=["KT"], partial=True)
        P.op("act", lambda e: e.activation(V[:, t, :, 0:vd], self.bank(base + 2).rearrange("p (h d) -> p h d", d=vd), AF.Copy),
             reads=["pb%d" % (base + 2), "Vones"], writes=["V"], partial=True)

    def qkv_seq(self, src, s, wt, wkey, QT, KT, V, vd, rope, B):
        P = self.P
        for t in range(0, T, 2):
            recs = []
            for sl in (0, 1):
                r = Rec()
                self.P = r
                self.qkv_tile(src, s * T + t + sl, t + sl, sl, wt, wkey, QT, KT, V, vd, (self.ropeA[:, t + sl, :] if rope else None), B)
                recs.append(r)
            self.P = P
            interleave(P, recs)

    def dense_attn(self, maps, QT, KT, V, dk, dv, scale, epilogue):
        P = self.P
        E = self._E
        steps = [(qc, mi, kp) for qc in range(4) for mi in range(len(maps)) for kp in range(8)]
        ps_all = self.PS[:, 2 * 512:6 * 512].rearrange("p (b c) -> p b c", c=512)
        pairs = [0, 6]

        def qk(i):
            qc, mi, kp = steps[i]
            kblk, qblk, r0, vi = maps[mi]
            b0 = pairs[i % 2]
            P.op("pe", multi([lambda e, j=j: e.matmul(self.bank(b0 + j), KT[r0:r0 + dk, kblk, (2 * kp + j) * 128:(2 * kp + j + 1) * 128],
                                                      QT[r0:r0 + dk, qblk, qc * 512:(qc + 1) * 512], start=True, stop=True) for j in range(2)]),
                 reads=["QT", "KT"], writes=["pb%d" % b0, "pb%d" % (b0 + 1)])
            eb = i % 3
            P.op("act", lambda e: e.activation(E[eb][:], self.bank(b0, 2), AF.Exp, scale=scale), reads=["pb%d" % b0, "pb%d" % (b0 + 1)], writes=["E%d" % eb])

        def pv(i):
            qc, mi, kp = steps[i]
            kblk, qblk, r0, vi = maps[mi]
            eb = i % 3
            P.op("pe", multi([lambda e, qi=qi, j=j: e.matmul(self.bank(2 + qi)[:, 0:dv + 1 - DVX], E[eb][:, j * 512 + qi * 128:j * 512 + (qi + 1) * 128],
                                                             V[:, 2 * kp + j, vi, 0:dv + 1 - DVX], start=(kp == 0 and j == 0), stop=(kp == 7 and j == 1))
                              for j in range(2) for qi in range(4)]),
                 reads=["E%d" % eb, "V"], writes=["pb2", "pb3", "pb4", "pb5"], partial=(kp != 0))
            if kp == 7 and not None:
                epilogue(qc, mi, ps_all)
        n = len(steps)
        if None:
            return
        for i in range(n + 1):
            if i < n:
                qk(i)
            if i >= 1:
                pv(i - 1)

    def even_attn(self, l, src):
        P = self.P
        j = l // 2
        W = self.w
        lam_init = 0.8 - 0.6 * math.exp(-0.3 * l)
        if "A" not in self.phases:
            return self.even_attn_B(l, src)
        P.begin()
        self.mk_eps()
        wt = P.sbuf("wA", [128, 8, 1536], BF16)
        self.load_w_bf16(wt, W["even_w_in"][j][:, 0:1536], 8, 1536, "wA")
        xt = [P.sbuf("xt", [128, D], F32) for _ in range(2)]
        xb = [P.sbuf("xb", [128, D], BF16) for _ in range(2)]
        xT = [P.sbuf("xT", [128, 8, 128], BF16) for _ in range(2)]
        QT = P.sbuf("QT", [128, 8, S], BF16)
        KT = P.sbuf("KT", [128, 4, S], BF16)
        V = P.sbuf("V", [128, 16, 4, 129], BF16)
        P.op("pool", lambda e: e.memset(QT[:], 0.0), writes=["QTz"])
        qkb = [P.sbuf("qkb", [128, 1024], BF16) for _ in range(2)]
        rts = [(P.sbuf("rt1", [128, 16, 32], F32), P.sbuf("rt2", [128, 16, 32], F32)) for _ in range(2)]
        self._E = [P.sbuf("E", [128, 1024], BF16) for _ in range(3)]
        Om = [P.sbuf("Om", [128, 4, 128], F32) for _ in range(2)]
        rec = P.sbuf("rec", [128, 4], F32)
        dd = P.sbuf("dd", [128, 4, 128], F32)
        sq = P.sbuf("sq", [128, 4, 128], F32)
        ss = P.sbuf("ss", [128, 4], F32)
        AO = [P.sbuf("AO", [128, 4, 512], BF16) for _ in range(2)]
        lamv = P.sbuf("lamv", [128, 4, 64], F32)
        P.dma("sp", lambda e: e.dma_start(out=lamv[:], in_=W["even_lam"][j].unsqueeze(0).broadcast_to([128, 4, 64])), "bc_lamv", writes=["lamv"])
        lp = P.sbuf("lp", [128, 2, 64], F32)
        ls = P.sbuf("ls", [128, 2], F32)
        nlam = P.sbuf("nlam", [128, 1], F32)
        P.op("dve", lambda e: e.tensor_tensor(lp[:], lamv[:, 0:4:2, :], lamv[:, 1:4:2, :], ALU.mult), reads=["lamv"], writes=["lp"])
        P.op("dve", lambda e: e.tensor_reduce(ls[:], lp[:], AX.X, ALU.add), reads=["lp"], writes=["ls"])
        P.op("act", lambda e: e.activation(ls[:], ls[:], AF.Exp), reads=["ls"], writes=["ls"])
        P.op("dve", lambda e: e.tensor_tensor(nlam[:], ls[:, 1:2], ls[:, 0:1], ALU.subtract), reads=["ls"], writes=["nlam"])
        P.op("dve", lambda e: e.tensor_scalar(nlam[:], nlam[:], -lam_init, None, ALU.add), reads=["nlam"], writes=["nlam"])
        gsub = self.bcast_load("gsub", W["even_subln_g"][j], 128, "gsub")
        P.op("dve", lambda e: e.tensor_scalar(gsub[:], gsub[:], 1.0 - lam_init, None, ALU.mult), reads=["gsub"], writes=["gsub"])
        P.op("dve", lambda e: e.memset(V[:, :, :, 128:129], 1.0), writes=["Vones"])
        for s in range(self.nseq):
            self.qkv_seq(src, s, wt, "wA", QT, KT, V, 128, True, (xt, xb, xT, qkb, rts))

            def epi(qc, mi, ps_all, s=s):
                h, m = mi // 2, mi % 2
                ab = (qc % 2)
                P.op("dve", lambda e: e.reciprocal(rec[:], ps_all[:, :, 128]), reads=["pb2", "pb3", "pb4", "pb5"], writes=["rec"])
                P.op("dve", lambda e: e.tensor_tensor(Om[m][:], ps_all[:, :, 0:128], rec[:].unsqueeze(2).to_broadcast([128, 4, 128]), ALU.mult),
                     reads=["pb2", "pb3", "pb4", "pb5", "rec"], writes=["Om%d" % m])
                if m == 1:
                    P.op("dve", lambda e: e.scalar_tensor_tensor(dd[:], Om[1][:], nlam[:, 0:1], Om[0][:], ALU.mult, ALU.add), reads=["Om0", "Om1", "nlam"], writes=["dd"])
                    P.op("pool", lambda e: e.tensor_tensor(sq[:], dd[:], dd[:], ALU.mult), reads=["dd"], writes=["sq"])
                    P.op("dve", lambda e: e.tensor_reduce(ss[:], sq[:], AX.X, ALU.add), reads=["sq"], writes=["ss"])
                    self.rstd(ss[:], ss[:], 1.0 / 128, 1e-6, ["ss", "eps1e-06"], ["ss"])
                    P.op("dve", lambda e: e.tensor_tensor(dd[:], dd[:], ss[:].unsqueeze(2).to_broadcast([128, 4, 128]), ALU.mult), reads=["dd", "ss"], writes=["dd"])
                    P.op("pool", lambda e: e.tensor_tensor(AO[ab][:, :, h * 128:(h + 1) * 128], dd[:], gsub[:].unsqueeze(1).to_broadcast([128, 4, 128]), ALU.mult),
                         reads=["dd", "gsub"], writes=["AO%d" % ab], partial=True)
                    if h == 3:
                        r0 = s * S + qc * 512
                        P.dma("pool", lambda e: e.dma_start(out=self.ao[r0:r0 + 512, 0:512].rearrange("(q p) n -> p q n", p=128), in_=AO[ab][:]),
                              "ao%d" % ab, reads=["AO%d" % ab], writes=["ao_d"], partial=True)
            maps = [(g // 2, g, 0, g // 2) for g in range(8)]
            self.dense_attn(maps, QT, KT, V, 128, 128, 0.125, epi)
        P.end()
        self.even_attn_B(l, src)

    def even_attn_B(self, l, src):
        P = self.P
        j = l // 2
        W = self.w
        if "B" not in self.phases:
            return
        P.begin()
        self.mk_eps()
        wt = P.sbuf("wB", [128, 8, 416], BF16)
        self.load_w_bf16(wt, W["even_w_in"][j][:, 1536:1952], 8, 416, "wB")
        wuq = P.sbuf("wuq", [128, 2, 768], BF16)
        self.load_w_bf16(wuq, W["even_w_uq"][j], 2, 768, "wuq")
        wukv = P.sbuf("wukv", [128, 1, 1024], BF16)
        self.load_w_bf16(wukv, W["even_w_ukv"][j], 1, 1024, "wukv")
        qg = self.bcast_load("qg", W["even_q_norm_g"][j], 256, "qg")
        kvg = self.bcast_load("kvg", W["even_kv_norm_g"][j], 128, "kvg")
        xt = [P.sbuf("xt", [128, D], F32) for _ in range(2)]
        xb = [P.sbuf("xb", [128, D], BF16) for _ in range(2)]
        xT = [P.sbuf("xT", [128, 8, 128], BF16) for _ in range(2)]
        QT = P.sbuf("QT", [128, 8, S], BF16)
        KT = P.sbuf("KT", [128, 8, S], BF16)
        V = P.sbuf("V", [128, 16, 8, 65], BF16)
        rts = [(P.sbuf("rt1", [128, 8, 32], F32), P.sbuf("rt2", [128, 8, 32], F32)) for _ in range(2)]
        self._E = [P.sbuf("E", [128, 1024], BF16) for _ in range(3)]

        def two(name, shape, dt):
            return [P.sbuf(name, shape, dt) for _ in range(2)]
        junk, ssq, cn, cT = two("junk", [128, 384], F32), two("ssq", [128, 2], F32), two("cn", [128, 384], BF16), two("cT", [128, 3, 128], BF16)
        qb, kcat, krr = two("qb", [128, 8, 96], BF16), two("kcat", [128, 8, 96], BF16), two("krr", [128, 1, 32], BF16)
        rec = P.sbuf("rec", [128, 4], F32)
        AO = [P.sbuf("AO", [128, 4, 512], BF16) for _ in range(2)]
        P.op("dve", lambda e: e.memset(V[:, :, :, 64:65], 1.0), writes=["Vones"])

        def b_tile(tt, t, sl):
            P = self.P
            base = 4 * sl
            b0, b1, b2 = base, base + 1, base + 2
            kb = ["pb%d" % (base + i) for i in range(4)]

            def K(n):
                return "%s_%d" % (n, sl)
            self.x_front(src, tt, sl, (xt, xb, xT, b0))
            hB = self.bank(b1)
            P.op("pe", multi([lambda e, c=c: e.matmul(hB[:, 0:416], xT[sl][:, c, :], wt[:, c, :], start=(c == 0), stop=(c == 7)) for c in range(8)]),
                 reads=["xT%d" % sl, "wB"], writes=[kb[1]])
            P.op("act", lambda e: e.activation(junk[sl][:, 0:256], hB[:, 0:256], AF.Square, accum_out=ssq[sl][:, 0:1]), reads=[kb[1]], writes=[K("ssq0"), K("junk0")])
            P.op("act", lambda e: e.activation(junk[sl][:, 256:384], hB[:, 256:384], AF.Square, accum_out=ssq[sl][:, 1:2]), reads=[kb[1]], writes=[K("ssq1"), K("junk1")])
            self.rstd(ssq[sl][:, 0:1], ssq[sl][:, 0:1], 1.0 / 256, 1e-6, [K("ssq0"), "eps1e-06"], [K("ssq0")])
            self.rstd(ssq[sl][:, 1:2], ssq[sl][:, 1:2], 1.0 / 128, 1e-6, [K("ssq1"), "eps1e-06"], [K("ssq1")])
            P.op("dve", lambda e: e.scalar_tensor_tensor(cn[sl][:, 0:256], hB[:, 0:256], ssq[sl][:, 0:1], qg[:], ALU.mult, ALU.mult), reads=[kb[1], K("ssq0"), "qg"], writes=[K("cn")])
            P.op("dve", lambda e: e.scalar_tensor_tensor(cn[sl][:, 256:384], hB[:, 256:384], ssq[sl][:, 1:2], kvg[:], ALU.mult, ALU.mult), reads=[kb[1], K("ssq1"), "kvg"], writes=[K("cn")], partial=True)
            P.op("act", lambda e: e.activation(krr[sl][:, 0, :], hB[:, 384:416], AF.Copy), reads=[kb[1]], writes=[K("krr")])
            self.rope(hB[:, 384:416].unsqueeze(1), krr[sl][:], self.ropeB[:, t, :], 0, 16, 1, [kb[1], "ropeB"], [K("krr")], K("rt"), rts[sl])
            P.op("pool", lambda e: e.tensor_copy(kcat[sl][:, :, 64:96], krr[sl][:].to_broadcast([128, 8, 32])), reads=[K("krr")], writes=[K("kcatr")])
            pTc = self.bankbf(b0).rearrange("p (c t) -> p c t", t=128)
            P.op("pe", multi([lambda e, c=c: e.transpose(pTc[:, c, :], cn[sl][:, c * 128:(c + 1) * 128], self.ident[:]) for c in range(3)]), reads=[K("cn"), "ident"], writes=[kb[0]])
            P.op("dve", lambda e: e.tensor_copy(cT[sl][:], pTc[:, 0:3, :]), reads=[kb[0]], writes=[K("cT")])
            q0 = b2 * 512
            P.op("pe", multi([lambda e, c=c, c0=c0, n=n: e.matmul(self.PS[:, q0 + c0:q0 + c0 + n], cT[sl][:, c, :], wuq[:, c, c0:c0 + n], start=(c == 0), stop=(c == 1))
                              for (c0, n) in ((0, 512), (512, 256)) for c in range(2)]), reads=[K("cT"), "wuq"], writes=[kb[2], kb[3]])
            q3 = self.PS[:, q0:q0 + 768].rearrange("p (h d) -> p h d", d=96)
            P.op("act", lambda e: e.activation(qb[sl][:], q3, AF.Copy), reads=[kb[2], kb[3]], writes=[K("qb")])
            self.rope(q3, qb[sl][:], self.ropeB[:, t, :], 64, 16, 8, [kb[2], kb[3], "ropeB"], [K("qb")], K("rt"), rts[sl])
            P.op("pe", multi([lambda e, n=n: e.matmul(self.bank(b2 + n), cT[sl][:, 2, :], wukv[:, 0, n * 512:(n + 1) * 512], start=True, stop=True) for n in range(2)]),
                 reads=[K("cT"), "wukv"], writes=[kb[2], kb[3]])
            kv3 = self.PS[:, q0:q0 + 1024].rearrange("p (h d) -> p h d", d=128)
            P.op("act", lambda e: e.activation(kcat[sl][:, :, 0:64], kv3[:, :, 0:64], AF.Copy), reads=[kb[2], kb[3]], writes=[K("kcatn")])
            P.op("dve", lambda e: e.tensor_copy(V[:, t, :, 0:64], kv3[:, :, 64:128]), reads=[kb[2], kb[3], "Vones"], writes=["V"], partial=True)
            pTq = self.bankbf(b0).rearrange("p (c t) -> p c t", t=128)
            pTk = self.bankbf(b1).rearrange("p (c t) -> p c t", t=128)
            P.op("pe", multi([lambda e, h=h: e.transpose(pTq[0:96, h, :], qb[sl][:, h, :], self.ident[:]) for h in range(8)]), reads=[K("qb"), "ident"], writes=[kb[0]])
            P.op("dve", lambda e: e.tensor_copy(QT[0:96, :, t * 128:(t + 1) * 128], pTq[0:96, :, :]), reads=[kb[0]], writes=["QT"], partial=True)
            P.op("pe", multi([lambda e, h=h: e.transpose(pTk[0:96, h, :], kcat[sl][:, h, :], self.ident[:]) for h in range(8)]), reads=[K("kcatr"), K("kcatn"), "ident"], writes=[kb[1]])
            P.op("act", lambda e: e.activation(KT[0:96, :, t * 128:(t + 1) * 128], pTk[0:96, :, :], AF.Copy), reads=[kb[1]], writes=["KT"], partial=True)
        for s in range(self.nseq):
            for t in range(0, T, 2):
                recs = []
                for sl in (0, 1):
                    r_ = Rec()
                    self.P = r_
                    b_tile(s * T + t + sl, t + sl, sl)
                    recs.append(r_)
                self.P = P
                interleave(P, recs)

            def epi(qc, mi, ps_all, s=s):
                h = mi
                ab = qc % 2
                P.op("dve", lambda e: e.reciprocal(rec[:], ps_all[:, :, 64]), reads=["pb2", "pb3", "pb4", "pb5"], writes=["rec"])
                P.op("dve", lambda e: e.tensor_tensor(AO[ab][:, :, h * 64:(h + 1) * 64], ps_all[:, :, 0:64], rec[:].unsqueeze(2).to_broadcast([128, 4, 64]), ALU.mult),
                     reads=["pb2", "pb3", "pb4", "pb5", "rec"], writes=["AO%d" % ab], partial=True)
                if h == 7:
                    r0 = s * S + qc * 512
                    P.dma("pool", lambda e: e.dma_start(out=self.ao[r0:r0 + 512, 512:1024].rearrange("(q p) n -> p q n", p=128), in_=AO[ab][:]),
                          "ao%d" % ab, reads=["AO%d" % ab], writes=["ao_d"], partial=True)
            maps = [(h, h, 0, h) for h in range(8)]
            self.dense_attn(maps, QT, KT, V, 96, 64, 96 ** -0.5, epi)
        P.end()

    def odd_attn(self, l, src):
        P = self.P
        j = l // 2
        W = self.w
        pl = nbr_patterns()
        for hh in range(2):
            P.begin()
            self.mk_eps()
            wt = P.sbuf("wC", [128, 8, 1536], BF16)
            for i in range(3):
                self.load_w_bf16(wt, W["odd_w_qkv"][j][:, i * 1024 + hh * 512:i * 1024 + hh * 512 + 512], 8, 512, "wC", c0=i * 512)
            xt = [P.sbuf("xt", [128, D], F32) for _ in range(2)]
            xb = [P.sbuf("xb", [128, D], BF16) for _ in range(2)]
            xT = [P.sbuf("xT", [128, 8, 128], BF16) for _ in range(2)]
            QT = P.sbuf("QT", [128, 8, S], BF16)
            KT = P.sbuf("KT", [128, 4, S], BF16)
            V = P.sbuf("V", [128, 16, 8, 65], BF16)
            P.op("pool", lambda e: e.memset(QT[:], 0.0), writes=["QTz"])
            qkb = [P.sbuf("qkb", [128, 1024], BF16) for _ in range(2)]
            bias = [P.sbuf("bias", [128, NPAT, 128], F32) for _ in range(2)]
            tmp = [P.sbuf("tmp", [128, 5, 128], F32) for _ in range(3)]
            E = [P.sbuf("E", [128, 5, 128], BF16) for _ in range(3)]
            rec = P.sbuf("rec", [128, 4], F32)
            AO = P.sbuf("AO", [128, 16, 512], BF16)
            P.op("dve", lambda e: e.memset(V[:, :, :, 64:65], 1.0), writes=["Vones"])
            iters = [(h, qt) for h in range(8) for qt in range(16)]
            for s in range(self.nseq):
                self.qkv_seq(src, s, wt, "wC", QT, KT, V, 64, False, (xt, xb, xT, qkb, None))

                def front(it):
                    h, qt = iters[it]
                    bb = h % 2
                    if qt == 0:
                        hg = hh * 8 + h
                        P.dma("sp", lambda e: e.dma_start(out=bias[bb][:], in_=W["nbias"][j, hg]), "bias%d" % bb, writes=["bias%d" % bb])
                    blk, r0 = h // 2, (h % 2) * 64
                    lst = pl[qt]
                    n = len(lst)
                    rb = it % 3
                    sbk = 2 + 2 * rb
                    ps_s = self.PS[:, sbk * 512:sbk * 512 + 640].rearrange("p (i q) -> p i q", q=128)
                    P.op("pe", multi([lambda e, i=i, kt=kt: e.matmul(ps_s[:, i, :], KT[:, blk, kt * 128:(kt + 1) * 128], QT[:, h, qt * 128:(qt + 1) * 128], start=True, stop=True)
                                      for i, (kt, pat) in enumerate(lst)]),
                         reads=["QT", "KT"], writes=["pb%d" % sbk, "pb%d" % (sbk + 1)])
                    p0 = lst[0][1]
                    P.op("dve", lambda e: e.scalar_tensor_tensor(tmp[rb][:, 0:n, :], ps_s[:, 0:n, :], 0.125, bias[bb][:, p0:p0 + n, :], ALU.mult, ALU.add),
                         reads=["pb%d" % sbk, "pb%d" % (sbk + 1), "bias%d" % bb], writes=["tmp%d" % rb])
                    P.op("act", lambda e: e.activation(E[rb][:, 0:n, :], tmp[rb][:, 0:n, :], AF.Exp), reads=["tmp%d" % rb], writes=["E%d" % rb])

                def back(it):
                    h, qt = iters[it]
                    lst = pl[qt]
                    n = len(lst)
                    rb = it % 3
                    ob = (qt // 4) % 2
                    col = (qt % 4) * 128
                    P.op("pe", multi([lambda e, i=i, kt=kt: e.matmul(self.bank(ob)[:, col:col + 65], E[rb][:, i, :], V[:, kt, h, :], start=(i == 0), stop=(i == n - 1))
                                      for i, (kt, pat) in enumerate(lst)]),
                         reads=["E%d" % rb, "V"], writes=["pb%d" % ob], partial=(qt % 4 != 0))
                    if qt % 4 == 3:
                        ps4 = self.bank(ob).rearrange("p (q c) -> p q c", c=128)
                        q0 = qt - 3
                        P.op("dve", lambda e: e.reciprocal(rec[:], ps4[:, :, 64]), reads=["pb%d" % ob], writes=["rec"])
                        P.op("dve", lambda e: e.tensor_tensor(AO[:, q0:q0 + 4, h * 64:(h + 1) * 64], ps4[:, :, 0:64], rec[:].unsqueeze(2).to_broadcast([128, 4, 64]), ALU.mult),
                             reads=["pb%d" % ob, "rec"], writes=["AO"], partial=True)
                nit = len(iters)
                LOOK = 2
                for jx in range(nit + LOOK):
                    if jx < nit:
                        front(jx)
                    if jx >= LOOK:
                        back(jx - LOOK)
                r0_ = s * S
                P.dma("pool", lambda e: e.dma_start(out=self.ao[r0_:r0_ + S, hh * 512:(hh + 1) * 512].rearrange("(q p) n -> p q n", p=128), in_=AO[:]),
                      "aoC", reads=["AO"], writes=["ao_d"])
            P.end()

    def oproj(self, l, src):
        P = self.P
        j = l // 2
        W = self.w
        C = self.C
        P.begin()
        self.mk_eps()
        self.mk_lnt()
        wo = P.sbuf("wo", [128, 8, D], BF16)
        self.load_w_bf16(wo, (W["even_w_o"] if l % 2 == 0 else W["odd_w_o"])[j], 8, D, "wo")
        G = self.bcast_load("G", W["ln1_g"][l], D, "lnG")
        B = self.bcast_load("B", W["ln1_b"][l], D, "lnB")
        wr = P.sbuf("wr", [128, 8, 36], F32)
        P.dma("sp", lambda e: e.dma_start(out=wr[:], in_=W["w_router"][l].rearrange("(c p) n -> p c n", p=128)), "bc_wr", writes=["wr"])
        br = self.bcast_load("br", W["b_router"][l], 36, "br")

        def two(name, shape, dt):
            return [P.sbuf(name, shape, dt) for _ in range(2)]
        T_ = dict(aot=two("aot", [128, D], BF16), aoT=two("aoT", [128, 8, 128], BF16), xt=two("xt", [128, D], F32), r=two("r", [128, D], F32),
                  x1=[P.sbuf("x1", [128, D], F32) for _ in range(4)], x1b=[P.sbuf("x1b", [128, D], BF16) for _ in range(4)], x1T=two("x1T", [128, 8, 128], F32),
                  lg=two("lg", [128, 36], F32), sm=two("sm", [128, 16], F32), pen=two("pen", [128, 4], F32), me=two("me", [128, 32], F32),
                  top8=two("top8", [128, 8], F32), idx8=two("idx8", [128, 8], U32), ef=two("ef", [128, 2], F32), oh=two("oh", [128, 2, 32], F32),
                  Mb=two("Mb", [128, 32], BF16), pos=two("pos", [128, 32], F32), prod=two("prod", [128, 2, 32], F32), slotf=two("slotf", [128, 2], F32),
                  posf=two("posf", [128, 2], F32), ov=two("ov", [128, 2], F32), sidxf=two("sidxf", [128, 2], F32), gidxf=two("gidxf", [128, 2], F32),
                  sidx=two("sidx", [128, 2], I32))
        cnt = P.sbuf("cnt", [128, 32], F32)
        P.op("dve", lambda e: e.memset(cnt[:], 0.0), writes=["cnt"])
        XS = ["xs%d" % e_ for e_ in range(32)]
        prev2 = []
        for tt in range(0, self.NT, 2):
            p1, p2 = [], []
            for sl in (0, 1):
                r_ = Rec()
                self.P = r_
                self.oproj_tile(l, src, tt + sl, sl, T_, wo, G, B, wr, br, cnt, XS)
                k = r_.l.index(("mark",))
                a_, b_ = Rec(), Rec()
                a_.l, b_.l = r_.l[:k], r_.l[k + 1:]
                p1.append(a_)
                p2.append(b_)
            self.P = P
            interleave(P, p1 + prev2)
            prev2 = p2
        interleave(P, prev2)
        P.end()

    def oproj_tile(self, l, src, tt, b, T_, wo, G, B, wr, br, cnt, XS):
        P = self.P
        C = self.C
        b4 = ((tt // 2) % 2) * 2 + b
        aot, aoT, xt, r, x1, x1b, x1T = T_["aot"][b], T_["aoT"][b], T_["xt"][b], T_["r"][b], T_["x1"][b4], T_["x1b"][b4], T_["x1T"][b]
        lg, sm, pen, me, top8, idx8, ef, oh, Mb, pos, prod = (T_[k][b] for k in ("lg", "sm", "pen", "me", "top8", "idx8", "ef", "oh", "Mb", "pos", "prod"))
        slotf, posf, ov, sidxf, gidxf, sidx = (T_[k][b] for k in ("slotf", "posf", "ov", "sidxf", "gidxf", "sidx"))

        def K(n):
            if n in ("x1", "x1b", "x1o"):
                return "%s_%d" % (n, b4)
            return "%s_%d" % (n, b)
        base = 4 * b
        pb = ["pb%d" % (base + i) for i in range(4)]
        rows = slice(tt * 128, (tt + 1) * 128)
        P.dma("sp", lambda e: e.dma_start(out=aot[:], in_=self.ao[rows, :]), K("aot"), writes=[K("aot")])
        P.dma("sp", lambda e: e.dma_start(out=xt[:], in_=src[rows, :]), K("xt"), writes=[K("xt")])
        pT = self.bankbf(base + 0).rearrange("p (c t) -> p c t", t=128)
        P.op("pe", multi([lambda e, c=c: e.transpose(pT[:, c, :], aot[:, c * 128:(c + 1) * 128], self.ident[:]) for c in range(8)]), reads=[K("aot"), "ident"], writes=[pb[0]])
        P.op("dve", lambda e: e.tensor_copy(aoT[:], pT), reads=[pb[0]], writes=[K("aoT")])
        P.op("pe", multi([lambda e, n=n, c=c: e.matmul(self.bank(base + n), aoT[:, c, :], wo[:, c, n * 512:(n + 1) * 512], start=(c == 0), stop=(c == 7)) for n in range(2) for c in range(8)]),
             reads=[K("aoT"), "wo"], writes=[pb[0], pb[1]])
        P.op("dve", lambda e: e.scalar_tensor_tensor(r[:], xt[:], ALPHA, self.bank(base, 2), ALU.mult, ALU.add), reads=[K("xt"), pb[0], pb[1]], writes=[K("r")])
        self.layer_norm(r[:], x1[:], G, B, [K("r")], [K("x1")], sl=b)
        P.op("act", lambda e: e.activation(x1b[:], x1[:], AF.Copy), reads=[K("x1")], writes=[K("x1b")])
        P.dma("act", lambda e: e.dma_start(out=self.res1[rows, :], in_=x1[:]), K("x1o"), reads=[K("x1")], writes=["res1_d"], partial=True)
        P.l.append(("mark",))
        pTf = self.PS[:, (base + 2) * 512:(base + 4) * 512].rearrange("p (c t) -> p c t", t=128)
        P.op("pe", multi([lambda e, c=c: e.transpose(pTf[:, c, :], x1[:, c * 128:(c + 1) * 128], self.identf[:]) for c in range(8)]), reads=[K("x1"), "identf"], writes=[pb[2], pb[3]])
        P.op("act", lambda e: e.activation(x1T[:], pTf, AF.Copy), reads=[pb[2], pb[3]], writes=[K("x1T")])
        pl_ = self.bank(base + 2)[:, 0:36]
        P.op("pe", multi([lambda e, c=c: e.matmul(pl_, x1T[:, c, :], wr[:, c, :], start=(c == 0), stop=(c == 7)) for c in range(8)]), reads=[K("x1T"), "wr"], writes=[pb[2]])
        P.op("dve", lambda e: e.tensor_tensor(lg[:], pl_, br[:], ALU.add), reads=[pb[2], "br"], writes=[K("lg")])
        P.op("dve", lambda e: e.tensor_reduce(sm[:, 0:1], lg[:, 0:4], AX.X, ALU.max), reads=[K("lg")], writes=[K("sm0")])
        P.op("dve", lambda e: e.tensor_scalar(pen[:], lg[:, 0:4], sm[:, 0:1], None, ALU.subtract), reads=[K("lg"), K("sm0")], writes=[K("pen")])
        P.op("act", lambda e: e.activation(sm[:, 4:8], pen[:], AF.Exp, accum_out=sm[:, 1:2]), reads=[K("pen")], writes=[K("sm1")])
        P.op("dve", lambda e: e.reciprocal(sm[:, 2:3], sm[:, 1:2]), reads=[K("sm1")], writes=[K("sm2")])
        P.op("dve", lambda e: e.tensor_scalar(pen[:], pen[:], 0.0, 10000.0, ALU.is_ge, ALU.mult), reads=[K("pen")], writes=[K("pen")])
        P.op("dve", lambda e: e.tensor_scalar(pen[:], pen[:], -10000.0, None, ALU.add), reads=[K("pen")], writes=[K("pen")])
        P.op("dve", lambda e: e.tensor_tensor(me[:].rearrange("p (g k) -> p g k", k=8), lg[:, 4:36].rearrange("p (g k) -> p g k", k=8),
                                              pen[:].unsqueeze(2).to_broadcast([128, 4, 8]), ALU.add), reads=[K("lg"), K("pen")], writes=[K("me")])
        P.op("dve", lambda e: e.max(top8[:], me[:]), reads=[K("me")], writes=[K("top8")])
        P.op("dve", lambda e: e.max_index(idx8[:], top8[:], me[:]), reads=[K("me"), K("top8")], writes=[K("idx8")])
        P.op("dve", lambda e: e.tensor_tensor(sm[:, 8:9], top8[:, 1:2], top8[:, 0:1], ALU.subtract), reads=[K("top8")], writes=[K("sm8")])
        P.op("act", lambda e: e.activation(sm[:, 9:10], sm[:, 8:9], AF.Exp), reads=[K("sm8")], writes=[K("sm9")])
        P.op("dve", lambda e: e.tensor_scalar(sm[:, 10:11], sm[:, 9:10], 1.0, None, ALU.add), reads=[K("sm9")], writes=[K("sm10")])
        P.op("dve", lambda e: e.reciprocal(sm[:, 10:11], sm[:, 10:11]), reads=[K("sm10")], writes=[K("sm10")])
        P.op("dve", lambda e: e.tensor_tensor(sm[:, 12:13], sm[:, 10:11], sm[:, 2:3], ALU.mult), reads=[K("sm10"), K("sm2")], writes=[K("sm12")])
        P.op("dve", lambda e: e.tensor_tensor(sm[:, 13:14], sm[:, 12:13], sm[:, 9:10], ALU.mult), reads=[K("sm12"), K("sm9")], writes=[K("sm13")])
        P.op("dve", lambda e: e.tensor_copy(ef[:], idx8[:, 0:2]), reads=[K("idx8")], writes=[K("ef")])
        for k in range(2):
            P.op("dve", lambda e, k=k: e.tensor_scalar(oh[:, k, :], self.iota32[:], ef[:, k:k + 1], None, ALU.is_equal), reads=[K("ef"), "iota32"], writes=[K("oh%d" % k)])
        P.op("dve", lambda e: e.tensor_tensor(Mb[:], oh[:, 0, :], oh[:, 1, :], ALU.add), reads=[K("oh0"), K("oh1")], writes=[K("Mb")])
        pr = self.bank(base + 3)[:, 0:32]
        pr2 = self.bank(base + 3)[:, 64:96]
        P.op("pe", multi([lambda e: e.matmul(pr, self.U[:], Mb[:], start=True, stop=True), lambda e: e.matmul(pr2, self.ones[:], Mb[:], start=True, stop=True)]),
             reads=["U", "ones", K("Mb")], writes=[pb[3]])
        P.op("dve", lambda e: e.tensor_tensor(pos[:], pr, cnt[:], ALU.add), reads=[pb[3], "cnt"], writes=[K("pos")])
        if b == 1:
            tot0 = self.bank(3)[:, 64:96]
            P.op("dve", lambda e: e.tensor_tensor(pos[:], tot0, pos[:], ALU.add), reads=["pb3", K("pos")], writes=[K("pos")])
            P.op("dve", lambda e: e.tensor_tensor(cnt[:], tot0, cnt[:], ALU.add), reads=["pb3", "cnt"], writes=["cnt"])
            P.op("dve", lambda e: e.tensor_tensor(cnt[:], pr2, cnt[:], ALU.add), reads=[pb[3], "cnt"], writes=["cnt"])
        P.op("dve", lambda e: e.tensor_tensor(prod[:], oh[:], pos[:].unsqueeze(1).to_broadcast([128, 2, 32]), ALU.mult), reads=[K("oh0"), K("oh1"), K("pos")], writes=[K("prod")])
        P.op("dve", lambda e: e.tensor_reduce(posf[:], prod[:], AX.X, ALU.add), reads=[K("prod")], writes=[K("posf")])
        P.op("dve", lambda e: e.tensor_scalar(slotf[:], ef[:], float(C), None, ALU.mult), reads=[K("ef")], writes=[K("slotf")])
        P.op("dve", lambda e: e.tensor_tensor(slotf[:], slotf[:], posf[:], ALU.add), reads=[K("slotf"), K("posf")], writes=[K("slotf")])
        P.op("dve", lambda e: e.tensor_scalar(ov[:], posf[:], float(C), None, ALU.is_ge), reads=[K("posf")], writes=[K("ov")])
        P.op("dve", lambda e: e.scalar_tensor_tensor(sidxf[:], ov[:], 1.0e6, slotf[:], ALU.mult, ALU.add), reads=[K("ov"), K("slotf")], writes=[K("sidxf")])
        P.op("dve", lambda e: e.tensor_copy(sidx[:], sidxf[:]), reads=[K("sidxf")], writes=[K("sidx")])
        P.op("dve", lambda e: e.tensor_scalar(gidxf[:], slotf[:], -1.0, float(self.NSLOT), ALU.mult, ALU.add), reads=[K("slotf")], writes=[K("gidxf")])
        P.op("dve", lambda e: e.tensor_tensor(gidxf[:], gidxf[:], ov[:], ALU.mult), reads=[K("gidxf"), K("ov")], writes=[K("gidxf")])
        P.op("dve", lambda e: e.tensor_tensor(gidxf[:], gidxf[:], slotf[:], ALU.add), reads=[K("gidxf"), K("slotf")], writes=[K("gidxf")])
        P.op("dve", lambda e: e.tensor_copy(self.gidx[:, tt, :], gidxf[:]), reads=[K("gidxf")], writes=["gidx"], partial=True)
        P.op("dve", lambda e: e.tensor_scalar(ov[:], ov[:], -1.0, 1.0, ALU.mult, ALU.add), reads=[K("ov")], writes=[K("ov")])
        P.op("dve", lambda e: e.tensor_tensor(self.gates[:, tt, :], sm[:, 12:14], ov[:], ALU.mult), reads=[K("sm12"), K("sm13"), K("ov")], writes=["gates"], partial=True)
        for k in range(0 if None else 2):
            P.dma("pool", lambda e, k=k: e.indirect_dma_start(out=self.xs[:, :], out_offset=bass.IndirectOffsetOnAxis(ap=sidx[:, k:k + 1], axis=0),
                                                              in_=x1b[:], in_offset=None, bounds_check=self.bc_reg, oob_is_err=False),
                  K("scat"), reads=[K("x1b"), K("sidx")], writes=XS, partial=True, wfull=([] if None else ["xs_chain"]))

    def experts(self, l):
        P = self.P
        W = self.w
        C = self.C
        NS = C // 128
        HW = C // 2
        P.begin()
        wg = [P.sbuf("wg", [128, 8, 512], BF16) for _ in range(2)]
        wu = [P.sbuf("wu", [128, 8, 512], BF16) for _ in range(2)]
        wd = [P.sbuf("wd", [128, 4, D], BF16) for _ in range(2)]
        xst = [P.sbuf("xst", [128, D], BF16) for _ in range(2)]
        xsT = [P.sbuf("xsT", [128, 8, C], BF16) for _ in range(2)]
        hT = [P.sbuf("hT", [128, 4, C], BF16) for _ in range(2)]
        sg = [P.sbuf("sg", [128, HW], F32) for _ in range(2)]
        yt = [P.sbuf("yt", [128, D], F32) for _ in range(2)]
        it = 0
        for ex in range(32):
            eb = ex % 2
            self.load_w_bf16(wg[eb], W["w_gate"][l, ex], 8, 512, "wg%d" % eb)
            self.load_w_bf16(wu[eb], W["w_up"][l, ex], 8, 512, "wu%d" % eb)
            self.load_w_bf16(wd[eb], W["w_down"][l, ex], 4, D, "wd%d" % eb)
            for st in range(NS):
                b = st % 2
                r0 = ex * C + st * 128
                P.dma("sp", lambda e, r0=r0, b=b: e.dma_start(out=xst[b][:], in_=self.xs[r0:r0 + 128, :]), "xst%d" % b, reads=["xs%d" % ex], writes=["xst%d" % b])
                pT = self.bankbf(st % 2).rearrange("p (c t) -> p c t", t=128)
                P.op("pe", multi([lambda e, c=c: e.transpose(pT[:, c, :], xst[b][:, c * 128:(c + 1) * 128], self.ident[:]) for c in range(8)]), reads=["xst%d" % b, "ident"], writes=["pb%d" % (st % 2)])
                if st % 2 == 0:
                    P.op("dve", lambda e, st=st, pT=pT: e.tensor_copy(xsT[eb][:, :, st * 128:(st + 1) * 128], pT), reads=["pb0"], writes=["xsT%d" % eb], partial=True)
                else:
                    P.op("act", lambda e, st=st, pT=pT: e.activation(xsT[eb][:, :, st * 128:(st + 1) * 128], pT, AF.Copy), reads=["pb1"], writes=["xsT%d" % eb], partial=True)
            for f in range(4):
                for hf in range(2):
                    ib = it % 2
                    it += 1
                    pg = self.bank(2 + 2 * ib)[:, 0:HW]
                    pu = self.bank(3 + 2 * ib)[:, 0:HW]
                    P.op("pe", multi([lambda e, c=c: e.matmul(pg, wg[eb][:, c, f * 128:(f + 1) * 128], xsT[eb][:, c, hf * HW:(hf + 1) * HW], start=(c == 0), stop=(c == 7)) for c in range(8)]),
                         reads=["wg%d" % eb, "xsT%d" % eb], writes=["pb%d" % (2 + 2 * ib)])
                    P.op("pe", multi([lambda e, c=c: e.matmul(pu, wu[eb][:, c, f * 128:(f + 1) * 128], xsT[eb][:, c, hf * HW:(hf + 1) * HW], start=(c == 0), stop=(c == 7)) for c in range(8)]),
                         reads=["wu%d" % eb, "xsT%d" % eb], writes=["pb%d" % (3 + 2 * ib)])
                    P.op("act", lambda e, ib=ib, pg=pg: e.activation(sg[ib][:], pg, AF.Silu), reads=["pb%d" % (2 + 2 * ib)], writes=["sg%d" % ib])
                    P.op("dve", lambda e, ib=ib, pu=pu, f=f, hf=hf: e.tensor_tensor(hT[eb][:, f, hf * HW:(hf + 1) * HW], sg[ib][:], pu, ALU.mult),
                         reads=["sg%d" % ib, "pb%d" % (3 + 2 * ib)], writes=["hT%d" % eb], partial=True)
            for st in range(NS):
                b = st % 2
                r0 = ex * C + st * 128
                yb0 = 6 if st % 2 == 0 else 0
                P.op("pe", multi([lambda e, n=n, f=f: e.matmul(self.bank(yb0 + n), hT[eb][:, f, st * 128:(st + 1) * 128], wd[eb][:, f, n * 512:(n + 1) * 512], start=(f == 0), stop=(f == 3)) for n in range(2) for f in range(4)]),
                     reads=["hT%d" % eb, "wd%d" % eb], writes=["pb%d" % yb0, "pb%d" % (yb0 + 1)])
                if st % 2 == 0:
                    P.op("dve", lambda e, b=b: e.tensor_copy(yt[b][:], self.bank(yb0, 2)), reads=["pb%d" % yb0, "pb%d" % (yb0 + 1)], writes=["yt%d" % b])
                else:
                    P.op("act", lambda e, b=b: e.activation(yt[b][:], self.bank(yb0, 2), AF.Copy), reads=["pb%d" % yb0, "pb%d" % (yb0 + 1)], writes=["yt%d" % b])
                P.dma("act", lambda e, r0=r0, b=b: e.dma_start(out=self.ys[r0:r0 + 128, :], in_=yt[b][:]), "yo%d" % b, reads=["yt%d" % b], writes=["ys%d" % ex], partial=True)
        P.end()

    def combine(self, l, dst):
        P = self.P
        W = self.w
        P.begin()
        self.mk_eps()
        self.mk_lnt()
        G = self.bcast_load("G", W["ln2_g"][l], D, "lnG")
        B = self.bcast_load("B", W["ln2_b"][l], D, "lnB")
        x1 = [P.sbuf("x1", [128, D], F32) for _ in range(4)]
        y0 = [P.sbuf("y0", [128, D], F32) for _ in range(4)]
        y1 = [P.sbuf("y1", [128, D], F32) for _ in range(4)]
        f = [P.sbuf("f", [128, D], F32) for _ in range(2)]
        o = [P.sbuf("o", [128, D], F32) for _ in range(2)]
        YS = ["ys%d" % e_ for e_ in range(32)] + ["ysd"]

        def tile(tt, b):
            P = self.P
            b4 = ((tt // 2) % 2) * 2 + b
            rows = slice(tt * 128, (tt + 1) * 128)
            P.dma("sp", lambda e: e.dma_start(out=x1[b4][:], in_=self.res1[rows, :]), "x1l%d" % b4, reads=["res1_d"], writes=["x1_%d" % b4])
            for k, yb in enumerate((y0, y1)):
                P.dma("pool", lambda e, k=k, yb=yb: e.indirect_dma_start(out=yb[b4][:], out_offset=None, in_=self.ys[:, :],
                                                                        in_offset=bass.IndirectOffsetOnAxis(ap=self.gidx[:, tt, k:k + 1], axis=0)),
                      "gat%d_%d" % (k, b4), reads=YS + ["gidx"], writes=["y%d_%d" % (k, b4)])
            P.l.append(("mark",))
            P.op("dve", lambda e: e.tensor_scalar(f[b][:], y0[b4][:], self.gates[:, tt, 0:1], None, ALU.mult), reads=["y0_%d" % b4, "gates"], writes=["f%d" % b])
            P.op("dve", lambda e: e.scalar_tensor_tensor(f[b][:], y1[b4][:], self.gates[:, tt, 1:2], f[b][:], ALU.mult, ALU.add), reads=["y1_%d" % b4, "gates", "f%d" % b], writes=["f%d" % b])
            P.op("dve", lambda e: e.scalar_tensor_tensor(f[b][:], x1[b4][:], ALPHA, f[b][:], ALU.mult, ALU.add), reads=["x1_%d" % b4, "f%d" % b], writes=["f%d" % b])
            self.layer_norm(f[b][:], o[b][:], G, B, ["f%d" % b], ["o%d" % b], sl=b)
            P.dma("act", lambda e: e.dma_start(out=dst[rows, :], in_=o[b][:]), "oo%d" % b, reads=["o%d" % b], writes=["dst_d"], partial=True)
        prev2 = []
        for tt in range(0, self.NT, 2):
            p1, p2 = [], []
            for sl in (0, 1):
                r_ = Rec()
                self.P = r_
                tile(tt + sl, sl)
                k = r_.l.index(("mark",))
                a_, b_ = Rec(), Rec()
                a_.l, b_.l = r_.l[:k], r_.l[k + 1:]
                p1.append(a_)
                p2.append(b_)
            self.P = P
            interleave(P, p1)
            interleave(P, prev2)
            prev2 = p2
        interleave(P, prev2)
        P.end()


def prep_shared(inp):
    f32 = np.float32
    sh = {}
    sh["even_w_in"] = np.ascontiguousarray(inp["even_w_in"], f32)
    sh["even_lam"] = np.ascontiguousarray(np.stack([inp["even_lam_q1"], inp["even_lam_k1"], inp["even_lam_q2"], inp["even_lam_k2"]], axis=1), f32)
    for k in ("even_subln_g", "even_q_norm_g", "even_w_uq", "even_kv_norm_g", "even_w_ukv", "even_w_o", "odd_w_qkv", "odd_w_o",
              "ln1_g", "ln1_b", "ln2_g", "ln2_b", "w_gate", "w_up", "w_down"):
        sh[k] = np.ascontiguousarray(inp[k], f32)
    sh["w_router"] = np.ascontiguousarray(np.concatenate([inp["w_router_group"], inp["w_router_expert"]], axis=2), f32)
    sh["b_router"] = np.ascontiguousarray(np.concatenate([inp["b_router_group"], inp["b_router_expert"]], axis=1), f32)
    idx = nbr_index_table()
    rb = np.asarray(inp["odd_rel_bias"], f32).reshape(2, 16, 15 * 31)
    ext = np.concatenate([rb, np.full((2, 16, 1), NEG, f32)], axis=2)
    sh["nbias"] = np.ascontiguousarray(ext[:, :, idx], f32)
    ra, rb_ = rope_host()
    sh["ropeA"], sh["ropeB"] = ra, rb_
    return sh


def kernel(**inputs):
    from concourse.bass_utils import run_bass_kernel_spmd
    n = 8
    nseq = 4
    inp = {k: np.asarray(v) for k, v in inputs.items()}
    sh = prep_shared(inp)
    x = np.ascontiguousarray(inp["x"], np.float32)
    mk = MK(nseq=nseq, C=768, layers=(0, 1, 2, 3), debug=False)
    in_maps = []
    for c in range(n):
        d = {k: v for k, v in sh.items() if k in mk.w}
        d["x"] = np.ascontiguousarray(x[c * nseq:(c + 1) * nseq].reshape(nseq * S, D))
        in_maps.append(d)
    res = run_bass_kernel_spmd(mk.nc, in_maps, core_ids=list(range(n)))
    out = np.concatenate([np.asarray(r["y"], np.float32).reshape(nseq, S, D) for r in res.results], axis=0)
    return out
```
